# Optimizing a Trainium2 kernel written in Bass

```python
import jax, jax.numpy as jnp
from jax import lax
import numpy as np

D_MODEL = 2048
BATCH = 4
SEQ = 2048
DEPTH = 1

D_MIX = D_MODEL
ATT_WIDTH = D_MIX // 2
ATT_HEAD_DIM = 64
ATT_HEADS = ATT_WIDTH // ATT_HEAD_DIM
DILATED_PAIRS = ((128, 1), (512, 4), (2048, 16))
ATT_BLOCK = 128
ROT_DIM = ATT_HEAD_DIM // 4
ROPE_THETA = 500000.0
HG_WIDTH = D_MIX - ATT_WIDTH
HG_EXPAND = 128
HG_HEADS = HG_WIDTH // HG_EXPAND
HG_HEAD_K = HG_EXPAND
HG_HEAD_V = HG_WIDTH // HG_HEADS
HG_CHUNK = 64
IN_COLS = 3 * ATT_WIDTH + 4 * HG_WIDTH
N_EXPERTS = 32
TOP_K = 4
D_FF_EXPERT = D_MODEL
SWIGLU_ALPHA = 1.702
SWIGLU_LIMIT = 7.0
MOE_BLOCK = 128
ALPHA_DEEPNORM = (2.0 * DEPTH) ** 0.25
BETA_DEEPNORM = (8.0 * DEPTH) ** -0.25
LN_EPS = 1e-5
RMS_EPS = 1e-6
NEG_INF = -1e30

kernel_name = "hymba_dilated_hgrn2_moe_deepnorm_adaln"


def layer_norm(t, g, b):
    t32 = t.astype(jnp.float32)
    mu = jnp.mean(t32, axis=-1, keepdims=True)
    var = jnp.mean(jnp.square(t32 - mu), axis=-1, keepdims=True)
    return ((t32 - mu) * lax.rsqrt(var + LN_EPS) * g + b).astype(t.dtype)


def partial_rope(t, positions):
    half = ROT_DIM // 2
    inv = ROPE_THETA ** (-(jnp.arange(0, ROT_DIM, 2, dtype=jnp.float32) / ROT_DIM))
    ang = positions.astype(jnp.float32)[..., None] * inv
    cos = jnp.cos(ang)[:, :, None, :]
    sin = jnp.sin(ang)[:, :, None, :]
    x1, x2, rest = t[..., :half], t[..., half:ROT_DIM], t[..., ROT_DIM:]
    return jnp.concatenate([x1 * cos - x2 * sin, x1 * sin + x2 * cos, rest], axis=-1)


def dilated_branch(q, k, v, window, dilation):
    B_, S_, H_, Dh = q.shape
    w_steps = window // dilation
    Q = ATT_BLOCK
    nb = -(-S_ // (dilation * Q))
    S_pad = nb * Q * dilation
    pad = ((0, 0), (0, S_pad - S_), (0, 0), (0, 0))

    def to_streams(t):
        return jnp.pad(t, pad).reshape(B_, nb, Q, dilation, H_, Dh)

    def with_prev(t):
        prev = jnp.pad(t, ((0, 0), (1, 0), (0, 0), (0, 0), (0, 0), (0, 0)))[:, :-1]
        return jnp.concatenate([prev, t], axis=2)

    qs = to_streams(q)
    kb = with_prev(to_streams(k))
    vb = with_prev(to_streams(v))
    s = jnp.einsum('bnqrhd,bnkrhd->bnrhqk', qs, kb) * (Dh ** -0.5)
    qi = jnp.arange(Q)[:, None]
    kj = jnp.arange(2 * Q)[None, :]
    dist = qi + Q - kj
    blk = jnp.arange(nb)[:, None, None]
    valid = (dist >= 0) & (dist <= w_steps) & ((blk > 0) | (kj >= Q))
    s = jnp.where(valid[None, :, None, None], s, NEG_INF)
    m = jnp.max(s, axis=-1, keepdims=True)
    p = jnp.exp(s - m)
    den = jnp.sum(p, axis=-1)
    o = jnp.einsum('bnrhqk,bnkrhd->bnqrhd', p, vb)
    o = o / jnp.moveaxis(den, -1, 2)[..., None]
    lse = jnp.moveaxis(m[..., 0] + jnp.log(den), -1, 2)
    o = o.reshape(B_, S_pad, H_, Dh)[:, :S_]
    lse = lse.reshape(B_, S_pad, H_)[:, :S_]
    return o, lse


def dilated_attention(q, k, v, positions):
    B_, S_, _ = q.shape
    shp = (B_, S_, ATT_HEADS, ATT_HEAD_DIM)
    qh = partial_rope(q.reshape(shp).astype(jnp.float32), positions)
    kh = partial_rope(k.reshape(shp).astype(jnp.float32), positions)
    vh = v.reshape(shp).astype(jnp.float32)
    outs, lses = [], []
    for window, dilation in DILATED_PAIRS:
        o, lse = dilated_branch(qh, kh, vh, window, dilation)
        outs.append(o)
        lses.append(lse)
    wts = jax.nn.softmax(jnp.stack(lses, axis=0), axis=0)
    o = jnp.sum(wts[..., None] * jnp.stack(outs, axis=0), axis=0)
    return o.reshape(B_, S_, ATT_WIDTH)


def hgrn2(q, f_logit, i, gate, lb, gnorm_w):
    B_, S_, _ = q.shape
    nc = S_ // HG_CHUNK
    f = lb + (1.0 - lb) * jax.nn.sigmoid(f_logit.astype(jnp.float32))
    log_f = jnp.log(f)
    k = 1.0 - f

    def chunks(t):
        return t.astype(jnp.float32).reshape(B_, nc, HG_CHUNK, HG_HEADS, -1).transpose(1, 0, 3, 2, 4)

    qc, kc, vc, gc = chunks(q), chunks(k), chunks(i), chunks(log_f)
    causal = jnp.tril(jnp.ones((HG_CHUNK, HG_CHUNK), dtype=bool))[:, :, None]

    def step(state, inp):
        qb, kb, vb, gb = inp
        b = jnp.cumsum(gb, axis=2)
        o_inter = jnp.einsum('bhtk,bhkv->bhtv', qb * jnp.exp(b), state)
        diff = b[:, :, :, None, :] - b[:, :, None, :, :]
        decay = jnp.exp(jnp.where(causal, diff, NEG_INF))
        a = jnp.einsum('bhtk,bhsk,bhtsk->bhts', qb, kb, decay)
        o = o_inter + jnp.einsum('bhts,bhsv->bhtv', a, vb)
        b_last = b[:, :, -1:, :]
        new_state = jnp.exp(b_last[:, :, 0, :])[..., None] * state + jnp.einsum(
            'bhsk,bhsv->bhkv', kb * jnp.exp(b_last - b), vb)
        return new_state, o

    s0 = jnp.zeros((B_, HG_HEADS, HG_HEAD_K, HG_HEAD_V), jnp.float32)
    _, o = lax.scan(step, s0, (qc, kc, vc, gc))
    o = o.transpose(1, 0, 3, 2, 4).reshape(B_, S_, HG_HEADS, HG_HEAD_V)
    o = o * lax.rsqrt(jnp.mean(jnp.square(o), axis=-1, keepdims=True) + RMS_EPS)
    o = o * gnorm_w.astype(jnp.float32).reshape(HG_HEADS, HG_HEAD_V)
    o = o * jax.nn.silu(gate.astype(jnp.float32)).reshape(B_, S_, HG_HEADS, HG_HEAD_V)
    return o.reshape(B_, S_, HG_WIDTH)


def hybrid_mixer(h, positions, w_in, lb, gnorm_w, w_o):
    proj = h @ w_in
    A, G = ATT_WIDTH, HG_WIDTH
    q_a, k_a, v_a, q_r, f_r, i_r, g_r = jnp.split(
        proj, [A, 2 * A, 3 * A, 3 * A + G, 3 * A + 2 * G, 3 * A + 3 * G], axis=-1)
    att = dilated_attention(q_a, k_a, v_a, positions)
    rec = hgrn2(q_r, f_r, i_r, g_r, lb, gnorm_w)
    mixed = jnp.concatenate([att, rec], axis=-1).astype(h.dtype)
    return mixed @ w_o


def moe_ffn(h, router_w, router_b, w1, b1, w2, b2):
    B_, S_, D_ = h.shape
    T = B_ * S_
    hf = h.reshape(T, D_)
    logits = hf.astype(jnp.float32) @ router_w.astype(jnp.float32) + router_b.astype(jnp.float32)
    top_val, top_idx = lax.top_k(logits, TOP_K)
    gates = jax.nn.softmax(top_val, axis=-1)
    n_assign = T * TOP_K
    flat_e = top_idx.reshape(n_assign).astype(jnp.int32)
    flat_tok = jnp.arange(n_assign, dtype=jnp.int32) // TOP_K
    order = jnp.argsort(flat_e)
    sorted_e = flat_e[order]
    sorted_tok = flat_tok[order]
    sorted_gate = gates.reshape(n_assign)[order]
    counts = jnp.zeros((N_EXPERTS,), jnp.int32).at[flat_e].add(1)
    starts = jnp.cumsum(counts) - counts
    padded = (counts + MOE_BLOCK - 1) // MOE_BLOCK * MOE_BLOCK
    pends = jnp.cumsum(padded)
    pstarts = pends - padded
    dest = pstarts[sorted_e] + jnp.arange(n_assign, dtype=jnp.int32) - starts[sorted_e]
    n_rows = -(-n_assign // MOE_BLOCK) * MOE_BLOCK + N_EXPERTS * MOE_BLOCK
    n_rblk = n_rows // MOE_BLOCK
    row_tok = jnp.zeros((n_rows,), jnp.int32).at[dest].set(sorted_tok)
    blk_start = jnp.arange(n_rblk, dtype=jnp.int32) * MOE_BLOCK
    blk_e = jnp.minimum(jnp.searchsorted(pends, blk_start, side='right'), N_EXPERTS - 1)
    xin = hf[row_tok].reshape(n_rblk, MOE_BLOCK, D_)

    def expert_block(args):
        xb, e = args
        hb = (xb @ w1[e] + b1[e]).astype(jnp.float32)
        x_glu = jnp.minimum(hb[:, 0::2], SWIGLU_LIMIT)
        x_lin = jnp.clip(hb[:, 1::2], -SWIGLU_LIMIT, SWIGLU_LIMIT)
        act = x_glu * jax.nn.sigmoid(SWIGLU_ALPHA * x_glu) * (x_lin + 1.0)
        return act.astype(xb.dtype) @ w2[e] + b2[e]

    y = lax.map(expert_block, (xin, blk_e)).reshape(n_rows, D_)
    contrib = y[dest].astype(jnp.float32) * sorted_gate[:, None]
    out = jax.ops.segment_sum(contrib, sorted_tok, num_segments=T)
    return out.reshape(B_, S_, D_).astype(h.dtype)


def setup_inputs(seed: int = 0) -> dict:
    key = jax.random.key(seed)
    ks = jax.random.split(key, 20)
    nrm = jax.random.normal
    d_in = D_MODEL ** -0.5
    return {
        "x": nrm(ks[0], (BATCH, SEQ, D_MODEL)),
        "c": nrm(ks[1], (BATCH, D_MODEL)),
        "positions": jnp.broadcast_to(jnp.arange(SEQ, dtype=jnp.int32), (BATCH, SEQ)),
        "w_ada": nrm(ks[2], (DEPTH, D_MODEL, 6 * D_MODEL)) * (0.2 * d_in),
        "b_ada": nrm(ks[3], (DEPTH, 6 * D_MODEL)) * 0.02,
        "w_in": nrm(ks[4], (DEPTH, D_MODEL, IN_COLS)) * d_in,
        "hgrn_lb": 1.0 + 0.1 * nrm(ks[5], (DEPTH + 1, HG_WIDTH)),
        "gnorm_w": 1.0 + 0.1 * nrm(ks[6], (DEPTH, HG_WIDTH)),
        "w_o": nrm(ks[7], (DEPTH, D_MIX, D_MODEL)) * (D_MIX ** -0.5 * BETA_DEEPNORM),
        "ln1_g": 1.0 + 0.1 * nrm(ks[8], (DEPTH, D_MODEL)),
        "ln1_b": 0.02 * nrm(ks[9], (DEPTH, D_MODEL)),
        "router_w": nrm(ks[10], (DEPTH, D_MODEL, N_EXPERTS)) * d_in,
        "router_b": 0.01 * nrm(ks[11], (DEPTH, N_EXPERTS)),
        "w1": nrm(ks[12], (DEPTH, N_EXPERTS, D_MODEL, 2 * D_FF_EXPERT)) * d_in,
        "b1": 0.02 * nrm(ks[13], (DEPTH, N_EXPERTS, 2 * D_FF_EXPERT)),
        "w2": nrm(ks[14], (DEPTH, N_EXPERTS, D_FF_EXPERT, D_MODEL)) * (D_FF_EXPERT ** -0.5 * BETA_DEEPNORM),
        "b2": 0.02 * nrm(ks[15], (DEPTH, N_EXPERTS, D_MODEL)),
        "ln2_g": 1.0 + 0.1 * nrm(ks[16], (DEPTH, D_MODEL)),
        "ln2_b": 0.02 * nrm(ks[17], (DEPTH, D_MODEL)),
    }


def reference(x, c, positions, w_ada, b_ada, w_in, hgrn_lb, gnorm_w, w_o, ln1_g, ln1_b,
              router_w, router_b, w1, b1, w2, b2, ln2_g, ln2_b):
    lb_all = jnp.cumsum(jax.nn.softmax(hgrn_lb.astype(jnp.float32), axis=0), axis=0)
    for l in range(DEPTH):
        mod = jax.nn.silu(c) @ w_ada[l] + b_ada[l]
        sh_a, sc_a, gt_a, sh_f, sc_f, gt_f = [m[:, None, :] for m in jnp.split(mod, 6, axis=-1)]
        h = x * (1.0 + sc_a) + sh_a
        mix = hybrid_mixer(h, positions, w_in[l], lb_all[l], gnorm_w[l], w_o[l])
        x = layer_norm(ALPHA_DEEPNORM * x + (1.0 + gt_a) * mix, ln1_g[l], ln1_b[l])
        h = x * (1.0 + sc_f) + sh_f
        ff = moe_ffn(h, router_w[l], router_b[l], w1[l], b1[l], w2[l], b2[l])
        x = layer_norm(ALPHA_DEEPNORM * x + (1.0 + gt_f) * ff, ln2_g[l], ln2_b[l])
    return x
```

```python
import numpy as np
from contextlib import ExitStack
import concourse.bass as bass
import concourse.mybir as mybir
from concourse.bass_utils import run_bass_kernel_spmd

F32 = mybir.dt.float32
BF16 = mybir.dt.bfloat16
I32 = mybir.dt.int32
AF = mybir.ActivationFunctionType
ALU = mybir.AluOpType
AX = mybir.AxisListType

D = 2048
T = 1024
NE = 32
CAP = 256
ALPHA = 2.0 ** 0.25
PI = float(np.pi)

_c = {}
_off = 0
for _n, _w in [("ident", 128), ("mcur", 128), ("mprev", 128), ("mprevctx", 128), ("mask3", 64), ("rmat", 128),
               ("ltri", 128), ("chunkind", 2), ("bdmask", 128), ("ustrict", 128), ("ones", 128), ("iota", 256),
               ("iotac", 2), ("invp", 1), ("signp", 1), ("flag", 1)]:
    _c[_n] = (_off, _w)
    _off += _w
NCONST = _off


def make_consts(half):
    c = np.zeros((128, NCONST), np.float32)

    def put(name, arr):
        o, w = _c[name]
        c[:, o:o + w] = arr
    k = np.arange(128)[:, None]
    q = np.arange(128)[None, :]
    flag = 1.0 if half == 1 else 0.0
    put("ident", np.eye(128))
    put("mcur", (k <= q))
    put("mprev", (k >= q))
    put("mprevctx", (k >= q) * flag)
    m3 = (k <= (np.arange(64)[None, :] + 64)).astype(np.float32)
    m3[:64] *= flag
    put("mask3", m3)
    rm = np.zeros((128, 128), np.float32)
    invp = np.zeros((128, 1), np.float32)
    signp = np.zeros((128, 1), np.float32)
    for m in range(128):
        d = m % 64
        if d < 8:
            rm[m + 8, m] = 1.0
            signp[m] = -1.0
        elif d < 16:
            rm[m - 8, m] = 1.0
            signp[m] = 1.0
        if d < 16:
            invp[m] = 500000.0 ** (-(2.0 * (d % 8)) / 16.0)
    put("rmat", rm)
    put("invp", invp)
    put("signp", signp)
    same = (k // 64) == (q // 64)
    put("ltri", (k <= q) & same)
    put("bdmask", (k <= q) & same)
    ci = np.zeros((128, 2), np.float32)
    ci[:64, 0] = 1
    ci[64:, 1] = 1
    put("chunkind", ci)
    put("ustrict", (k < q))
    put("ones", np.ones((128, 128)))
    put("iota", np.tile(np.arange(256, dtype=np.float32)[None], (128, 1)))
    put("iotac", np.stack([np.arange(128), np.arange(128) + 128], 1))
    put("flag", np.full((128, 1), flag))
    return c


class Buf:
    __slots__ = ("name", "w", "r")

    def __init__(self, name):
        self.name = name
        self.w = None
        self.r = []


class Eng:
    def __init__(self, name, h, sem):
        self.name, self.h, self.sem, self.cnt, self.seen = name, h, sem, 0, {}


class Chan:
    def __init__(self, sem):
        self.sem, self.cnt = sem, 0


class Sched:
    def __init__(self, nc, es):
        self.nc, self.es = nc, es
        self.nsem = 0
        self.pe = Eng("pe", nc.tensor, self.sem("pe"))
        self.act = Eng("act", nc.scalar, self.sem("act"))
        self.dve = Eng("dve", nc.vector, self.sem("dve"))
        self.pool = Eng("pool", nc.gpsimd, self.sem("pool"))
        self.sp = Eng("sp", nc.sync, self.sem("sp"))
        self.engs = [self.pe, self.act, self.dve, self.pool, self.sp]
        self.chans = []

    def sem(self, name):
        self.nsem += 1
        return self.es.enter_context(self.nc.semaphore("s_%s_%d" % (name, self.nsem)))

    def chan(self, name="c"):
        c = Chan(self.sem(name))
        self.chans.append(c)
        return c

    def _wait(self, eng, ev):
        if ev is None:
            return
        sem, val = ev
        if eng.seen.get(sem, 0) >= val:
            return
        eng.h.wait_ge(sem, val)
        eng.seen[sem] = val

    def op(self, eng, fn, reads=(), writes=()):
        for b in reads:
            self._wait(eng, b.w)
        for b in writes:
            if b.w is not None and b.w[0] is not eng.sem:
                self._wait(eng, b.w)
            for ev in b.r:
                if ev[0] is not eng.sem:
                    self._wait(eng, ev)
        ins = fn(eng.h)
        eng.cnt += 1
        ins.then_inc(eng.sem, 1)
        ev = (eng.sem, eng.cnt)
        for b in writes:
            b.w = ev
            b.r = []
        for b in reads:
            b.r = [e for e in b.r if e[0] is not eng.sem] + [ev]
        return ev

    def dma(self, q, out, in_, chan, reads=(), writes=()):
        for b in reads:
            self._wait(q, b.w)
        for b in writes:
            self._wait(q, b.w)
            for ev in b.r:
                self._wait(q, ev)
        q.h.dma_start(out=out, in_=in_).then_inc(chan.sem, 16)
        chan.cnt += 16
        ev = (chan.sem, chan.cnt)
        for b in writes:
            b.w = ev
            b.r = []
        for b in reads:
            b.r = b.r + [ev]
        return ev

    def barrier(self):
        for e in self.engs:
            for f in self.engs:
                if f is not e and f.cnt > 0:
                    self._wait(e, (f.sem, f.cnt))
            for c in self.chans:
                if c.cnt > 0:
                    self._wait(e, (c.sem, c.cnt))


def build_nc(stage=99, debug=False):
    nc = bass.Bass("TRN2", target_bir_lowering=False)

    def din(name, shape, dt=F32):
        return nc.dram_tensor(name, list(shape), dt, kind="ExternalInput").ap()
    x_own = din("x_own", [T, D])
    x_ctx = din("x_ctx", [T, D])
    cT = din("cT", [128, 16])
    pos = din("pos", [1, 2048], I32)
    w_ada = din("w_ada", [D, 6 * D])
    b_ada = din("b_ada", [1, 6 * D])
    w_in = din("w_in", [D, 7168])
    hgrn_lb = din("hgrn_lb", [1, 2048])
    gnw = din("gnw", [128, 8])
    w_o = din("w_o", [D, D])
    lnp = din("lnp", [4, D])
    router_w = din("router_w", [128, 16 * NE])
    router_b = din("router_b", [1, NE])
    if stage >= 5:
        w1 = din("w1", [NE, D, 2 * D])
        b1 = din("b1", [128, NE * 32])
        w2 = din("w2", [NE, D, D])
        b2 = din("b2", [NE, D])
    consts = din("consts", [128, NCONST])
    out = nc.dram_tensor("out", [T, D], F32, kind="ExternalOutput").ap()
    mod_d = nc.dram_tensor("mod_d", [6, D], F32, kind="Internal").ap()
    x1_d = nc.dram_tensor("x1_d", [T, D], F32, kind="Internal").ap()
    dbg = {}
    if debug:
        dbg["mixT"] = nc.dram_tensor("d_mixT", [128, 16 * T], BF16, kind="ExternalOutput").ap()
        dbg["x1"] = nc.dram_tensor("d_x1", [T, D], F32, kind="ExternalOutput").ap()
        dbg["mod"] = nc.dram_tensor("d_mod", [6, D], F32, kind="ExternalOutput").ap()
        dbg["logits"] = nc.dram_tensor("d_logits", [128, 8 * NE], F32, kind="ExternalOutput").ap()
        dbg["ffn"] = nc.dram_tensor("d_ffn", [T, D], F32, kind="ExternalOutput").ap()

    with ExitStack() as es:
        S = Sched(nc, es)
        PE, ACT, DVE, POOL, SP = S.pe, S.act, S.dve, S.pool, S.sp
        uid = [0]

        def sb(scope, shape, dt, name="t"):
            uid[0] += 1
            return scope.enter_context(nc.sbuf_tensor("%s_%d" % (name, uid[0]), list(shape), dt))

        def ps(scope, shape, dt=F32, name="p"):
            uid[0] += 1
            return scope.enter_context(nc.psum_tensor("%s_%d" % (name, uid[0]), list(shape), dt))

        cst = sb(es, [128, NCONST], F32, "cst")
        cstb = sb(es, [128, NCONST], BF16, "cstb")
        B_cst = Buf("cst")
        ch_misc = S.chan("misc")
        S.dma(SP, cst[:], consts, ch_misc, writes=[B_cst])
        S.op(DVE, lambda e: e.tensor_copy(out=cstb[:], in_=cst[:]), reads=[B_cst], writes=[B_cst])

        def C(name, bf=False, rows=slice(0, 128)):
            o, w = _c[name]
            return (cstb if bf else cst)[rows, o:o + w]

        def make_ring(scope, nslot, ncols):
            ring = [sb(scope, [128, 16, ncols], BF16, "ring") for _ in range(nslot)]
            ring_b = [Buf("ring%d" % i) for i in range(nslot)]
            ring_c = [S.chan("ring") for _ in range(nslot)]
            ring_i = [0]

            def load_w(src2d, nc_):
                i = ring_i[0] % nslot
                ring_i[0] += 1
                S.dma(POOL, ring[i][:, :, 0:nc_], src2d.rearrange("(k p) c -> p k c", p=128), ring_c[i], writes=[ring_b[i]])
                return ring[i], ring_b[i]
            return load_w

        S.barrier()

        B_mod = [Buf("mod%d" % i) for i in range(6)]
        with ExitStack() as ph:
            NSLOT = 3
            load_w = make_ring(ph, NSLOT, 512)
            cts = sb(ph, [128, 16], F32)
            sg = sb(ph, [128, 16], F32)
            silb = sb(ph, [128, 16], BF16)
            brow = [sb(ph, [1, 512], F32) for _ in range(2)]
            mrow = [sb(ph, [1, 512], F32) for _ in range(2)]
            psA = [ps(ph, [1, 512]) for _ in range(2)]
            Bc, Bsil = Buf("c"), Buf("sil")
            Bbrow = [Buf("brow"), Buf("brow")]
            Bmrow = [Buf("mrow"), Buf("mrow")]
            BpsA = [Buf("psA"), Buf("psA")]
            ch_b = [S.chan("brow"), S.chan("brow")]
            ch_m = [S.chan("mrow"), S.chan("mrow")]
            S.dma(SP, cts[:], cT, ch_misc, writes=[Bc])
            S.op(ACT, lambda e: e.activation(out=sg[:], in_=cts[:], func=AF.Sigmoid), reads=[Bc], writes=[Bsil])
            S.op(DVE, lambda e: e.tensor_tensor(out=silb[:], in0=cts[:], in1=sg[:], op=ALU.mult), reads=[Bc, Bsil], writes=[Bsil])
            pend = [load_w(w_ada[:, g * 512:(g + 1) * 512], 512) for g in range(NSLOT)]
            for g in range(24):
                wt, wb = pend.pop(0)
                k = g % 2
                S.dma(SP, brow[k][:], b_ada[0:1, g * 512:(g + 1) * 512], ch_b[k], writes=[Bbrow[k]])

                def mm(e, wt=wt, k=k):
                    for kc in range(16):
                        ins = e.matmul(psA[k][:], lhsT=silb[:, kc:kc + 1], rhs=wt[:, kc, :], start=(kc == 0), stop=(kc == 15))
                    return ins
                S.op(PE, mm, reads=[wb, Bsil], writes=[BpsA[k]])
                addc = 1.0 if (g // 4) in (1, 2, 4, 5) else 0.0
                S.op(DVE, lambda e, k=k, addc=addc: e.scalar_tensor_tensor(out=mrow[k][:], in0=psA[k][:], scalar=addc, in1=brow[k][:], op0=ALU.add, op1=ALU.add),
                     reads=[BpsA[k], Bbrow[k]], writes=[Bmrow[k]])
                S.dma(SP, mod_d[g // 4:g // 4 + 1, (g % 4) * 512:(g % 4 + 1) * 512], mrow[k][:], ch_m[k], reads=[Bmrow[k]], writes=[B_mod[g // 4]])
                if g + NSLOT < 24:
                    pend.append(load_w(w_ada[:, (g + NSLOT) * 512:(g + NSLOT + 1) * 512], 512))
            S.barrier()
        if debug:
            with ExitStack() as ph:
                t = sb(ph, [6, D], F32)
                Bt = Buf("t")
                S.dma(SP, t[:], mod_d, ch_misc, reads=B_mod, writes=[Bt])
                S.dma(SP, dbg["mod"], t[:], ch_misc, reads=[Bt])
                S.barrier()

        def bcast_row(scope, src_row_ap, chan, reads=()):
            t = sb(scope, [128, D], F32, "row")
            b = Buf("row")
            S.dma(SP, t[:], src_row_ap.partition_broadcast(128), chan, reads=reads, writes=[b])
            return t, b

        gate = sb(es, [128, 8, NE], F32, "gate")
        mfall = sb(es, [128, 8, NE], F32, "mfall")
        mb = sb(es, [128, 8, NE], BF16, "mb")
        posm = sb(es, [128, 8, NE], F32, "posm")
        posmb = sb(es, [128, 8, NE], BF16, "posmb")
        logit = sb(es, [128, 8, NE], F32, "logit")
        Bh2bf, Bgate, Bmf, Bmb, Bposm, Bposmb, Blogit = [Buf(n) for n in ("h2bf", "gate", "mf", "mb", "posm", "posmb", "logit")]
        mixT_scope = ExitStack()
        es.enter_context(mixT_scope)
        mixT = sb(mixT_scope, [128, 16, T], BF16, "mixT")
        B_mix = [Buf("mix%d" % i) for i in range(16)]
        h2bf = mixT[:].rearrange("p (j a) t -> p j (a t)", a=2)

        with ExitStack() as phBC:
            hT = sb(phBC, [128, 16, 2048], BF16, "hT")
            Ct = sb(phBC, [128, 2048], BF16, "Ct")
            St = sb(phBC, [128, 2048], BF16, "St")
            with ExitStack() as ph2:
                posi = sb(ph2, [128, 2048], I32)
                t1 = sb(ph2, [128, 2048], F32)
                t2 = sb(ph2, [128, 2048], F32)
                t3 = sb(ph2, [128, 2048], F32)
                Bp, B1, B2, B3, BCt, BSt = [Buf(n) for n in ("posi", "t1", "t2", "t3", "Ct", "St")]
                S.dma(SP, posi[:], pos.partition_broadcast(128), ch_misc, writes=[Bp])
                S.op(DVE, lambda e: e.tensor_copy(out=t1[:], in_=posi[:]), reads=[Bp], writes=[B1])
                S.op(DVE, lambda e: e.tensor_scalar(out=t1[:], in0=t1[:], scalar1=C("invp"), scalar2=None, op0=ALU.mult), reads=[B1], writes=[B1])
                for which, shift in (("sin", 0.0), ("cos", PI / 2)):
                    S.op(DVE, lambda e, shift=shift: e.tensor_scalar(out=t2[:], in0=t1[:], scalar1=shift, scalar2=None, op0=ALU.add), reads=[B1], writes=[B2])
                    S.op(DVE, lambda e: e.tensor_scalar(out=t3[:], in0=t2[:], scalar1=1.0 / (2 * PI), scalar2=None, op0=ALU.mult), reads=[B2], writes=[B3])
                    S.op(DVE, lambda e: e.tensor_copy(out=posi[:], in_=t3[:]), reads=[B3], writes=[Bp])
                    S.op(DVE, lambda e: e.tensor_copy(out=t3[:], in_=posi[:]), reads=[Bp], writes=[B3])
                    S.op(DVE, lambda e: e.scalar_tensor_tensor(out=t3[:], in0=t3[:], scalar=-2 * PI, in1=t2[:], op0=ALU.mult, op1=ALU.add), reads=[B3, B2], writes=[B3])
                    S.op(ACT, lambda e: e.activation(out=t2[:], in_=t3[:], func=AF.Sin), reads=[B3], writes=[B2])
                    if which == "sin":
                        S.op(DVE, lambda e: e.tensor_scalar(out=St[:], in0=t2[:], scalar1=C("signp"), scalar2=None, op0=ALU.mult), reads=[B2], writes=[BSt])
                    else:
                        S.op(DVE, lambda e: e.tensor_copy(out=Ct[:], in_=t2[:]), reads=[B2], writes=[BCt])
                S.barrier()
            with ExitStack() as ph:
                sc1a, Bsc = bcast_row(ph, mod_d[1:2, :], ch_misc, reads=[B_mod[1]])
                sha, Bsh = bcast_row(ph, mod_d[0:1, :], ch_misc, reads=[B_mod[0]])
                xin = [sb(ph, [128, D], F32, "xin") for _ in range(2)]
                Bx = [Buf("xin"), Buf("xin")]
                chx = [S.chan("xin"), S.chan("xin")]
                hb = sb(ph, [128, D], BF16, "hb")
                Bhb = Buf("hb")
                pst = [ps(ph, [128, 8, 128], BF16) for _ in range(2)]
                Bpst = [Buf("pst"), Buf("pst")]
                BhT = Buf("hT")
                for tt in range(16):
                    k = tt % 2
                    src = x_ctx[tt * 128:(tt + 1) * 128, :] if tt < 8 else x_own[(tt - 8) * 128:(tt - 7) * 128, :]
                    S.dma(SP, xin[k][:], src, chx[k], writes=[Bx[k]])
                    S.op(DVE, lambda e, k=k: e.tensor_tensor(out=xin[k][:], in0=xin[k][:], in1=sc1a[:], op=ALU.mult), reads=[Bx[k], Bsc], writes=[Bx[k]])
                    S.op(DVE, lambda e, k=k: e.tensor_tensor(out=hb[:], in0=xin[k][:], in1=sha[:], op=ALU.add), reads=[Bx[k], Bsh], writes=[Bhb])
                    for hh in range(2):
                        def tr(e, hh=hh):
                            for j in range(8):
                                kc = hh * 8 + j
                                ins = e.transpose(out=pst[hh][:, j, :], in_=hb[:, kc * 128:(kc + 1) * 128], identity=C("ident", True))
                            return ins
                        S.op(PE, tr, reads=[Bhb], writes=[Bpst[hh]])
                        S.op(ACT, lambda e, hh=hh, tt=tt: e.activation(out=hT[:, hh * 8:(hh + 1) * 8, tt * 128:(tt + 1) * 128], in_=pst[hh][:], func=AF.Copy),
                             reads=[Bpst[hh]], writes=[BhT])
                S.barrier()
            if stage >= 2:
                with ExitStack() as ph:
                    load_w = make_ring(ph, 3, 256)
                    V = {o: sb(ph, [128, 16, 256], BF16, "V%d" % o) for o in (1, 4, 16)}
                    QT = sb(ph, [128, 2, T], BF16, "QT")
                    KT = sb(ph, [128, 2, 2048], BF16, "KT")
                    qraw = sb(ph, [128, 512], BF16, "qraw")
                    ta = sb(ph, [128, 512], F32, "ta")
                    tb = sb(ph, [128, 512], F32, "tb")
                    acc = sb(ph, [128, 2, T], F32, "acc")
                    rden = sb(ph, [128, T], F32, "rden")
                    mpair = sb(ph, [128, 3, 256], BF16, "mpair")
                    pe_sb = [sb(ph, [128, 256], BF16, "pe") for _ in range(2)]
                    pm_sb = [sb(ph, [128, 256], BF16, "pm") for _ in range(2)]
                    psP = [ps(ph, [128, 512]) for _ in range(2)]
                    psR = ps(ph, [128, 512])
                    psS = [ps(ph, [128, 512]) for _ in range(2)]
                    psO = ps(ph, [128, 2, 512])
                    BV = {o: Buf("V") for o in (1, 4, 16)}
                    BQT, BKT, Bqraw, Bta, Btb, Bacc, Brden, Bmp = [Buf(n) for n in ("QT", "KT", "qraw", "ta", "tb", "acc", "rden", "mp")]
                    Bpe = [Buf("pe"), Buf("pe")]
                    Bpm = [Buf("pm"), Buf("pm")]
                    BpsP = [Buf("psP"), Buf("psP")]
                    BpsS = [Buf("psS"), Buf("psS")]
                    BpsR, BpsO = Buf("psR"), Buf("psO")
                    onesb = C("ones", True)
                    S.op(DVE, lambda e: e.tensor_copy(out=mpair[:, 0, 0:128], in_=C("mprev", True)), writes=[Bmp])
                    S.op(DVE, lambda e: e.tensor_copy(out=mpair[:, 0, 128:256], in_=C("mcur", True)), writes=[Bmp])
                    S.op(DVE, lambda e: e.tensor_copy(out=mpair[:, 1, 0:128], in_=C("mprevctx", True)), writes=[Bmp])
                    S.op(DVE, lambda e: e.tensor_copy(out=mpair[:, 1, 128:256], in_=C("mcur", True)), writes=[Bmp])
                    for s4 in range(4):
                        S.op(DVE, lambda e, s4=s4: e.tensor_copy(out=mpair[:, 2, s4 * 64:(s4 + 1) * 64], in_=C("mask3", True)), writes=[Bmp])
                    pcnt = [0]
                    scnt = [0]

                    def proj_fm(wt, wb, colsl, tok0, dst_ap, scale, BD):
                        k = pcnt[0] % 2
                        pcnt[0] += 1

                        def mm(e):
                            for kc in range(16):
                                ins = e.matmul(psP[k][:], lhsT=wt[:, kc, colsl], rhs=hT[:, kc, tok0:tok0 + 512], start=(kc == 0), stop=(kc == 15))
                            return ins
                        S.op(PE, mm, reads=[wb], writes=[BpsP[k]])
                        S.op(ACT, lambda e: e.activation(out=qraw[:], in_=psP[k][:], func=AF.Identity, scale=scale), reads=[BpsP[k]], writes=[Bqraw])
                        S.op(PE, lambda e: e.matmul(psR[:], lhsT=C("rmat", True), rhs=qraw[:], start=True, stop=True), reads=[Bqraw], writes=[BpsR])
                        S.op(DVE, lambda e: e.tensor_tensor(out=ta[:], in0=qraw[:], in1=Ct[:, tok0:tok0 + 512], op=ALU.mult), reads=[Bqraw], writes=[Bta])
                        S.op(DVE, lambda e: e.tensor_tensor(out=tb[:], in0=psR[:], in1=St[:, tok0:tok0 + 512], op=ALU.mult), reads=[BpsR], writes=[Btb])
                        S.op(DVE, lambda e: e.tensor_tensor(out=dst_ap, in0=ta[:], in1=tb[:], op=ALU.add), reads=[Bta, Btb], writes=[BD])

                    def tokset(o, kt):
                        if o == 1:
                            return slice(kt * 128, (kt + 1) * 128)
                        if o == 4:
                            m, r = kt // 4, kt % 4
                            return slice(512 * m + r, 512 * (m + 1), 4)
                        return slice(kt, 2048, 16)

                    def score_unit(lhs_list, rhs_list, ncol, mask_ap, pv_list, out_col0, pb):
                        k = scnt[0] % 2
                        scnt[0] += 1
                        n = len(lhs_list)

                        def mm(e):
                            for i in range(n):
                                ins = e.matmul(psS[k][:, i * ncol:(i + 1) * ncol], lhsT=lhs_list[i], rhs=rhs_list[i], start=True, stop=True)
                            return ins
                        S.op(PE, mm, reads=[BKT, BQT], writes=[BpsS[k]])
                        S.op(ACT, lambda e: e.activation(out=pe_sb[k][:, 0:n * ncol], in_=psS[k][:, 0:n * ncol], func=AF.Exp), reads=[BpsS[k]], writes=[Bpe[k]])
                        S.op(DVE, lambda e: e.tensor_tensor(out=pm_sb[k][:, 0:n * ncol], in0=pe_sb[k][:, 0:n * ncol], in1=mask_ap, op=ALU.mult), reads=[Bpe[k], Bmp], writes=[Bpm[k]])

                        def pv(e):
                            for t_ in range(2):
                                for i in range(n):
                                    vt, oc, st, sp_ = pv_list[i]
                                    lhs = vt if t_ == 0 else onesb
                                    ins = e.matmul(psO[:, t_, out_col0 + oc:out_col0 + oc + ncol], lhsT=lhs, rhs=pm_sb[k][:, i * ncol:(i + 1) * ncol], start=st, stop=sp_)
                            return ins
                        S.op(PE, pv, reads=[Bpm[k], BV[1], BV[4], BV[16]], writes=[BpsO])

                    for grp in range(4):
                        c0 = grp * 256
                        wq, wqb = load_w(w_in[:, c0:c0 + 256], 256)
                        wk, wkb = load_w(w_in[:, 1024 + c0:1024 + c0 + 256], 256)
                        wv, wvb = load_w(w_in[:, 2048 + c0:2048 + c0 + 256], 256)
                        for j in range(2):
                            for nt in range(2):
                                proj_fm(wq, wqb, slice(j * 128, (j + 1) * 128), 1024 + nt * 512, QT[:, j, nt * 512:(nt + 1) * 512], 0.125, BQT)
                            for nt in range(4):
                                proj_fm(wk, wkb, slice(j * 128, (j + 1) * 128), nt * 512, KT[:, j, nt * 512:(nt + 1) * 512], 1.0, BKT)
                        for o in (1, 4, 16):
                            for kt in range(16):
                                k = pcnt[0] % 2
                                pcnt[0] += 1
                                tok = tokset(o, kt)

                                def mm(e, k=k, tok=tok):
                                    for kc in range(16):
                                        ins = e.matmul(psP[k][:, 0:256], lhsT=hT[:, kc, tok], rhs=wv[:, kc, 0:256], start=(kc == 0), stop=(kc == 15))
                                    return ins
                                S.op(PE, mm, reads=[wvb], writes=[BpsP[k]])
                                S.op(ACT, lambda e, k=k, o=o, kt=kt: e.activation(out=V[o][:, kt, :], in_=psP[k][:, 0:256], func=AF.Copy), reads=[BpsP[k]], writes=[BV[o]])
                        for j in range(2):
                            for hh in range(2):
                                pb = 64 * hh
                                KTh = KT[pb:pb + 64, j, :]
                                QTh = QT[pb:pb + 64, j, :]
                                vc = slice(j * 128, (j + 1) * 128)
                                for batch in range(2):
                                    for bi in range(4):
                                        n = 8 + batch * 4 + bi
                                        qs = slice((n - 8) * 128, (n - 7) * 128)
                                        score_unit([KTh[:, (n - 1) * 128:n * 128], KTh[:, n * 128:(n + 1) * 128]], [QTh[:, qs], QTh[:, qs]], 128,
                                                   mpair[:, 1 if n == 8 else 0, :],
                                                   [(V[1][:, n - 1, vc], 0, True, False), (V[1][:, n, vc], 0, False, True)], bi * 128, pb)
                                    S.op(DVE, lambda e, batch=batch: e.tensor_copy(out=acc[:, :, batch * 512:(batch + 1) * 512], in_=psO[:]), reads=[BpsO], writes=[Bacc])
                                for m in (2, 3):
                                    for r in range(4):
                                        kp = slice(512 * (m - 1) + r, 512 * m, 4)
                                        kcu = slice(512 * m + r, 512 * (m + 1), 4)
                                        qs = slice(512 * (m - 2) + r, 512 * (m - 1), 4)
                                        score_unit([KTh[:, kp], KTh[:, kcu]], [QTh[:, qs], QTh[:, qs]], 128, mpair[:, 1 if m == 2 else 0, :],
                                                   [(V[4][:, 4 * (m - 1) + r, vc], 0, True, False), (V[4][:, 4 * m + r, vc], 0, False, True)], r * 128, pb)
                                    av = acc[:, :, 512 * (m - 2):512 * (m - 1)].rearrange("p t (i r) -> p t r i", r=4)
                                    S.op(DVE, lambda e, av=av: e.tensor_tensor(out=av, in0=av, in1=psO[:].rearrange("p t (r i) -> p t r i", r=4), op=ALU.add), reads=[BpsO, Bacc], writes=[Bacc])
                                for h8 in range(2):
                                    for rq in range(2):
                                        rs = [h8 * 8 + rq * 4 + s_ for s_ in range(4)]
                                        score_unit([KTh[:, r:2048:16] for r in rs], [QTh[:, r:1024:16] for r in rs], 64, mpair[:, 2, :],
                                                   [(V[16][:, r, vc], i_ * 64, True, True) for i_, r in enumerate(rs)], rq * 256, pb)
                                    av = acc[:, :, :].rearrange("p t (i r) -> p t r i", r=16)[:, :, h8 * 8:(h8 + 1) * 8, :]
                                    S.op(DVE, lambda e, av=av: e.tensor_tensor(out=av, in0=av, in1=psO[:].rearrange("p t (r i) -> p t r i", r=8), op=ALU.add), reads=[BpsO, Bacc], writes=[Bacc])
                                ci_ = grp * 2 + j
                                S.op(DVE, lambda e, pb=pb: e.reciprocal(out=rden[pb:pb + 64, :], in_=acc[pb:pb + 64, 1, :]), reads=[Bacc], writes=[Brden])
                                S.op(DVE, lambda e, pb=pb, ci_=ci_: e.tensor_tensor(out=mixT[pb:pb + 64, ci_, :], in0=acc[pb:pb + 64, 0, :], in1=rden[pb:pb + 64, :], op=ALU.mult),
                                     reads=[Bacc, Brden], writes=[B_mix[ci_]])
                    S.barrier()
            if stage >= 2:
                with ExitStack() as ph:
                    load_w = make_ring(ph, 4, 256)
                    lb = sb(ph, [128, 1024], F32, "lb")
                    oml = sb(ph, [128, 1024], F32, "oml")
                    gnws = sb(ph, [128, 8], F32, "gnws")
                    ke = sb(ph, [128, 16, 256], BF16, "ke")
                    vv = sb(ph, [128, 16, 256], BF16, "vv")
                    keT = sb(ph, [128, 2, 2048], BF16, "keT")
                    qeT = sb(ph, [128, 2, T], BF16, "qeT")
                    gT = sb(ph, [128, 2, T], BF16, "gT")
                    ebl = sb(ph, [128, 2, 16, 2], F32, "ebl")
                    sgm = sb(ph, [128, 256], F32, "sgm")
                    fv = sb(ph, [128, 256], F32, "fv")
                    gl = sb(ph, [128, 256], F32, "gl")
                    kk = sb(ph, [128, 256], F32, "kk")
                    eb = sb(ph, [128, 256], F32, "eb")
                    enb = sb(ph, [128, 256], F32, "enb")
                    qe_t = sb(ph, [128, 256], BF16, "qe_t")
                    S32 = [sb(ph, [128, 128], F32, "S32") for _ in range(2)]
                    Sb_ = [sb(ph, [128, 128], BF16, "Sb") for _ in range(2)]
                    stmp = [sb(ph, [128, 128], F32, "stmp") for _ in range(2)]
                    aT = [sb(ph, [128, 128], BF16, "aT") for _ in range(2)]
                    sq = sb(ph, [128, 128], BF16, "sq")
                    rt = sb(ph, [128, 128], F32, "rt")
                    ot = sb(ph, [128, 128], F32, "ot")
                    psP = [ps(ph, [128, 512]) for _ in range(2)]
                    psB = ps(ph, [128, 512])
                    psT = ps(ph, [128, 4, 128], BF16)
                    psX = ps(ph, [128, 2, 128])
                    psY = ps(ph, [128, 2, 128])
                    (Blb, Bke, Bvv, BkeT, BqeT, BgT, Bebl, Bsgm, Bfv, Bgl, Bkk, Beb, Benb, Bqe, Bsq, Brt, Bot, BpsB, BpsT, BpsXa, BpsXs, BpsYd, BpsYo, Bgnw) = [
                        Buf(n) for n in ("lb", "ke", "vv", "keT", "qeT", "gT", "ebl", "sgm", "fv", "gl", "kk", "eb", "enb", "qe", "sq", "rt", "ot", "psB", "psT", "psXa", "psXs", "psYd", "psYo", "gnw")]
                    BS32 = [Buf("S32"), Buf("S32")]
                    BSb = [Buf("Sb"), Buf("Sb")]
                    Bstmp = [Buf("stmp"), Buf("stmp")]
                    BaT = [Buf("aT"), Buf("aT")]
                    BpsP = [Buf("psP"), Buf("psP")]
                    onesb = C("ones", True)
                    S.dma(SP, gnws[:], gnw, ch_misc, writes=[Bgnw])
                    S.dma(SP, lb[:], hgrn_lb[0:1, 0:1024].partition_broadcast(128), ch_misc, writes=[Blb])
                    S.dma(SP, oml[:], hgrn_lb[0:1, 1024:2048].partition_broadcast(128), ch_misc, writes=[Blb])
                    S.op(DVE, lambda e: e.tensor_tensor(out=oml[:], in0=lb[:], in1=oml[:], op=ALU.subtract), reads=[Blb], writes=[Blb])
                    S.op(ACT, lambda e: e.activation(out=lb[:], in_=oml[:], func=AF.Sigmoid), reads=[Blb], writes=[Blb])
                    S.op(DVE, lambda e: e.tensor_scalar(out=oml[:], in0=lb[:], scalar1=-1.0, scalar2=1.0, op0=ALU.mult, op1=ALU.add), reads=[Blb], writes=[Blb])
                    pcnt = [0]

                    def proj_tm(wt, wb, tt):
                        k = pcnt[0] % 2
                        pcnt[0] += 1

                        def mm(e):
                            for kc in range(16):
                                ins = e.matmul(psP[k][:, 0:256], lhsT=hT[:, kc, tt * 128:(tt + 1) * 128], rhs=wt[:, kc, 0:256], start=(kc == 0), stop=(kc == 15))
                            return ins
                        S.op(PE, mm, reads=[wb], writes=[BpsP[k]])
                        return k

                    for hp in range(4):
                        wqr, wqrb = load_w(w_in[:, 3072 + hp * 256:3072 + (hp + 1) * 256], 256)
                        wf, wfb = load_w(w_in[:, 4096 + hp * 256:4096 + (hp + 1) * 256], 256)
                        wi, wib = load_w(w_in[:, 5120 + hp * 256:5120 + (hp + 1) * 256], 256)
                        wg, wgb = load_w(w_in[:, 6144 + hp * 256:6144 + (hp + 1) * 256], 256)
                        lsl = slice(hp * 256, (hp + 1) * 256)
                        for hd in range(2):
                            for nt in range(2):
                                k = pcnt[0] % 2
                                pcnt[0] += 1

                                def mm(e, k=k, hd=hd, nt=nt):
                                    for kc in range(16):
                                        ins = e.matmul(psP[k][:], lhsT=wg[:, kc, hd * 128:(hd + 1) * 128], rhs=hT[:, kc, 1024 + nt * 512:1024 + (nt + 1) * 512], start=(kc == 0), stop=(kc == 15))
                                    return ins
                                S.op(PE, mm, reads=[wgb], writes=[BpsP[k]])
                                S.op(ACT, lambda e, k=k, hd=hd, nt=nt: e.activation(out=gT[:, hd, nt * 512:(nt + 1) * 512], in_=psP[k][:], func=AF.Silu), reads=[BpsP[k]], writes=[BgT])
                        for tt in range(16):
                            own = tt >= 8
                            k = proj_tm(wf, wfb, tt)
                            S.op(ACT, lambda e, k=k: e.activation(out=sgm[:], in_=psP[k][:, 0:256], func=AF.Sigmoid), reads=[BpsP[k]], writes=[Bsgm])
                            S.op(DVE, lambda e: e.tensor_tensor(out=fv[:], in0=sgm[:], in1=oml[:, lsl], op=ALU.mult), reads=[Bsgm, Blb], writes=[Bfv])
                            S.op(DVE, lambda e: e.tensor_tensor(out=fv[:], in0=fv[:], in1=lb[:, lsl], op=ALU.add), reads=[Bfv, Blb], writes=[Bfv])
                            S.op(ACT, lambda e: e.activation(out=gl[:], in_=fv[:], func=AF.Ln), reads=[Bfv], writes=[Bgl])
                            S.op(DVE, lambda e: e.tensor_scalar(out=kk[:], in0=fv[:], scalar1=-1.0, scalar2=1.0, op0=ALU.mult, op1=ALU.add), reads=[Bfv], writes=[Bkk])

                            def cs(e):
                                e.matmul(psB[:, 0:256], lhsT=C("ltri"), rhs=gl[:], start=True, stop=True)
                                for hd in range(2):
                                    ins = e.matmul(psB[:, 256 + 2 * hd:258 + 2 * hd], lhsT=gl[:, hd * 128:(hd + 1) * 128], rhs=C("chunkind"), start=True, stop=True)
                                return ins
                            S.op(PE, cs, reads=[Bgl], writes=[BpsB])
                            S.op(ACT, lambda e: e.activation(out=enb[:], in_=psB[:, 0:256], func=AF.Exp, scale=-1.0), reads=[BpsB], writes=[Benb])
                            if own:
                                S.op(ACT, lambda e: e.activation(out=eb[:], in_=psB[:, 0:256], func=AF.Exp), reads=[BpsB], writes=[Beb])
                            S.op(ACT, lambda e, tt=tt: e.activation(out=ebl[:, :, tt, :], in_=psB[:, 256:260].rearrange("p (h c) -> p h c", c=2), func=AF.Exp), reads=[BpsB], writes=[Bebl])
                            S.op(DVE, lambda e, tt=tt: e.tensor_tensor(out=ke[:, tt, :], in0=kk[:], in1=enb[:], op=ALU.mult), reads=[Bkk, Benb], writes=[Bke])
                            k = proj_tm(wi, wib, tt)
                            S.op(ACT, lambda e, k=k, tt=tt: e.activation(out=vv[:, tt, :], in_=psP[k][:, 0:256], func=AF.Copy), reads=[BpsP[k]], writes=[Bvv])
                            if own:
                                k = proj_tm(wqr, wqrb, tt)
                                S.op(DVE, lambda e, k=k: e.tensor_tensor(out=qe_t[:], in0=psP[k][:, 0:256], in1=eb[:], op=ALU.mult), reads=[BpsP[k], Beb], writes=[Bqe])

                            def tr(e, tt=tt, own=own):
                                for hd in range(2):
                                    ins = e.transpose(out=psT[:, hd, :], in_=ke[:, tt, hd * 128:(hd + 1) * 128], identity=C("ident", True))
                                if own:
                                    for hd in range(2):
                                        ins = e.transpose(out=psT[:, 2 + hd, :], in_=qe_t[:, hd * 128:(hd + 1) * 128], identity=C("ident", True))
                                return ins
                            S.op(PE, tr, reads=[Bke] + ([Bqe] if own else []), writes=[BpsT])
                            S.op(ACT, lambda e, tt=tt: e.activation(out=keT[:, :, tt * 128:(tt + 1) * 128], in_=psT[:, 0:2, :], func=AF.Copy), reads=[BpsT], writes=[BkeT])
                            if own:
                                S.op(ACT, lambda e, tt=tt: e.activation(out=qeT[:, :, (tt - 8) * 128:(tt - 7) * 128], in_=psT[:, 2:4, :], func=AF.Copy), reads=[BpsT], writes=[BqeT])
                        for hd in range(2):
                            S.op(DVE, lambda e, hd=hd: e.memset(S32[hd][:], 0.0), writes=[BS32[hd]])
                            S.op(ACT, lambda e, hd=hd: e.activation(out=Sb_[hd][:], in_=S32[hd][:], func=AF.Copy), reads=[BS32[hd]], writes=[BSb[hd]])
                        for tt in range(16):
                            own = tt >= 8
                            for hd in range(2):
                                hsl = slice(hd * 128, (hd + 1) * 128)
                                if own:
                                    qsl = slice((tt - 8) * 128, (tt - 7) * 128)
                                    S.op(PE, lambda e, hd=hd, tt=tt, qsl=qsl: e.matmul(psX[:, 0, :], lhsT=keT[:, hd, tt * 128:(tt + 1) * 128], rhs=qeT[:, hd, qsl], start=True, stop=True),
                                         reads=[BkeT, BqeT], writes=[BpsXa])
                                    S.op(DVE, lambda e, hd=hd: e.tensor_tensor(out=aT[hd][:], in0=psX[:, 0, :], in1=C("bdmask", True), op=ALU.mult), reads=[BpsXa], writes=[BaT[hd]])
                                for c in range(2):
                                    rows = slice(c * 64, (c + 1) * 64)
                                    if own:
                                        def om(e, hd=hd, tt=tt, c=c, rows=rows, hsl=hsl):
                                            e.matmul(psY[:, 1, c * 64:(c + 1) * 64], lhsT=vv[rows, tt, hsl], rhs=aT[hd][rows, c * 64:(c + 1) * 64], start=True, stop=False)
                                            return e.matmul(psY[:, 1, c * 64:(c + 1) * 64], lhsT=Sb_[hd][:], rhs=qeT[:, hd, (tt - 8) * 128 + c * 64:(tt - 8) * 128 + (c + 1) * 64], start=False, stop=True)
                                        S.op(PE, om, reads=[Bvv, BaT[hd], BSb[hd], BqeT], writes=[BpsYo])
                                    S.op(PE, lambda e, tt=tt, rows=rows, hsl=hsl: e.matmul(psY[:, 0, :], lhsT=ke[rows, tt, hsl], rhs=vv[rows, tt, hsl], start=True, stop=True),
                                         reads=[Bke, Bvv], writes=[BpsYd])
                                    S.op(DVE, lambda e, hd=hd: e.tensor_tensor(out=stmp[hd][:], in0=psY[:, 0, :], in1=S32[hd][:], op=ALU.add), reads=[BpsYd, BS32[hd]], writes=[Bstmp[hd]])
                                    S.op(DVE, lambda e, hd=hd, tt=tt, c=c: e.tensor_scalar(out=S32[hd][:], in0=stmp[hd][:], scalar1=ebl[:, hd, tt, c:c + 1], scalar2=None, op0=ALU.mult),
                                         reads=[Bstmp[hd], Bebl], writes=[BS32[hd]])
                                    if tt == 7 and c == 1:
                                        S.op(DVE, lambda e, hd=hd: e.tensor_scalar(out=S32[hd][:], in0=S32[hd][:], scalar1=C("flag"), scalar2=None, op0=ALU.mult), reads=[BS32[hd]], writes=[BS32[hd]])
                                    S.op(ACT, lambda e, hd=hd: e.activation(out=Sb_[hd][:], in_=S32[hd][:], func=AF.Copy), reads=[BS32[hd]], writes=[BSb[hd]])
                                if own:
                                    ci_ = 8 + hp * 2 + hd
                                    S.op(ACT, lambda e: e.activation(out=sq[:], in_=psY[:, 1, :], func=AF.Square), reads=[BpsYo], writes=[Bsq])
                                    S.op(PE, lambda e: e.matmul(psX[:, 1, :], lhsT=onesb, rhs=sq[:], start=True, stop=True), reads=[Bsq], writes=[BpsXs])
                                    S.op(ACT, lambda e: e.activation(out=rt[:], in_=psX[:, 1, :], func=AF.Sqrt, scale=1.0 / 128.0, bias=C("ones")[:, 0:1] if False else 1e-6), reads=[BpsXs], writes=[Brt])
                                    S.op(DVE, lambda e: e.reciprocal(out=rt[:], in_=rt[:]), reads=[Brt], writes=[Brt])
                                    S.op(DVE, lambda e: e.tensor_tensor(out=ot[:], in0=psY[:, 1, :], in1=rt[:], op=ALU.mult), reads=[BpsYo, Brt], writes=[Bot])
                                    S.op(DVE, lambda e, ci_=ci_, qsl=qsl, hd=hd, hp=hp: e.scalar_tensor_tensor(out=mixT[:, ci_, qsl], in0=ot[:], scalar=gnws[:, hp * 2 + hd:hp * 2 + hd + 1], in1=gT[:, hd, qsl], op0=ALU.mult, op1=ALU.mult),
                                         reads=[Bot, BgT, Bgnw], writes=[B_mix[ci_]])
                    S.barrier()
        if debug:
            S.dma(SP, dbg["mixT"], mixT[:].rearrange("p c t -> p (c t)"), ch_misc, reads=B_mix)
            S.barrier()
        if stage >= 3:
            def ln_tile(yb, junk, st, Byb, Bjunk, Bst, grow, brow_, Bg, Bb):
                S.op(DVE, lambda e: e.memset(st[:], 0.0), writes=[Bst])
                S.op(DVE, lambda e: e.reduce_sum(out=st[:, 0:1], in_=yb[:], axis=AX.X), reads=[Byb], writes=[Bst])
                S.op(DVE, lambda e: e.tensor_scalar(out=st[:, 1:2], in0=st[:, 0:1], scalar1=-1.0 / D, scalar2=None, op0=ALU.mult), reads=[Bst], writes=[Bst])
                S.op(DVE, lambda e: e.tensor_scalar(out=yb[:], in0=yb[:], scalar1=st[:, 1:2], scalar2=None, op0=ALU.add), reads=[Byb, Bst], writes=[Byb])
                S.op(ACT, lambda e: e.activation(out=junk[:], in_=yb[:], func=AF.Square, accum_out=st[:, 2:3]), reads=[Byb, Bst], writes=[Bjunk, Bst])
                S.op(ACT, lambda e: e.activation(out=st[:, 3:4], in_=st[:, 2:3], func=AF.Sqrt, scale=1.0 / D, bias=1e-5), reads=[Bst], writes=[Bst])
                S.op(DVE, lambda e: e.reciprocal(out=st[:, 4:5], in_=st[:, 3:4]), reads=[Bst], writes=[Bst])
                S.op(DVE, lambda e: e.scalar_tensor_tensor(out=yb[:], in0=yb[:], scalar=st[:, 4:5], in1=grow[:], op0=ALU.mult, op1=ALU.mult), reads=[Byb, Bst, Bg], writes=[Byb])
                S.op(DVE, lambda e: e.tensor_tensor(out=yb[:], in0=yb[:], in1=brow_[:], op=ALU.add), reads=[Byb, Bb], writes=[Byb])

            B_x1d = Buf("x1d")
            with ExitStack() as ph:
                wo = sb(ph, [128, 16, D], BF16, "wo")
                Bwo = Buf("wo")
                ch_wo = S.chan("wo")
                for n in range(4):
                    S.dma(POOL, wo[:, :, n * 512:(n + 1) * 512], w_o[:, n * 512:(n + 1) * 512].rearrange("(k p) c -> p k c", p=128), ch_wo, writes=[Bwo])
                gt1, Bgt1 = bcast_row(ph, mod_d[2:3, :], ch_misc, reads=[B_mod[2]])
                g1, Bg1 = bcast_row(ph, lnp[0:1, :], ch_misc)
                b1r, Bb1r = bcast_row(ph, lnp[1:2, :], ch_misc)
                scf, Bscf = bcast_row(ph, mod_d[4:5, :], ch_misc, reads=[B_mod[4]])
                shf, Bshf = bcast_row(ph, mod_d[3:4, :], ch_misc, reads=[B_mod[3]])
                rw = sb(ph, [128, 16, NE], F32, "rw")
                rb = sb(ph, [128, NE], F32, "rb")
                Brw = Buf("rw")
                S.dma(SP, rw[:], router_w.rearrange("p (k e) -> p k e", e=NE), ch_misc, writes=[Brw])
                S.dma(SP, rb[:], router_b.partition_broadcast(128), ch_misc, writes=[Brw])
                xin = sb(ph, [128, D], F32, "xin")
                yb = sb(ph, [128, D], F32, "yb")
                h2T = sb(ph, [128, 16, 128], F32, "h2T")
                st = sb(ph, [128, 8], F32, "st")
                t8 = sb(ph, [128, 8], F32, "t8")
                ex = sb(ph, [128, NE], F32, "ex")
                sm = sb(ph, [128, 4], F32, "sm")
                Bxin, Byb, Bh2T, Bst, Bt8, Bex, Bsm = [Buf(n) for n in ("xin", "yb", "h2T", "st", "t8", "ex", "sm")]
                chx = S.chan("xinD")
                chy = S.chan("x1st")
                psM = ps(ph, [128, 4, 512])
                psT4 = ps(ph, [128, 4, 128])
                psL = ps(ph, [128, NE])
                BpsM, BpsT4, BpsL = Buf("psM"), Buf("psT4"), Buf("psL")
                for j in range(8):
                    tsl = slice(j * 128, (j + 1) * 128)
                    S.dma(SP, xin[:], x_own[tsl, :], chx, writes=[Bxin])

                    def mm(e, tsl=tsl):
                        for n in range(4):
                            for kc in range(16):
                                ins = e.matmul(psM[:, n, :], lhsT=mixT[:, kc, tsl], rhs=wo[:, kc, n * 512:(n + 1) * 512], start=(kc == 0), stop=(kc == 15))
                        return ins
                    S.op(PE, mm, reads=[Bwo] + B_mix, writes=[BpsM])
                    S.op(DVE, lambda e: e.tensor_tensor(out=yb[:], in0=psM[:].rearrange("p n c -> p (n c)"), in1=gt1[:], op=ALU.mult), reads=[BpsM, Bgt1], writes=[Byb])
                    S.op(DVE, lambda e: e.scalar_tensor_tensor(out=yb[:], in0=xin[:], scalar=ALPHA, in1=yb[:], op0=ALU.mult, op1=ALU.add), reads=[Bxin, Byb], writes=[Byb])
                    ln_tile(yb, xin, st, Byb, Bxin, Bst, g1, b1r, Bg1, Bb1r)
                    S.dma(SP, x1_d[tsl, :], yb[:], chy, reads=[Byb], writes=[B_x1d])
                    if debug:
                        S.dma(SP, dbg["x1"][tsl, :], yb[:], chy, reads=[Byb])
                S.barrier()
                for j in range(8):
                    tsl = slice(j * 128, (j + 1) * 128)
                    S.dma(SP, yb[:], x1_d[tsl, :], chx, reads=[B_x1d], writes=[Byb])
                    S.op(DVE, lambda e: e.tensor_tensor(out=xin[:], in0=yb[:], in1=scf[:], op=ALU.mult), reads=[Byb, Bscf], writes=[Bxin])
                    S.op(DVE, lambda e: e.tensor_tensor(out=xin[:], in0=xin[:], in1=shf[:], op=ALU.add), reads=[Bxin, Bshf], writes=[Bxin])
                    S.op(ACT, lambda e, j=j: e.activation(out=h2bf[:, j, :], in_=xin[:], func=AF.Copy), reads=[Bxin], writes=[Bh2bf])
                    for q4 in range(4):
                        def tr(e, q4=q4):
                            for i in range(4):
                                kc = q4 * 4 + i
                                ins = e.transpose(out=psT4[:, i, :], in_=xin[:, kc * 128:(kc + 1) * 128], identity=C("ident"))
                            return ins
                        S.op(PE, tr, reads=[Bxin], writes=[BpsT4])
                        S.op(ACT, lambda e, q4=q4: e.activation(out=h2T[:, q4 * 4:(q4 + 1) * 4, :], in_=psT4[:], func=AF.Copy), reads=[BpsT4], writes=[Bh2T])

                    def lg(e):
                        for kc in range(16):
                            ins = e.matmul(psL[:], lhsT=h2T[:, kc, :], rhs=rw[:, kc, :], start=(kc == 0), stop=(kc == 15))
                        return ins
                    S.op(PE, lg, reads=[Bh2T, Brw], writes=[BpsL])
                    S.op(DVE, lambda e, j=j: e.tensor_tensor(out=logit[:, j, :], in0=psL[:], in1=rb[:], op=ALU.add), reads=[BpsL, Brw], writes=[Blogit])
                    S.op(DVE, lambda e, j=j: e.max(out=t8[:], in_=logit[:, j, :]), reads=[Blogit], writes=[Bt8])
                    S.op(DVE, lambda e, j=j: e.tensor_scalar(out=mfall[:, j, :], in0=logit[:, j, :], scalar1=t8[:, 3:4], scalar2=None, op0=ALU.is_ge), reads=[Blogit, Bt8], writes=[Bmf])
                    S.op(DVE, lambda e: e.tensor_scalar(out=sm[:, 0:1], in0=t8[:, 0:1], scalar1=-1.0, scalar2=None, op0=ALU.mult), reads=[Bt8], writes=[Bsm])
                    S.op(ACT, lambda e, j=j: e.activation(out=ex[:], in_=logit[:, j, :], func=AF.Exp, bias=sm[:, 0:1], scale=1.0), reads=[Blogit, Bsm], writes=[Bex])
                    S.op(DVE, lambda e, j=j: e.tensor_tensor(out=ex[:], in0=ex[:], in1=mfall[:, j, :], op=ALU.mult), reads=[Bex, Bmf], writes=[Bex])
                    S.op(DVE, lambda e: e.reduce_sum(out=sm[:, 1:2], in_=ex[:], axis=AX.X), reads=[Bex], writes=[Bsm])
                    S.op(DVE, lambda e: e.reciprocal(out=sm[:, 2:3], in_=sm[:, 1:2]), reads=[Bsm], writes=[Bsm])
                    S.op(DVE, lambda e, j=j: e.tensor_scalar(out=gate[:, j, :], in0=ex[:], scalar1=sm[:, 2:3], scalar2=None, op0=ALU.mult), reads=[Bex, Bsm], writes=[Bgate])
                    S.op(DVE, lambda e, j=j: e.tensor_copy(out=mb[:, j, :], in_=mfall[:, j, :]), reads=[Bmf], writes=[Bmb])
                for j in range(8):
                    def pc(e, j=j):
                        for i in range(j):
                            e.matmul(psL[:], lhsT=C("ones", True), rhs=mb[:, i, :], start=(i == 0), stop=False)
                        return e.matmul(psL[:], lhsT=C("ustrict", True), rhs=mb[:, j, :], start=(j == 0), stop=True)
                    S.op(PE, pc, reads=[Bmb], writes=[BpsL])
                    S.op(DVE, lambda e, j=j: e.scalar_tensor_tensor(out=posm[:, j, :], in0=psL[:], scalar=1.0, in1=mfall[:, j, :], op0=ALU.add, op1=ALU.mult), reads=[BpsL, Bmf], writes=[Bposm])
                    S.op(DVE, lambda e, j=j: e.tensor_scalar(out=posm[:, j, :], in0=posm[:, j, :], scalar1=-1.0, scalar2=float(CAP), op0=ALU.add, op1=ALU.min), reads=[Bposm], writes=[Bposm])
                    S.op(DVE, lambda e, j=j: e.tensor_copy(out=posmb[:, j, :], in_=posm[:, j, :]), reads=[Bposm], writes=[Bposmb])
                if debug:
                    S.dma(SP, dbg["logits"], logit[:].rearrange("p j e -> p (j e)"), ch_misc, reads=[Blogit])
                S.barrier()
        if stage >= 5:
            accF_scope = ExitStack()
            es.enter_context(accF_scope)
            accF = sb(accF_scope, [128, 8, D], F32, "accF")
            BaccF = Buf("accF")
            S.op(DVE, lambda e: e.memset(accF[:], 0.0), writes=[BaccF])
            with ExitStack() as ph:
                NSLOT = 3
                load_w = make_ring(ph, NSLOT, 512)
                b1s = sb(ph, [128, NE * 32], F32, "b1s")
                Bb1 = Buf("b1s")
                S.dma(SP, b1s[:], b1, ch_misc, writes=[Bb1])
                Pe = sb(ph, [128, 8, CAP], BF16, "Pe")
                PT = sb(ph, [128, 2, T], BF16, "PT")
                xg = sb(ph, [128, 16, CAP], BF16, "xg")
                actT = sb(ph, [128, 16, CAP], BF16, "actT")
                ysb = sb(ph, [128, 2, D], BF16, "ysb")
                xgl = sb(ph, [128, CAP], F32, "xgl")
                sgd = sb(ph, [128, CAP], F32, "sgd")
                xl = sb(ph, [128, CAP], F32, "xl")
                BPe, BPT, Bxg, BactT, Bysb, Bxgl, Bsgd, Bxl = [Buf(n) for n in ("Pe", "PT", "xg", "actT", "ysb", "xgl", "sgd", "xl")]
                psPT = ps(ph, [128, 2, 512])
                psG = ps(ph, [128, 2, CAP])
                psH = [ps(ph, [128, 2, CAP]) for _ in range(2)]
                psY2 = [ps(ph, [128, 512]) for _ in range(2)]
                BpsPT, BpsG = Buf("psPT"), Buf("psG")
                BpsH = [Buf("psH"), Buf("psH")]
                BpsY2 = [Buf("psY2"), Buf("psY2")]
                items = []
                for e_ in range(NE):
                    for g8 in range(8):
                        items.append(("w1", e_, g8))
                    for n in range(4):
                        items.append(("w2", e_, n))

                def issue(it):
                    kind, e_, g = it
                    if kind == "w1":
                        return load_w(w1[e_, :, g * 512:(g + 1) * 512], 512)
                    return load_w(w2[e_, :, g * 512:(g + 1) * 512], 512)
                pend = [issue(items[i]) for i in range(NSLOT)]
                nxt = [NSLOT]

                def next_w():
                    r = pend.pop(0)
                    return r

                def refill():
                    if nxt[0] < len(items):
                        pend.append(issue(items[nxt[0]]))
                        nxt[0] += 1
                hcnt = [0]
                for e_ in range(NE):
                    for j in range(8):
                        S.op(DVE, lambda e, j=j, e_=e_: e.tensor_scalar(out=Pe[:, j, :], in0=C("iota")[:, 0:CAP], scalar1=posm[:, j, e_:e_ + 1], scalar2=None, op0=ALU.is_equal),
                             reads=[Bposm], writes=[BPe])

                    def ptm(e, e_=e_):
                        for j in range(8):
                            ins = e.matmul(psPT[:, j // 4, (j % 4) * 128:(j % 4 + 1) * 128], lhsT=posmb[:, j, e_:e_ + 1].to_broadcast([128, 128]), rhs=C("ident", True), start=True, stop=True)
                        return ins
                    S.op(PE, ptm, reads=[Bposmb], writes=[BpsPT])
                    for p in range(2):
                        S.op(DVE, lambda e, p=p: e.tensor_scalar(out=PT[:, p, :], in0=psPT[:].rearrange("p a c -> p (a c)"), scalar1=C("iotac")[:, p:p + 1], scalar2=None, op0=ALU.is_equal),
                             reads=[BpsPT], writes=[BPT])
                    for kq in range(8):
                        def gm(e, kq=kq):
                            for k2 in range(2):
                                kc = kq * 2 + k2
                                for j in range(8):
                                    ins = e.matmul(psG[:, k2, :], lhsT=h2bf[:, j, kc * 128:(kc + 1) * 128], rhs=Pe[:, j, :], start=(j == 0), stop=(j == 7))
                            return ins
                        S.op(PE, gm, reads=[Bh2bf, BPe], writes=[BpsG])
                        S.op(ACT, lambda e, kq=kq: e.activation(out=xg[:, kq * 2:(kq + 1) * 2, :], in_=psG[:], func=AF.Copy), reads=[BpsG], writes=[Bxg])
                    for g8 in range(8):
                        wt, wb = next_w()
                        for f2 in range(2):
                            fc = g8 * 2 + f2
                            k = hcnt[0] % 2
                            hcnt[0] += 1

                            def hm(e, wt=wt, f2=f2, k=k):
                                for t_ in range(2):
                                    for kc in range(16):
                                        ins = e.matmul(psH[k][:, t_, :], lhsT=wt[:, kc, f2 * 256 + t_:(f2 + 1) * 256:2], rhs=xg[:, kc, :], start=(kc == 0), stop=(kc == 15))
                                return ins
                            S.op(PE, hm, reads=[wb, Bxg], writes=[BpsH[k]])
                            bi = e_ * 32 + fc * 2
                            S.op(DVE, lambda e, k=k, bi=bi: e.tensor_scalar(out=xgl[:], in0=psH[k][:, 0, :], scalar1=b1s[:, bi:bi + 1], scalar2=7.0, op0=ALU.add, op1=ALU.min), reads=[BpsH[k], Bb1], writes=[Bxgl])
                            S.op(ACT, lambda e: e.activation(out=sgd[:], in_=xgl[:], func=AF.Sigmoid, scale=1.702), reads=[Bxgl], writes=[Bsgd])
                            S.op(DVE, lambda e, k=k, bi=bi: e.tensor_scalar(out=xl[:], in0=psH[k][:, 1, :], scalar1=b1s[:, bi + 1:bi + 2], scalar2=7.0, op0=ALU.add, op1=ALU.min), reads=[BpsH[k], Bb1], writes=[Bxl])
                            S.op(DVE, lambda e: e.tensor_scalar(out=xl[:], in0=xl[:], scalar1=-7.0, scalar2=1.0, op0=ALU.max, op1=ALU.add), reads=[Bxl], writes=[Bxl])
                            S.op(DVE, lambda e: e.tensor_tensor(out=xgl[:], in0=xgl[:], in1=sgd[:], op=ALU.mult), reads=[Bxgl, Bsgd], writes=[Bxgl])
                            S.op(DVE, lambda e, fc=fc: e.tensor_tensor(out=actT[:, fc, :], in0=xgl[:], in1=xl[:], op=ALU.mult), reads=[Bxgl, Bxl], writes=[BactT])
                        refill()
                    for n in range(4):
                        wt, wb = next_w()
                        for p in range(2):
                            k = hcnt[0] % 2
                            hcnt[0] += 1

                            def ym(e, wt=wt, p=p, k=k):
                                for fc in range(16):
                                    ins = e.matmul(psY2[k][:], lhsT=actT[:, fc, p * 128:(p + 1) * 128], rhs=wt[:, fc, :], start=(fc == 0), stop=(fc == 15))
                                return ins
                            S.op(PE, ym, reads=[wb, BactT], writes=[BpsY2[k]])
                            S.op(ACT, lambda e, k=k, p=p, n=n: e.activation(out=ysb[:, p, n * 512:(n + 1) * 512], in_=psY2[k][:], func=AF.Copy), reads=[BpsY2[k]], writes=[Bysb])
                        refill()
                    for j in range(8):
                        for hf_ in range(2):
                            def sm_(e, j=j, hf_=hf_):
                                for n2 in range(2):
                                    for p in range(2):
                                        ins = e.matmul(psPT[:, n2, :], lhsT=PT[:, p, j * 128:(j + 1) * 128], rhs=ysb[:, p, hf_ * 1024 + n2 * 512:hf_ * 1024 + (n2 + 1) * 512], start=(p == 0), stop=(p == 1))
                                return ins
                            S.op(PE, sm_, reads=[BPT, Bysb], writes=[BpsPT])
                            av = accF[:, j, hf_ * 1024:(hf_ + 1) * 1024]
                            S.op(DVE, lambda e, av=av, j=j, e_=e_: e.scalar_tensor_tensor(out=av, in0=psPT[:].rearrange("p a c -> p (a c)"), scalar=gate[:, j, e_:e_ + 1], in1=av, op0=ALU.mult, op1=ALU.add),
                                 reads=[BpsPT, Bgate, BaccF], writes=[BaccF])
                S.barrier()
            with ExitStack() as ph:
                gT_sb = sb(ph, [NE, T], F32, "gTsb")
                b2s = sb(ph, [NE, D], F32, "b2s")
                Bg_, Bb2 = Buf("gTsb"), Buf("b2s")
                S.dma(SP, b2s[:], b2, ch_misc, writes=[Bb2])
                gtf, Bgtf = bcast_row(ph, mod_d[5:6, :], ch_misc, reads=[B_mod[5]])
                g2, Bg2 = bcast_row(ph, lnp[2:3, :], ch_misc)
                b2r, Bb2r = bcast_row(ph, lnp[3:4, :], ch_misc)
                xin = sb(ph, [128, D], F32, "xin")
                yb = sb(ph, [128, D], F32, "yb")
                st = sb(ph, [128, 8], F32, "st")
                Bxin, Byb, Bst = Buf("xin"), Buf("yb"), Buf("st")
                chx = S.chan("xinG")
                cho = S.chan("outG")
                psGT = ps(ph, [NE, 128])
                psM = ps(ph, [128, 4, 512])
                BpsGT, BpsM = Buf("psGT"), Buf("psM")
                for j in range(8):
                    S.op(PE, lambda e, j=j: e.transpose(out=psGT[:], in_=gate[:, j, :], identity=C("ident")), reads=[Bgate], writes=[BpsGT])
                    S.op(ACT, lambda e, j=j: e.activation(out=gT_sb[:, j * 128:(j + 1) * 128], in_=psGT[:], func=AF.Copy), reads=[BpsGT], writes=[Bg_])
                for j in range(8):
                    tsl = slice(j * 128, (j + 1) * 128)
                    S.dma(SP, xin[:], x1_d[tsl, :], chx, reads=[B_x1d], writes=[Bxin])

                    def bm(e, tsl=tsl):
                        for n in range(4):
                            ins = e.matmul(psM[:, n, :], lhsT=gT_sb[:, tsl], rhs=b2s[:, n * 512:(n + 1) * 512], start=True, stop=True)
                        return ins
                    S.op(PE, bm, reads=[Bg_, Bb2], writes=[BpsM])
                    S.op(DVE, lambda e, j=j: e.tensor_tensor(out=yb[:], in0=psM[:].rearrange("p n c -> p (n c)"), in1=accF[:, j, :], op=ALU.add), reads=[BpsM, BaccF], writes=[Byb])
                    if debug:
                        S.dma(SP, dbg["ffn"][tsl, :], yb[:], cho, reads=[Byb])
                    S.op(DVE, lambda e: e.tensor_tensor(out=yb[:], in0=yb[:], in1=gtf[:], op=ALU.mult), reads=[Byb, Bgtf], writes=[Byb])
                    S.op(DVE, lambda e: e.scalar_tensor_tensor(out=yb[:], in0=xin[:], scalar=ALPHA, in1=yb[:], op0=ALU.mult, op1=ALU.add), reads=[Bxin, Byb], writes=[Byb])
                    ln_tile(yb, xin, st, Byb, Bxin, Bst, g2, b2r, Bg2, Bb2r)
                    S.dma(SP, out[tsl, :], yb[:], cho, reads=[Byb])
                S.barrier()
    return nc


def make_in_maps(inp, stage=99):
    f = lambda a: np.ascontiguousarray(np.asarray(a), dtype=np.float32)
    x = f(inp["x"])
    c = f(inp["c"])
    posn = np.asarray(inp["positions"]).astype(np.int32)
    shared = {
        "w_ada": f(inp["w_ada"][0]), "b_ada": f(inp["b_ada"][0]).reshape(1, -1), "w_in": f(inp["w_in"][0]),
        "hgrn_lb": f(inp["hgrn_lb"]).reshape(1, 2048),
        "gnw": np.ascontiguousarray(f(inp["gnorm_w"][0]).reshape(8, 128).T),
        "w_o": f(inp["w_o"][0]),
        "lnp": np.stack([f(inp["ln1_g"][0]), f(inp["ln1_b"][0]), f(inp["ln2_g"][0]), f(inp["ln2_b"][0])], 0),
        "router_w": np.ascontiguousarray(f(inp["router_w"][0]).reshape(16, 128, NE).transpose(1, 0, 2).reshape(128, 16 * NE)),
        "router_b": f(inp["router_b"][0]).reshape(1, NE),
    }
    if stage >= 5:
        shared["w1"] = f(inp["w1"][0])
        shared["w2"] = f(inp["w2"][0])
        shared["b1"] = np.ascontiguousarray(f(inp["b1"][0]).reshape(NE, 16, 128, 2).transpose(2, 0, 1, 3).reshape(128, NE * 32))
        shared["b2"] = f(inp["b2"][0])
    maps = []
    cc = [make_consts(0), make_consts(1)]
    for core in range(8):
        b, half = core // 2, core % 2
        m = dict(shared)
        m["x_own"] = np.ascontiguousarray(x[b, half * T:(half + 1) * T])
        m["x_ctx"] = np.ascontiguousarray(x[b, 0:T]) if half == 1 else np.zeros((T, D), np.float32)
        m["cT"] = np.ascontiguousarray(c[b].reshape(16, 128).T)
        pp = np.zeros((1, 2048), np.int32)
        pp[0, T:] = posn[b, half * T:(half + 1) * T]
        if half == 1:
            pp[0, :T] = posn[b, 0:T]
        m["pos"] = pp
        m["consts"] = cc[half]
        maps.append(m)
    return maps


_NC_CACHE = {}


def kernel(**inputs):
    if "nc" not in _NC_CACHE:
        _NC_CACHE["nc"] = build_nc()
    nc = _NC_CACHE["nc"]
    maps = make_in_maps(inputs)
    res = run_bass_kernel_spmd(nc, maps, core_ids=list(range(8)))
    outp = np.zeros((4, 2048, D), np.float32)
    for core in range(8):
        b, half = core // 2, core % 2
        outp[b, half * T:(half + 1) * T] = res.results[core]["out"]
    return outp
```

```python
import numpy as np
from contextlib import ExitStack
import concourse.bass as bass
import concourse.mybir as mybir
from concourse.bass_utils import run_bass_kernel_spmd

F32 = mybir.dt.float32
BF16 = mybir.dt.bfloat16
I32 = mybir.dt.int32
AF = mybir.ActivationFunctionType
ALU = mybir.AluOpType
AX = mybir.AxisListType

D = 2048
T = 1024
NE = 32
CAP = 256
ALPHA = 2.0 ** 0.25
PI = float(np.pi)
STRICT = False

_c = {}
_off = 0
for _n, _w in [("ident", 128), ("mcur", 128), ("mprev", 128), ("mprevctx", 128), ("mask3", 64), ("rmat", 128),
               ("ltri", 128), ("chunkind", 2), ("bdmask", 128), ("ustrict", 128), ("ones", 128), ("iota", 256),
               ("iotac", 2), ("invp", 1), ("signp", 1), ("flag", 1)]:
    _c[_n] = (_off, _w)
    _off += _w
NCONST = _off


def make_consts(half):
    c = np.zeros((128, NCONST), np.float32)

    def put(name, arr):
        o, w = _c[name]
        c[:, o:o + w] = arr
    k = np.arange(128)[:, None]
    q = np.arange(128)[None, :]
    flag = 1.0 if half == 1 else 0.0
    put("ident", np.eye(128))
    put("mcur", (k <= q))
    put("mprev", (k >= q))
    put("mprevctx", (k >= q) * flag)
    m3 = (k <= (np.arange(64)[None, :] + 64)).astype(np.float32)
    m3[:64] *= flag
    put("mask3", m3)
    rm = np.zeros((128, 128), np.float32)
    invp = np.zeros((128, 1), np.float32)
    signp = np.zeros((128, 1), np.float32)
    for m in range(128):
        d = m % 64
        if d < 8:
            rm[m + 8, m] = 1.0
            signp[m] = -1.0
        elif d < 16:
            rm[m - 8, m] = 1.0
            signp[m] = 1.0
        if d < 16:
            invp[m] = 500000.0 ** (-(2.0 * (d % 8)) / 16.0)
    put("rmat", rm)
    put("invp", invp)
    put("signp", signp)
    same = (k // 64) == (q // 64)
    put("ltri", (k <= q) & same)
    put("bdmask", (k <= q) & same)
    ci = np.zeros((128, 2), np.float32)
    ci[:64, 0] = 1
    ci[64:, 1] = 1
    put("chunkind", ci)
    put("ustrict", (k < q))
    put("ones", np.ones((128, 128)))
    put("iota", np.tile(np.arange(256, dtype=np.float32)[None], (128, 1)))
    put("iotac", np.stack([np.arange(128), np.arange(128) + 128], 1))
    put("flag", np.full((128, 1), flag))
    return c


class Buf:
    __slots__ = ("name", "w", "r", "chan")

    def __init__(self, name):
        self.name = name
        self.w = None
        self.r = []
        self.chan = None


class Eng:
    def __init__(self, name, h, sem):
        self.name, self.h, self.sem, self.cnt, self.seen = name, h, sem, 0, {}


class Chan:
    def __init__(self, sem):
        self.sem, self.cnt = sem, 0


class Sched:
    def __init__(self, nc, es):
        self.nc, self.es = nc, es
        self.nsem = 0
        self.pe = Eng("pe", nc.tensor, self.sem("pe"))
        self.act = Eng("act", nc.scalar, self.sem("act"))
        self.dve = Eng("dve", nc.vector, self.sem("dve"))
        self.pool = Eng("pool", nc.gpsimd, self.sem("pool"))
        self.sp = Eng("sp", nc.sync, self.sem("sp"))
        self.engs = [self.pe, self.act, self.dve, self.pool, self.sp]
        self.chans = []

    def sem(self, name):
        self.nsem += 1
        return self.es.enter_context(self.nc.semaphore("s_%s_%d" % (name, self.nsem)))

    def chan(self, name="c", real=True):
        c = Chan(self.sem(name))
        self.chans.append(c)
        return c

    def _wait(self, eng, ev):
        if ev is None:
            return
        sem, val = ev
        if eng.seen.get(sem, 0) >= val:
            return
        eng.h.wait_ge(sem, val)
        eng.seen[sem] = val

    def op(self, eng, fn, reads=(), writes=()):
        for b in reads:
            self._wait(eng, b.w)
        for b in writes:
            if b.w is not None and (STRICT or b.w[0] is not eng.sem):
                self._wait(eng, b.w)
            for ev in b.r:
                if STRICT or ev[0] is not eng.sem:
                    self._wait(eng, ev)
        ins = fn(eng.h)
        eng.cnt += 1
        ins.then_inc(eng.sem, 1)
        ev = (eng.sem, eng.cnt)
        for b in writes:
            b.w = ev
            b.r = []
        for b in reads:
            b.r = [e for e in b.r if e[0] is not eng.sem] + [ev]
        return ev

    def dma(self, q, out, in_, chan=None, reads=(), writes=(), cbuf=None):
        cb = cbuf if cbuf is not None else (writes[0] if writes else reads[0])
        if cb.chan is None:
            cb.chan = self.chan(cb.name)
        chan = cb.chan
        for b in reads:
            self._wait(q, b.w)
        for b in writes:
            if b.w is not None and b.w[0] is not chan.sem:
                self._wait(q, b.w)
            for ev in b.r:
                self._wait(q, ev)
        q.h.dma_start(out=out, in_=in_).then_inc(chan.sem, 16)
        chan.cnt += 16
        ev = (chan.sem, chan.cnt)
        for b in writes:
            b.w = ev
            b.r = []
        for b in reads:
            b.r = b.r + [ev]
        return ev

    def barrier(self):
        for e in self.engs:
            for f in self.engs:
                if f is not e and f.cnt > 0:
                    self._wait(e, (f.sem, f.cnt))
            for c in self.chans:
                if c.cnt > 0:
                    self._wait(e, (c.sem, c.cnt))


def build_nc(stage=99, debug=False):
    nc = bass.Bass("TRN2", target_bir_lowering=False)

    def din(name, shape, dt=F32):
        return nc.dram_tensor(name, list(shape), dt, kind="ExternalInput").ap()
    x_own = din("x_own", [T, D])
    x_ctx = din("x_ctx", [T, D])
    cT = din("cT", [128, 16])
    pos = din("pos", [1, 2048], I32)
    w_ada = din("w_ada", [D, 6 * D])
    b_ada = din("b_ada", [1, 6 * D])
    w_in = din("w_in", [D, 7168])
    hgrn_lb = din("hgrn_lb", [1, 2048])
    gnw = din("gnw", [128, 8])
    w_o = din("w_o", [D, D])
    lnp = din("lnp", [4, D])
    router_w = din("router_w", [128, 16 * NE])
    router_b = din("router_b", [1, NE])
    if stage >= 5:
        w1 = din("w1", [NE, D, 2 * D])
        b1 = din("b1", [128, NE * 32])
        w2 = din("w2", [NE, D, D])
        b2 = din("b2", [NE, D])
    consts = din("consts", [128, NCONST])
    out = nc.dram_tensor("out", [T, D], F32, kind="ExternalOutput").ap()
    mod_d = nc.dram_tensor("mod_d", [6, D], F32, kind="Internal").ap()
    x1_d = nc.dram_tensor("x1_d", [T, D], F32, kind="Internal").ap()
    dbg = {}
    if debug:
        dbg["mixT"] = nc.dram_tensor("d_mixT", [128, 16 * T], BF16, kind="ExternalOutput").ap()
        dbg["x1"] = nc.dram_tensor("d_x1", [T, D], F32, kind="ExternalOutput").ap()
        dbg["mod"] = nc.dram_tensor("d_mod", [6, D], F32, kind="ExternalOutput").ap()
        dbg["logits"] = nc.dram_tensor("d_logits", [128, 8 * NE], F32, kind="ExternalOutput").ap()
        dbg["ffn"] = nc.dram_tensor("d_ffn", [T, D], F32, kind="ExternalOutput").ap()

    with ExitStack() as es:
        S = Sched(nc, es)
        PE, ACT, DVE, POOL, SP = S.pe, S.act, S.dve, S.pool, S.sp
        uid = [0]

        def sb(scope, shape, dt, name="t"):
            uid[0] += 1
            return scope.enter_context(nc.sbuf_tensor("%s_%d" % (name, uid[0]), list(shape), dt))

        def ps(scope, shape, dt=F32, name="p"):
            uid[0] += 1
            return scope.enter_context(nc.psum_tensor("%s_%d" % (name, uid[0]), list(shape), dt))

        cst = sb(es, [128, NCONST], F32, "cst")
        cstb = sb(es, [128, NCONST], BF16, "cstb")
        B_cst = Buf("cst")
        ch_misc = S.chan("misc")
        S.dma(SP, cst[:], consts, ch_misc, writes=[B_cst])
        S.op(DVE, lambda e: e.tensor_copy(out=cstb[:], in_=cst[:]), reads=[B_cst], writes=[B_cst])

        def C(name, bf=False, rows=slice(0, 128)):
            o, w = _c[name]
            return (cstb if bf else cst)[rows, o:o + w]

        def make_ring(scope, nslot, ncols):
            ring = [sb(scope, [128, 16, ncols], BF16, "ring") for _ in range(nslot)]
            ring_b = [Buf("ring%d" % i) for i in range(nslot)]
            ring_c = [S.chan("ring") for _ in range(nslot)]
            ring_i = [0]

            def load_w(src2d, nc_):
                i = ring_i[0] % nslot
                ring_i[0] += 1
                S.dma(POOL, ring[i][:, :, 0:nc_], src2d.rearrange("(k p) c -> p k c", p=128), ring_c[i], writes=[ring_b[i]])
                return ring[i], ring_b[i]
            return load_w

        S.barrier()

        B_mod = [Buf("mod%d" % i) for i in range(6)]
        with ExitStack() as ph:
            NSLOT = 3
            load_w = make_ring(ph, NSLOT, 512)
            cts = sb(ph, [128, 16], F32)
            sg = sb(ph, [128, 16], F32)
            silb = sb(ph, [128, 16], BF16)
            brow = [sb(ph, [1, 512], F32) for _ in range(2)]
            mrow = [sb(ph, [1, 512], F32) for _ in range(2)]
            psA = [ps(ph, [1, 512]) for _ in range(2)]
            Bc, Bsil = Buf("c"), Buf("sil")
            Bbrow = [Buf("brow"), Buf("brow")]
            Bmrow = [Buf("mrow"), Buf("mrow")]
            BpsA = [Buf("psA"), Buf("psA")]
            ch_b = [S.chan("brow"), S.chan("brow")]
            ch_m = [S.chan("mrow"), S.chan("mrow")]
            S.dma(SP, cts[:], cT, ch_misc, writes=[Bc])
            S.op(ACT, lambda e: e.activation(out=sg[:], in_=cts[:], func=AF.Sigmoid), reads=[Bc], writes=[Bsil])
            S.op(DVE, lambda e: e.tensor_tensor(out=silb[:], in0=cts[:], in1=sg[:], op=ALU.mult), reads=[Bc, Bsil], writes=[Bsil])
            pend = [load_w(w_ada[:, g * 512:(g + 1) * 512], 512) for g in range(NSLOT)]
            for g in range(24):
                wt, wb = pend.pop(0)
                k = g % 2
                S.dma(SP, brow[k][:], b_ada[0:1, g * 512:(g + 1) * 512], ch_b[k], writes=[Bbrow[k]])

                def mm(e, wt=wt, k=k):
                    for kc in range(16):
                        ins = e.matmul(psA[k][:], lhsT=silb[:, kc:kc + 1], rhs=wt[:, kc, :], start=(kc == 0), stop=(kc == 15))
                    return ins
                S.op(PE, mm, reads=[wb, Bsil], writes=[BpsA[k]])
                addc = 1.0 if (g // 4) in (1, 2, 4, 5) else 0.0
                S.op(DVE, lambda e, k=k, addc=addc: e.scalar_tensor_tensor(out=mrow[k][:], in0=psA[k][:], scalar=addc, in1=brow[k][:], op0=ALU.add, op1=ALU.add),
                     reads=[BpsA[k], Bbrow[k]], writes=[Bmrow[k]])
                S.dma(SP, mod_d[g // 4:g // 4 + 1, (g % 4) * 512:(g % 4 + 1) * 512], mrow[k][:], ch_m[k], reads=[Bmrow[k]], writes=[B_mod[g // 4]], cbuf=Bmrow[k])
                if g + NSLOT < 24:
                    pend.append(load_w(w_ada[:, (g + NSLOT) * 512:(g + NSLOT + 1) * 512], 512))
            S.barrier()
        if debug:
            with ExitStack() as ph:
                t = sb(ph, [6, D], F32)
                Bt = Buf("t")
                S.dma(SP, t[:], mod_d, ch_misc, reads=B_mod, writes=[Bt])
                S.dma(SP, dbg["mod"], t[:], ch_misc, reads=[Bt])
                S.barrier()

        def bcast_row(scope, src_row_ap, chan, reads=()):
            t = sb(scope, [128, D], F32, "row")
            b = Buf("row")
            S.dma(SP, t[:], src_row_ap.partition_broadcast(128), chan, reads=reads, writes=[b])
            return t, b

        gate = sb(es, [128, 8, NE], F32, "gate")
        mfall = sb(es, [128, 8, NE], F32, "mfall")
        mb = sb(es, [128, 8, NE], BF16, "mb")
        posm = sb(es, [128, 8, NE], F32, "posm")
        posmb = sb(es, [128, 8, NE], BF16, "posmb")
        logit = sb(es, [128, 8, NE], F32, "logit")
        Bh2bf, Bgate, Bmf, Bmb, Bposm, Bposmb, Blogit = [Buf(n) for n in ("h2bf", "gate", "mf", "mb", "posm", "posmb", "logit")]
        mixT_scope = ExitStack()
        es.enter_context(mixT_scope)
        mixT = sb(mixT_scope, [128, 16, T], BF16, "mixT")
        B_mix = [Buf("mix%d" % i) for i in range(16)]
        h2bf = mixT[:].rearrange("p (j a) t -> p j (a t)", a=2)

        with ExitStack() as phBC:
            hT = sb(phBC, [128, 16, 2048], BF16, "hT")
            Ct = sb(phBC, [128, 2048], BF16, "Ct")
            St = sb(phBC, [128, 2048], BF16, "St")
            with ExitStack() as ph2:
                posi = sb(ph2, [128, 2048], I32)
                t1 = sb(ph2, [128, 2048], F32)
                t2 = sb(ph2, [128, 2048], F32)
                t3 = sb(ph2, [128, 2048], F32)
                Bp, B1, B2, B3, BCt, BSt = [Buf(n) for n in ("posi", "t1", "t2", "t3", "Ct", "St")]
                S.dma(SP, posi[:], pos.partition_broadcast(128), ch_misc, writes=[Bp])
                S.op(DVE, lambda e: e.tensor_copy(out=t1[:], in_=posi[:]), reads=[Bp], writes=[B1])
                S.op(DVE, lambda e: e.tensor_scalar(out=t1[:], in0=t1[:], scalar1=C("invp"), scalar2=None, op0=ALU.mult), reads=[B1], writes=[B1])
                for which, shift in (("sin", 0.0), ("cos", PI / 2)):
                    S.op(DVE, lambda e, shift=shift: e.tensor_scalar(out=t2[:], in0=t1[:], scalar1=shift, scalar2=None, op0=ALU.add), reads=[B1], writes=[B2])
                    S.op(DVE, lambda e: e.tensor_scalar(out=t3[:], in0=t2[:], scalar1=1.0 / (2 * PI), scalar2=None, op0=ALU.mult), reads=[B2], writes=[B3])
                    S.op(DVE, lambda e: e.tensor_copy(out=posi[:], in_=t3[:]), reads=[B3], writes=[Bp])
                    S.op(DVE, lambda e: e.tensor_copy(out=t3[:], in_=posi[:]), reads=[Bp], writes=[B3])
                    S.op(DVE, lambda e: e.scalar_tensor_tensor(out=t3[:], in0=t3[:], scalar=-2 * PI, in1=t2[:], op0=ALU.mult, op1=ALU.add), reads=[B3, B2], writes=[B3])
                    S.op(DVE, lambda e: e.tensor_scalar(out=t3[:], in0=t3[:], scalar1=3.1415925, scalar2=-3.1415925, op0=ALU.min, op1=ALU.max), reads=[B3], writes=[B3])
                    S.op(ACT, lambda e: e.activation(out=t2[:], in_=t3[:], func=AF.Sin), reads=[B3], writes=[B2])
                    if which == "sin":
                        S.op(DVE, lambda e: e.tensor_scalar(out=St[:], in0=t2[:], scalar1=C("signp"), scalar2=None, op0=ALU.mult), reads=[B2], writes=[BSt])
                    else:
                        S.op(DVE, lambda e: e.tensor_copy(out=Ct[:], in_=t2[:]), reads=[B2], writes=[BCt])
                S.barrier()
            with ExitStack() as ph:
                sc1a, Bsc = bcast_row(ph, mod_d[1:2, :], ch_misc, reads=[B_mod[1]])
                sha, Bsh = bcast_row(ph, mod_d[0:1, :], ch_misc, reads=[B_mod[0]])
                xin = [sb(ph, [128, D], F32, "xin") for _ in range(2)]
                Bx = [Buf("xin"), Buf("xin")]
                chx = [S.chan("xin"), S.chan("xin")]
                hb = sb(ph, [128, D], BF16, "hb")
                Bhb = Buf("hb")
                pst = [ps(ph, [128, 8, 128], BF16) for _ in range(2)]
                Bpst = [Buf("pst"), Buf("pst")]
                BhT = Buf("hT")
                for tt in range(16):
                    k = tt % 2
                    src = x_ctx[tt * 128:(tt + 1) * 128, :] if tt < 8 else x_own[(tt - 8) * 128:(tt - 7) * 128, :]
                    S.dma(SP, xin[k][:], src, chx[k], writes=[Bx[k]])
                    S.op(DVE, lambda e, k=k: e.tensor_tensor(out=xin[k][:], in0=xin[k][:], in1=sc1a[:], op=ALU.mult), reads=[Bx[k], Bsc], writes=[Bx[k]])
                    S.op(DVE, lambda e, k=k: e.tensor_tensor(out=hb[:], in0=xin[k][:], in1=sha[:], op=ALU.add), reads=[Bx[k], Bsh], writes=[Bhb])
                    for hh in range(2):
                        def tr(e, hh=hh):
                            for j in range(8):
                                kc = hh * 8 + j
                                ins = e.transpose(out=pst[hh][:, j, :], in_=hb[:, kc * 128:(kc + 1) * 128], identity=C("ident", True))
                            return ins
                        S.op(PE, tr, reads=[Bhb], writes=[Bpst[hh]])
                        S.op(ACT, lambda e, hh=hh, tt=tt: e.activation(out=hT[:, hh * 8:(hh + 1) * 8, tt * 128:(tt + 1) * 128], in_=pst[hh][:], func=AF.Copy),
                             reads=[Bpst[hh]], writes=[BhT])
                S.barrier()
            if stage >= 2:
                with ExitStack() as ph:
                    load_w = make_ring(ph, 3, 256)
                    V = {o: sb(ph, [128, 16, 256], BF16, "V%d" % o) for o in (1, 4, 16)}
                    QT = sb(ph, [128, 2, T], BF16, "QT")
                    KT = sb(ph, [128, 2, 2048], BF16, "KT")
                    qraw = [sb(ph, [128, 512], BF16, "qraw") for _ in range(2)]
                    ta = sb(ph, [128, 512], F32, "ta")
                    tb = sb(ph, [128, 512], F32, "tb")
                    acc = sb(ph, [128, 2, T], F32, "acc")
                    rden = sb(ph, [128, T], F32, "rden")
                    mpair = sb(ph, [128, 3, 256], BF16, "mpair")
                    pe_sb = [sb(ph, [128, 256], BF16, "pe") for _ in range(4)]
                    pm_sb = [sb(ph, [128, 256], BF16, "pm") for _ in range(4)]
                    psP = [ps(ph, [128, 512]) for _ in range(2)]
                    psS = [ps(ph, [128, 512]) for _ in range(2)]
                    psO_ = [ps(ph, [128, 2, 512]) for _ in range(2)]
                    BV = {o: Buf("V") for o in (1, 4, 16)}
                    BQT, BKT, Bta, Btb, Bacc, Brden, Bmp = [Buf(n) for n in ("QT", "KT", "ta", "tb", "acc", "rden", "mp")]
                    Bqraw = [Buf("qraw"), Buf("qraw")]
                    Bpe = [Buf("pe") for _ in range(4)]
                    Bpm = [Buf("pm") for _ in range(4)]
                    BpsP = [Buf("psP"), Buf("psP")]
                    BpsS = [Buf("psS") for _ in range(4)]
                    BpsO_ = [Buf("psO"), Buf("psO")]
                    ocnt = [0]
                    onesb = C("ones", True)
                    S.op(DVE, lambda e: e.tensor_copy(out=mpair[:, 0, 0:128], in_=C("mprev", True)), writes=[Bmp])
                    S.op(DVE, lambda e: e.tensor_copy(out=mpair[:, 0, 128:256], in_=C("mcur", True)), writes=[Bmp])
                    S.op(DVE, lambda e: e.tensor_copy(out=mpair[:, 1, 0:128], in_=C("mprevctx", True)), writes=[Bmp])
                    S.op(DVE, lambda e: e.tensor_copy(out=mpair[:, 1, 128:256], in_=C("mcur", True)), writes=[Bmp])
                    for s4 in range(4):
                        S.op(DVE, lambda e, s4=s4: e.tensor_copy(out=mpair[:, 2, s4 * 64:(s4 + 1) * 64], in_=C("mask3", True)), writes=[Bmp])
                    pcnt = [0]
                    scnt = [0]

                    def proj_a(wt, wb, colsl, tok0, dst_ap, scale, BD):
                        k = pcnt[0] % 2
                        pcnt[0] += 1

                        def mm(e):
                            for kc in range(16):
                                ins = e.matmul(psP[k][:], lhsT=wt[:, kc, colsl], rhs=hT[:, kc, tok0:tok0 + 512], start=(kc == 0), stop=(kc == 15))
                            return ins
                        S.op(PE, mm, reads=[wb], writes=[BpsP[k]])
                        S.op(ACT, lambda e: e.activation(out=qraw[k][:], in_=psP[k][:], func=AF.Identity, scale=scale), reads=[BpsP[k]], writes=[Bqraw[k]])
                        return (k, tok0, dst_ap, BD)

                    def proj_b(st_):
                        k, tok0, dst_ap, BD = st_
                        S.op(PE, lambda e: e.matmul(psP[k][:], lhsT=C("rmat", True), rhs=qraw[k][:], start=True, stop=True), reads=[Bqraw[k]], writes=[BpsP[k]])
                        S.op(DVE, lambda e: e.tensor_tensor(out=ta[:], in0=qraw[k][:], in1=Ct[:, tok0:tok0 + 512], op=ALU.mult), reads=[Bqraw[k]], writes=[Bta])
                        S.op(DVE, lambda e: e.tensor_tensor(out=tb[:], in0=psP[k][:], in1=St[:, tok0:tok0 + 512], op=ALU.mult), reads=[BpsP[k]], writes=[Btb])
                        S.op(DVE, lambda e: e.tensor_tensor(out=dst_ap, in0=ta[:], in1=tb[:], op=ALU.add), reads=[Bta, Btb], writes=[BD])

                    def tokset(o, kt):
                        if o == 1:
                            return slice(kt * 128, (kt + 1) * 128)
                        if o == 4:
                            m, r = kt // 4, kt % 4
                            return slice(512 * m + r, 512 * (m + 1), 4)
                        return slice(kt, 2048, 16)

                    def score_unit(lhs_list, rhs_list, ncol, mask_ap, pv_list, out_col0, ob):
                        k = scnt[0] % 4
                        k2 = scnt[0] % 2
                        scnt[0] += 1
                        n = len(lhs_list)
                        psO = psO_[ob]
                        BpsO = BpsO_[ob]

                        def mm(e):
                            for i in range(n):
                                ins = e.matmul(psS[k2][:, i * ncol:(i + 1) * ncol], lhsT=lhs_list[i], rhs=rhs_list[i], start=True, stop=True)
                            return ins
                        S.op(PE, mm, reads=[BKT, BQT], writes=[BpsS[k2]])
                        S.op(ACT, lambda e: e.activation(out=pe_sb[k][:, 0:n * ncol], in_=psS[k2][:, 0:n * ncol], func=AF.Exp), reads=[BpsS[k2]], writes=[Bpe[k]])
                        S.op(DVE, lambda e: e.tensor_tensor(out=pm_sb[k][:, 0:n * ncol], in0=pe_sb[k][:, 0:n * ncol], in1=mask_ap, op=ALU.mult), reads=[Bpe[k], Bmp], writes=[Bpm[k]])

                        def pv(e):
                            for t_ in range(2):
                                for i in range(n):
                                    vt, oc, st, sp_ = pv_list[i]
                                    lhs = vt if t_ == 0 else onesb
                                    ins = e.matmul(psO[:, t_, out_col0 + oc:out_col0 + oc + ncol], lhsT=lhs, rhs=pm_sb[k][:, i * ncol:(i + 1) * ncol], start=st, stop=sp_)
                            return ins
                        S.op(PE, pv, reads=[Bpm[k], BV[1], BV[4], BV[16]], writes=[BpsO])

                    for grp in range(4):
                        c0 = grp * 256
                        wq, wqb = load_w(w_in[:, c0:c0 + 256], 256)
                        wk, wkb = load_w(w_in[:, 1024 + c0:1024 + c0 + 256], 256)
                        wv, wvb = load_w(w_in[:, 2048 + c0:2048 + c0 + 256], 256)
                        plist = []
                        for j in range(2):
                            for nt in range(2):
                                plist.append((wq, wqb, slice(j * 128, (j + 1) * 128), 1024 + nt * 512, QT[:, j, nt * 512:(nt + 1) * 512], 0.125, BQT))
                            for nt in range(4):
                                plist.append((wk, wkb, slice(j * 128, (j + 1) * 128), nt * 512, KT[:, j, nt * 512:(nt + 1) * 512], 1.0, BKT))
                        prev_ = None
                        for pa in plist:
                            cur_ = proj_a(*pa)
                            if prev_ is not None:
                                proj_b(prev_)
                            prev_ = cur_
                        proj_b(prev_)
                        for o in (1, 4, 16):
                            for kt in range(16):
                                k = pcnt[0] % 2
                                pcnt[0] += 1
                                tok = tokset(o, kt)

                                def mm(e, k=k, tok=tok):
                                    for kc in range(16):
                                        ins = e.matmul(psP[k][:, 0:256], lhsT=hT[:, kc, tok], rhs=wv[:, kc, 0:256], start=(kc == 0), stop=(kc == 15))
                                    return ins
                                S.op(PE, mm, reads=[wvb], writes=[BpsP[k]])
                                S.op(ACT, lambda e, k=k, o=o, kt=kt: e.activation(out=V[o][:, kt, :], in_=psP[k][:, 0:256], func=AF.Copy), reads=[BpsP[k]], writes=[BV[o]])
                        for j in range(2):
                            for hh in range(2):
                                pb = 64 * hh
                                KTh = KT[pb:pb + 64, j, :]
                                QTh = QT[pb:pb + 64, j, :]
                                vc = slice(j * 128, (j + 1) * 128)
                                for batch in range(2):
                                    ob = ocnt[0] % 2
                                    ocnt[0] += 1
                                    for bi in range(4):
                                        n = 8 + batch * 4 + bi
                                        qs = slice((n - 8) * 128, (n - 7) * 128)
                                        score_unit([KTh[:, (n - 1) * 128:n * 128], KTh[:, n * 128:(n + 1) * 128]], [QTh[:, qs], QTh[:, qs]], 128,
                                                   mpair[:, 1 if n == 8 else 0, :],
                                                   [(V[1][:, n - 1, vc], 0, True, False), (V[1][:, n, vc], 0, False, True)], bi * 128, ob)
                                    S.op(DVE, lambda e, batch=batch, ob=ob: e.tensor_copy(out=acc[:, :, batch * 512:(batch + 1) * 512], in_=psO_[ob][:]), reads=[BpsO_[ob]], writes=[Bacc])
                                for m in (2, 3):
                                    ob = ocnt[0] % 2
                                    ocnt[0] += 1
                                    for r in range(4):
                                        kp = slice(512 * (m - 1) + r, 512 * m, 4)
                                        kcu = slice(512 * m + r, 512 * (m + 1), 4)
                                        qs = slice(512 * (m - 2) + r, 512 * (m - 1), 4)
                                        score_unit([KTh[:, kp], KTh[:, kcu]], [QTh[:, qs], QTh[:, qs]], 128, mpair[:, 1 if m == 2 else 0, :],
                                                   [(V[4][:, 4 * (m - 1) + r, vc], 0, True, False), (V[4][:, 4 * m + r, vc], 0, False, True)], r * 128, ob)
                                    av = acc[:, :, 512 * (m - 2):512 * (m - 1)].rearrange("p t (i r) -> p t r i", r=4)
                                    S.op(DVE, lambda e, av=av, ob=ob: e.tensor_tensor(out=av, in0=av, in1=psO_[ob][:].rearrange("p t (r i) -> p t r i", r=4), op=ALU.add), reads=[BpsO_[ob], Bacc], writes=[Bacc])
                                for h8 in range(2):
                                    ob = ocnt[0] % 2
                                    ocnt[0] += 1
                                    for rq in range(2):
                                        rs = [h8 * 8 + rq * 4 + s_ for s_ in range(4)]
                                        score_unit([KTh[:, r:2048:16] for r in rs], [QTh[:, r:1024:16] for r in rs], 64, mpair[:, 2, :],
                                                   [(V[16][:, r, vc], i_ * 64, True, True) for i_, r in enumerate(rs)], rq * 256, ob)
                                    av = acc[:, :, :].rearrange("p t (i r) -> p t r i", r=16)[:, :, h8 * 8:(h8 + 1) * 8, :]
                                    S.op(DVE, lambda e, av=av, ob=ob: e.tensor_tensor(out=av, in0=av, in1=psO_[ob][:].rearrange("p t (r i) -> p t r i", r=8), op=ALU.add), reads=[BpsO_[ob], Bacc], writes=[Bacc])
                                ci_ = grp * 2 + j
                                S.op(DVE, lambda e, pb=pb: e.reciprocal(out=rden[pb:pb + 64, :], in_=acc[pb:pb + 64, 1, :]), reads=[Bacc], writes=[Brden])
                                S.op(DVE, lambda e, pb=pb, ci_=ci_: e.tensor_tensor(out=mixT[pb:pb + 64, ci_, :], in0=acc[pb:pb + 64, 0, :], in1=rden[pb:pb + 64, :], op=ALU.mult),
                                     reads=[Bacc, Brden], writes=[B_mix[ci_]])
                    S.barrier()
            if stage >= 2:
                with ExitStack() as ph:
                    load_w = make_ring(ph, 4, 256)
                    lb = sb(ph, [128, 1024], F32, "lb")
                    oml = sb(ph, [128, 1024], F32, "oml")
                    gnws = sb(ph, [128, 8], F32, "gnws")
                    ke = sb(ph, [128, 16, 256], BF16, "ke")
                    vv = sb(ph, [128, 16, 256], BF16, "vv")
                    keT = sb(ph, [128, 2, 2048], BF16, "keT")
                    qeT = sb(ph, [128, 2, T], BF16, "qeT")
                    gT = sb(ph, [128, 2, T], BF16, "gT")
                    ebl = sb(ph, [128, 2, 16, 2], F32, "ebl")
                    sgm = sb(ph, [128, 256], F32, "sgm")
                    fv = sb(ph, [128, 256], F32, "fv")
                    gl = sb(ph, [128, 256], F32, "gl")
                    kk = sb(ph, [128, 256], F32, "kk")
                    eb = sb(ph, [128, 256], F32, "eb")
                    enb = sb(ph, [128, 256], F32, "enb")
                    qe_t = sb(ph, [128, 256], BF16, "qe_t")
                    S32 = [sb(ph, [128, 128], F32, "S32") for _ in range(2)]
                    Sb_ = [sb(ph, [128, 128], BF16, "Sb") for _ in range(2)]
                    stmp = [sb(ph, [128, 128], F32, "stmp") for _ in range(2)]
                    aT = [sb(ph, [128, 128], BF16, "aT") for _ in range(2)]
                    sq = [sb(ph, [128, 128], BF16, "sq") for _ in range(2)]
                    rt = [sb(ph, [128, 128], F32, "rt") for _ in range(2)]
                    ot = [sb(ph, [128, 128], F32, "ot") for _ in range(2)]
                    psP = [ps(ph, [128, 512]) for _ in range(2)]
                    psB = ps(ph, [128, 512])
                    psT = ps(ph, [128, 4, 128], BF16)
                    psX = ps(ph, [128, 4, 128])
                    psOT = ps(ph, [128, 2, 128 * 2])
                    psD = [ps(ph, [128, 512]) for _ in range(2)]
                    (Blb, Bke, Bvv, BkeT, BqeT, BgT, Bebl, Bsgm, Bfv, Bgl, Bkk, Beb, Benb, Bqe, BpsB, BpsT, Bgnw) = [
                        Buf(n) for n in ("lb", "ke", "vv", "keT", "qeT", "gT", "ebl", "sgm", "fv", "gl", "kk", "eb", "enb", "qe", "psB", "psT", "gnw")]
                    Bsq, Brt, Bot, BpsYd = [[Buf(n), Buf(n)] for n in ("sq", "rt", "ot", "psYd")]
                    _bx, _bo = Buf("psX"), Buf("psOT")
                    BpsXa, BpsXs, BpsYo = [_bx, _bx], [_bx, _bx], [_bo, _bo]
                    BS32 = [Buf("S32"), Buf("S32")]
                    BSb = [Buf("Sb"), Buf("Sb")]
                    Bstmp = [Buf("stmp"), Buf("stmp")]
                    BaT = [Buf("aT"), Buf("aT")]
                    BpsP = [Buf("psP"), Buf("psP")]
                    onesb = C("ones", True)
                    S.dma(SP, gnws[:], gnw, ch_misc, writes=[Bgnw])
                    S.dma(SP, lb[:], hgrn_lb[0:1, 0:1024].partition_broadcast(128), ch_misc, writes=[Blb])
                    S.dma(SP, oml[:], hgrn_lb[0:1, 1024:2048].partition_broadcast(128), ch_misc, writes=[Blb])
                    S.op(DVE, lambda e: e.tensor_tensor(out=oml[:], in0=lb[:], in1=oml[:], op=ALU.subtract), reads=[Blb], writes=[Blb])
                    S.op(ACT, lambda e: e.activation(out=lb[:], in_=oml[:], func=AF.Sigmoid), reads=[Blb], writes=[Blb])
                    S.op(DVE, lambda e: e.tensor_scalar(out=oml[:], in0=lb[:], scalar1=-1.0, scalar2=1.0, op0=ALU.mult, op1=ALU.add), reads=[Blb], writes=[Blb])
                    pcnt = [0]

                    def proj_tm(wt, wb, tt):
                        k = pcnt[0] % 2
                        pcnt[0] += 1

                        def mm(e):
                            for kc in range(16):
                                ins = e.matmul(psP[k][:, 0:256], lhsT=hT[:, kc, tt * 128:(tt + 1) * 128], rhs=wt[:, kc, 0:256], start=(kc == 0), stop=(kc == 15))
                            return ins
                        S.op(PE, mm, reads=[wb], writes=[BpsP[k]])
                        return k

                    for hp in range(4):
                        wqr, wqrb = load_w(w_in[:, 3072 + hp * 256:3072 + (hp + 1) * 256], 256)
                        wf, wfb = load_w(w_in[:, 4096 + hp * 256:4096 + (hp + 1) * 256], 256)
                        wi, wib = load_w(w_in[:, 5120 + hp * 256:5120 + (hp + 1) * 256], 256)
                        wg, wgb = load_w(w_in[:, 6144 + hp * 256:6144 + (hp + 1) * 256], 256)
                        lsl = slice(hp * 256, (hp + 1) * 256)
                        for hd in range(2):
                            for nt in range(2):
                                k = pcnt[0] % 2
                                pcnt[0] += 1

                                def mm(e, k=k, hd=hd, nt=nt):
                                    for kc in range(16):
                                        ins = e.matmul(psP[k][:], lhsT=wg[:, kc, hd * 128:(hd + 1) * 128], rhs=hT[:, kc, 1024 + nt * 512:1024 + (nt + 1) * 512], start=(kc == 0), stop=(kc == 15))
                                    return ins
                                S.op(PE, mm, reads=[wgb], writes=[BpsP[k]])
                                S.op(ACT, lambda e, k=k, hd=hd, nt=nt: e.activation(out=gT[:, hd, nt * 512:(nt + 1) * 512], in_=psP[k][:], func=AF.Silu), reads=[BpsP[k]], writes=[BgT])
                        for tt in range(16):
                            own = tt >= 8
                            k = proj_tm(wf, wfb, tt)
                            S.op(ACT, lambda e, k=k: e.activation(out=sgm[:], in_=psP[k][:, 0:256], func=AF.Sigmoid), reads=[BpsP[k]], writes=[Bsgm])
                            S.op(DVE, lambda e: e.tensor_tensor(out=fv[:], in0=sgm[:], in1=oml[:, lsl], op=ALU.mult), reads=[Bsgm, Blb], writes=[Bfv])
                            S.op(DVE, lambda e: e.tensor_tensor(out=fv[:], in0=fv[:], in1=lb[:, lsl], op=ALU.add), reads=[Bfv, Blb], writes=[Bfv])
                            S.op(ACT, lambda e: e.activation(out=gl[:], in_=fv[:], func=AF.Ln), reads=[Bfv], writes=[Bgl])
                            S.op(DVE, lambda e: e.tensor_scalar(out=kk[:], in0=fv[:], scalar1=-1.0, scalar2=1.0, op0=ALU.mult, op1=ALU.add), reads=[Bfv], writes=[Bkk])

                            def cs(e):
                                e.matmul(psB[:, 0:256], lhsT=C("ltri"), rhs=gl[:], start=True, stop=True)
                                for hd in range(2):
                                    ins = e.matmul(psB[:, 256 + 2 * hd:258 + 2 * hd], lhsT=gl[:, hd * 128:(hd + 1) * 128], rhs=C("chunkind"), start=True, stop=True)
                                return ins
                            S.op(PE, cs, reads=[Bgl], writes=[BpsB])
                            S.op(ACT, lambda e: e.activation(out=enb[:], in_=psB[:, 0:256], func=AF.Exp, scale=-1.0), reads=[BpsB], writes=[Benb])
                            if own:
                                S.op(ACT, lambda e: e.activation(out=eb[:], in_=psB[:, 0:256], func=AF.Exp), reads=[BpsB], writes=[Beb])
                            S.op(ACT, lambda e, tt=tt: e.activation(out=ebl[:, :, tt, :], in_=psB[:, 256:260].rearrange("p (h c) -> p h c", c=2), func=AF.Exp), reads=[BpsB], writes=[Bebl])
                            S.op(DVE, lambda e, tt=tt: e.tensor_tensor(out=ke[:, tt, :], in0=kk[:], in1=enb[:], op=ALU.mult), reads=[Bkk, Benb], writes=[Bke])
                            k = proj_tm(wi, wib, tt)
                            S.op(ACT, lambda e, k=k, tt=tt: e.activation(out=vv[:, tt, :], in_=psP[k][:, 0:256], func=AF.Copy), reads=[BpsP[k]], writes=[Bvv])
                            if own:
                                k = proj_tm(wqr, wqrb, tt)
                                S.op(DVE, lambda e, k=k: e.tensor_tensor(out=qe_t[:], in0=psP[k][:, 0:256], in1=eb[:], op=ALU.mult), reads=[BpsP[k], Beb], writes=[Bqe])

                            def tr(e, tt=tt, own=own):
                                for hd in range(2):
                                    ins = e.transpose(out=psT[:, hd, :], in_=ke[:, tt, hd * 128:(hd + 1) * 128], identity=C("ident", True))
                                if own:
                                    for hd in range(2):
                                        ins = e.transpose(out=psT[:, 2 + hd, :], in_=qe_t[:, hd * 128:(hd + 1) * 128], identity=C("ident", True))
                                return ins
                            S.op(PE, tr, reads=[Bke] + ([Bqe] if own else []), writes=[BpsT])
                            S.op(ACT, lambda e, tt=tt: e.activation(out=keT[:, :, tt * 128:(tt + 1) * 128], in_=psT[:, 0:2, :], func=AF.Copy), reads=[BpsT], writes=[BkeT])
                            if own:
                                S.op(ACT, lambda e, tt=tt: e.activation(out=qeT[:, :, (tt - 8) * 128:(tt - 7) * 128], in_=psT[:, 2:4, :], func=AF.Copy), reads=[BpsT], writes=[BqeT])
                        for hd in range(2):
                            S.op(DVE, lambda e, hd=hd: e.memset(S32[hd][:], 0.0), writes=[BS32[hd]])
                            S.op(ACT, lambda e, hd=hd: e.activation(out=Sb_[hd][:], in_=S32[hd][:], func=AF.Copy), reads=[BS32[hd]], writes=[BSb[hd]])
                        for tt in range(16):
                            own = tt >= 8
                            qsl = slice((tt - 8) * 128, (tt - 7) * 128)
                            if own:
                                for hd in range(2):
                                    S.op(PE, lambda e, hd=hd, tt=tt, qsl=qsl: e.matmul(psX[:, hd, :], lhsT=keT[:, hd, tt * 128:(tt + 1) * 128], rhs=qeT[:, hd, qsl], start=True, stop=True),
                                         reads=[BkeT, BqeT], writes=[BpsXa[hd]])
                                    S.op(DVE, lambda e, hd=hd: e.tensor_tensor(out=aT[hd][:], in0=psX[:, hd, :], in1=C("bdmask", True), op=ALU.mult), reads=[BpsXa[hd]], writes=[BaT[hd]])
                            for c in range(2):
                                rows = slice(c * 64, (c + 1) * 64)
                                for hd in range(2):
                                    hsl = slice(hd * 128, (hd + 1) * 128)
                                    if own:
                                        def om(e, hd=hd, tt=tt, c=c, rows=rows, hsl=hsl):
                                            e.matmul(psOT[:, hd, c * 64:(c + 1) * 64], lhsT=vv[rows, tt, hsl], rhs=aT[hd][rows, c * 64:(c + 1) * 64], start=True, stop=False)
                                            return e.matmul(psOT[:, hd, c * 64:(c + 1) * 64], lhsT=Sb_[hd][:], rhs=qeT[:, hd, (tt - 8) * 128 + c * 64:(tt - 8) * 128 + (c + 1) * 64], start=False, stop=True)
                                        S.op(PE, om, reads=[Bvv, BaT[hd], BSb[hd], BqeT], writes=[BpsYo[hd]])
                                    S.op(PE, lambda e, tt=tt, rows=rows, hsl=hsl, hd=hd: e.matmul(psD[hd][:, 0:128], lhsT=ke[rows, tt, hsl], rhs=vv[rows, tt, hsl], start=True, stop=True),
                                         reads=[Bke, Bvv], writes=[BpsYd[hd]])
                                    S.op(DVE, lambda e, hd=hd: e.tensor_tensor(out=stmp[hd][:], in0=psD[hd][:, 0:128], in1=S32[hd][:], op=ALU.add), reads=[BpsYd[hd], BS32[hd]], writes=[Bstmp[hd]])
                                    if tt == 7 and c == 1:
                                        S.op(DVE, lambda e, hd=hd: e.tensor_scalar(out=stmp[hd][:], in0=stmp[hd][:], scalar1=C("flag"), scalar2=None, op0=ALU.mult), reads=[Bstmp[hd]], writes=[Bstmp[hd]])
                                    S.op(ACT, lambda e, hd=hd, tt=tt, c=c: e.activation(out=Sb_[hd][:], in_=stmp[hd][:], func=AF.Identity, scale=ebl[:, hd, tt, c:c + 1]), reads=[Bstmp[hd], Bebl], writes=[BSb[hd]])
                                    S.op(DVE, lambda e, hd=hd, tt=tt, c=c: e.tensor_scalar(out=S32[hd][:], in0=stmp[hd][:], scalar1=ebl[:, hd, tt, c:c + 1], scalar2=None, op0=ALU.mult),
                                         reads=[Bstmp[hd], Bebl], writes=[BS32[hd]])
                            if own:
                                for hd in range(2):
                                    ci_ = 8 + hp * 2 + hd
                                    S.op(ACT, lambda e, hd=hd: e.activation(out=sq[hd][:], in_=psOT[:, hd, 0:128], func=AF.Square), reads=[BpsYo[hd]], writes=[Bsq[hd]])
                                    S.op(PE, lambda e, hd=hd: e.matmul(psX[:, 2 + hd, :], lhsT=onesb, rhs=sq[hd][:], start=True, stop=True), reads=[Bsq[hd]], writes=[BpsXs[hd]])
                                    S.op(ACT, lambda e, hd=hd: e.activation(out=rt[hd][:], in_=psX[:, 2 + hd, :], func=AF.Sqrt, scale=1.0 / 128.0, bias=1e-6), reads=[BpsXs[hd]], writes=[Brt[hd]])
                                    S.op(DVE, lambda e, hd=hd: e.reciprocal(out=rt[hd][:], in_=rt[hd][:]), reads=[Brt[hd]], writes=[Brt[hd]])
                                    S.op(DVE, lambda e, hd=hd: e.tensor_tensor(out=ot[hd][:], in0=psOT[:, hd, 0:128], in1=rt[hd][:], op=ALU.mult), reads=[BpsYo[hd], Brt[hd]], writes=[Bot[hd]])
                                    S.op(DVE, lambda e, ci_=ci_, qsl=qsl, hd=hd, hp=hp: e.scalar_tensor_tensor(out=mixT[:, ci_, qsl], in0=ot[hd][:], scalar=gnws[:, hp * 2 + hd:hp * 2 + hd + 1], in1=gT[:, hd, qsl], op0=ALU.mult, op1=ALU.mult),
                                         reads=[Bot[hd], BgT, Bgnw], writes=[B_mix[ci_]])
                    S.barrier()
        if debug:
            S.dma(SP, dbg["mixT"], mixT[:].rearrange("p c t -> p (c t)"), ch_misc, reads=B_mix)
            S.barrier()
        if stage >= 3:
            def ln_tile(yb, junk, st, Byb, Bjunk, Bst, grow, brow_, Bg, Bb):
                S.op(DVE, lambda e: e.memset(st[:], 0.0), writes=[Bst])
                S.op(DVE, lambda e: e.reduce_sum(out=st[:, 0:1], in_=yb[:], axis=AX.X), reads=[Byb], writes=[Bst])
                S.op(DVE, lambda e: e.tensor_scalar(out=st[:, 1:2], in0=st[:, 0:1], scalar1=-1.0 / D, scalar2=None, op0=ALU.mult), reads=[Bst], writes=[Bst])
                S.op(DVE, lambda e: e.tensor_scalar(out=yb[:], in0=yb[:], scalar1=st[:, 1:2], scalar2=None, op0=ALU.add), reads=[Byb, Bst], writes=[Byb])
                S.op(ACT, lambda e: e.activation(out=junk[:], in_=yb[:], func=AF.Square, accum_out=st[:, 2:3]), reads=[Byb, Bst], writes=[Bjunk, Bst])
                S.op(ACT, lambda e: e.activation(out=st[:, 3:4], in_=st[:, 2:3], func=AF.Sqrt, scale=1.0 / D, bias=1e-5), reads=[Bst], writes=[Bst])
                S.op(DVE, lambda e: e.reciprocal(out=st[:, 4:5], in_=st[:, 3:4]), reads=[Bst], writes=[Bst])
                S.op(DVE, lambda e: e.scalar_tensor_tensor(out=yb[:], in0=yb[:], scalar=st[:, 4:5], in1=grow[:], op0=ALU.mult, op1=ALU.mult), reads=[Byb, Bst, Bg], writes=[Byb])
                S.op(DVE, lambda e: e.tensor_tensor(out=yb[:], in0=yb[:], in1=brow_[:], op=ALU.add), reads=[Byb, Bb], writes=[Byb])

            B_x1d = Buf("x1d")
            with ExitStack() as ph:
                wo = sb(ph, [128, 16, D], BF16, "wo")
                Bwo = Buf("wo")
                ch_wo = S.chan("wo")
                for n in range(4):
                    S.dma(POOL, wo[:, :, n * 512:(n + 1) * 512], w_o[:, n * 512:(n + 1) * 512].rearrange("(k p) c -> p k c", p=128), ch_wo, writes=[Bwo])
                gt1, Bgt1 = bcast_row(ph, mod_d[2:3, :], ch_misc, reads=[B_mod[2]])
                g1, Bg1 = bcast_row(ph, lnp[0:1, :], ch_misc)
                b1r, Bb1r = bcast_row(ph, lnp[1:2, :], ch_misc)
                scf, Bscf = bcast_row(ph, mod_d[4:5, :], ch_misc, reads=[B_mod[4]])
                shf, Bshf = bcast_row(ph, mod_d[3:4, :], ch_misc, reads=[B_mod[3]])
                rw = sb(ph, [128, 16, NE], F32, "rw")
                rb = sb(ph, [128, NE], F32, "rb")
                Brw = Buf("rw")
                S.dma(SP, rw[:], router_w.rearrange("p (k e) -> p k e", e=NE), ch_misc, writes=[Brw])
                S.dma(SP, rb[:], router_b.partition_broadcast(128), ch_misc, writes=[Brw])
                xin = sb(ph, [128, D], F32, "xin")
                yb = sb(ph, [128, D], F32, "yb")
                h2T = sb(ph, [128, 16, 128], F32, "h2T")
                st = sb(ph, [128, 8], F32, "st")
                t8 = sb(ph, [128, 8], F32, "t8")
                ex = sb(ph, [128, NE], F32, "ex")
                sm = sb(ph, [128, 4], F32, "sm")
                Bxin, Byb, Bh2T, Bst, Bt8, Bex, Bsm = [Buf(n) for n in ("xin", "yb", "h2T", "st", "t8", "ex", "sm")]
                chx = S.chan("xinD")
                chy = S.chan("x1st")
                psM = ps(ph, [128, 4, 512])
                psT4 = ps(ph, [128, 4, 128])
                psL = ps(ph, [128, NE])
                BpsM, BpsT4, BpsL = Buf("psM"), Buf("psT4"), Buf("psL")
                for j in range(8):
                    tsl = slice(j * 128, (j + 1) * 128)
                    S.dma(SP, xin[:], x_own[tsl, :], chx, writes=[Bxin])

                    def mm(e, tsl=tsl):
                        for n in range(4):
                            for kc in range(16):
                                ins = e.matmul(psM[:, n, :], lhsT=mixT[:, kc, tsl], rhs=wo[:, kc, n * 512:(n + 1) * 512], start=(kc == 0), stop=(kc == 15))
                        return ins
                    S.op(PE, mm, reads=[Bwo] + B_mix, writes=[BpsM])
                    S.op(DVE, lambda e: e.tensor_tensor(out=yb[:], in0=psM[:].rearrange("p n c -> p (n c)"), in1=gt1[:], op=ALU.mult), reads=[BpsM, Bgt1], writes=[Byb])
                    S.op(DVE, lambda e: e.scalar_tensor_tensor(out=yb[:], in0=xin[:], scalar=ALPHA, in1=yb[:], op0=ALU.mult, op1=ALU.add), reads=[Bxin, Byb], writes=[Byb])
                    ln_tile(yb, xin, st, Byb, Bxin, Bst, g1, b1r, Bg1, Bb1r)
                    S.dma(SP, x1_d[tsl, :], yb[:], chy, reads=[Byb], writes=[B_x1d], cbuf=Byb)
                    if debug:
                        S.dma(SP, dbg["x1"][tsl, :], yb[:], chy, reads=[Byb])
                S.barrier()
                for j in range(8):
                    tsl = slice(j * 128, (j + 1) * 128)
                    S.dma(SP, yb[:], x1_d[tsl, :], chx, reads=[B_x1d], writes=[Byb])
                    S.op(DVE, lambda e: e.tensor_tensor(out=xin[:], in0=yb[:], in1=scf[:], op=ALU.mult), reads=[Byb, Bscf], writes=[Bxin])
                    S.op(DVE, lambda e: e.tensor_tensor(out=xin[:], in0=xin[:], in1=shf[:], op=ALU.add), reads=[Bxin, Bshf], writes=[Bxin])
                    S.op(ACT, lambda e, j=j: e.activation(out=h2bf[:, j, :], in_=xin[:], func=AF.Copy), reads=[Bxin], writes=[Bh2bf])
                    for q4 in range(4):
                        def tr(e, q4=q4):
                            for i in range(4):
                                kc = q4 * 4 + i
                                ins = e.transpose(out=psT4[:, i, :], in_=xin[:, kc * 128:(kc + 1) * 128], identity=C("ident"))
                            return ins
                        S.op(PE, tr, reads=[Bxin], writes=[BpsT4])
                        S.op(ACT, lambda e, q4=q4: e.activation(out=h2T[:, q4 * 4:(q4 + 1) * 4, :], in_=psT4[:], func=AF.Copy), reads=[BpsT4], writes=[Bh2T])

                    def lg(e):
                        for kc in range(16):
                            ins = e.matmul(psL[:], lhsT=h2T[:, kc, :], rhs=rw[:, kc, :], start=(kc == 0), stop=(kc == 15))
                        return ins
                    S.op(PE, lg, reads=[Bh2T, Brw], writes=[BpsL])
                    S.op(DVE, lambda e, j=j: e.tensor_tensor(out=logit[:, j, :], in0=psL[:], in1=rb[:], op=ALU.add), reads=[BpsL, Brw], writes=[Blogit])
                    S.op(DVE, lambda e, j=j: e.max(out=t8[:], in_=logit[:, j, :]), reads=[Blogit], writes=[Bt8])
                    S.op(DVE, lambda e, j=j: e.tensor_scalar(out=mfall[:, j, :], in0=logit[:, j, :], scalar1=t8[:, 3:4], scalar2=None, op0=ALU.is_ge), reads=[Blogit, Bt8], writes=[Bmf])
                    S.op(DVE, lambda e: e.tensor_scalar(out=sm[:, 0:1], in0=t8[:, 0:1], scalar1=-1.0, scalar2=None, op0=ALU.mult), reads=[Bt8], writes=[Bsm])
                    S.op(ACT, lambda e, j=j: e.activation(out=ex[:], in_=logit[:, j, :], func=AF.Exp, bias=sm[:, 0:1], scale=1.0), reads=[Blogit, Bsm], writes=[Bex])
                    S.op(DVE, lambda e, j=j: e.tensor_tensor(out=ex[:], in0=ex[:], in1=mfall[:, j, :], op=ALU.mult), reads=[Bex, Bmf], writes=[Bex])
                    S.op(DVE, lambda e: e.reduce_sum(out=sm[:, 1:2], in_=ex[:], axis=AX.X), reads=[Bex], writes=[Bsm])
                    S.op(DVE, lambda e: e.reciprocal(out=sm[:, 2:3], in_=sm[:, 1:2]), reads=[Bsm], writes=[Bsm])
                    S.op(DVE, lambda e, j=j: e.tensor_scalar(out=gate[:, j, :], in0=ex[:], scalar1=sm[:, 2:3], scalar2=None, op0=ALU.mult), reads=[Bex, Bsm], writes=[Bgate])
                    S.op(DVE, lambda e, j=j: e.tensor_copy(out=mb[:, j, :], in_=mfall[:, j, :]), reads=[Bmf], writes=[Bmb])
                for j in range(8):
                    def pc(e, j=j):
                        for i in range(j):
                            e.matmul(psL[:], lhsT=C("ones", True), rhs=mb[:, i, :], start=(i == 0), stop=False)
                        return e.matmul(psL[:], lhsT=C("ustrict", True), rhs=mb[:, j, :], start=(j == 0), stop=True)
                    S.op(PE, pc, reads=[Bmb], writes=[BpsL])
                    S.op(DVE, lambda e, j=j: e.scalar_tensor_tensor(out=posm[:, j, :], in0=psL[:], scalar=1.0, in1=mfall[:, j, :], op0=ALU.add, op1=ALU.mult), reads=[BpsL, Bmf], writes=[Bposm])
                    S.op(DVE, lambda e, j=j: e.tensor_scalar(out=posm[:, j, :], in0=posm[:, j, :], scalar1=-1.0, scalar2=float(CAP), op0=ALU.add, op1=ALU.min), reads=[Bposm], writes=[Bposm])
                    S.op(DVE, lambda e, j=j: e.tensor_copy(out=posmb[:, j, :], in_=posm[:, j, :]), reads=[Bposm], writes=[Bposmb])
                if debug:
                    S.dma(SP, dbg["logits"], logit[:].rearrange("p j e -> p (j e)"), ch_misc, reads=[Blogit])
                S.barrier()
        if stage >= 5:
            accF_scope = ExitStack()
            es.enter_context(accF_scope)
            accF = sb(accF_scope, [128, 8, D], F32, "accF")
            BaccF = Buf("accF")
            S.op(DVE, lambda e: e.memset(accF[:], 0.0), writes=[BaccF])
            with ExitStack() as ph:
                NSLOT = 3
                load_w = make_ring(ph, NSLOT, 512)
                b1s = sb(ph, [128, NE * 32], F32, "b1s")
                Bb1 = Buf("b1s")
                S.dma(SP, b1s[:], b1, ch_misc, writes=[Bb1])
                Pe = sb(ph, [128, 8, CAP], BF16, "Pe")
                PT = [sb(ph, [128, 2, T], BF16, "PT") for _ in range(2)]
                xg = sb(ph, [128, 16, CAP], BF16, "xg")
                actT = sb(ph, [128, 16, CAP], BF16, "actT")
                ysb = sb(ph, [128, 2, D], BF16, "ysb")
                xgl = [sb(ph, [128, CAP], F32, "xgl") for _ in range(2)]
                sgd = [sb(ph, [128, CAP], F32, "sgd") for _ in range(2)]
                xl = [sb(ph, [128, CAP], F32, "xl") for _ in range(2)]
                BPe, Bxg, BactT, Bysb = [Buf(n) for n in ("Pe", "xg", "actT", "ysb")]
                BPT = [Buf("PT"), Buf("PT")]
                Bxgl = [Buf("xgl"), Buf("xgl")]
                Bsgd = [Buf("sgd"), Buf("sgd")]
                Bxl = [Buf("xl"), Buf("xl")]
                psPT = ps(ph, [128, 512])
                psG = ps(ph, [128, 2, CAP])
                psH = [ps(ph, [128, 2, CAP]) for _ in range(2)]
                psY2 = [ps(ph, [128, 512]) for _ in range(2)]
                psSc = [ps(ph, [128, 512]) for _ in range(2)]
                BpsPT, BpsG = Buf("psPT"), Buf("psG")
                BpsH = [Buf("psH"), Buf("psH")]
                BpsY2 = [Buf("psY2"), Buf("psY2")]
                BpsSc = [Buf("psSc"), Buf("psSc")]
                items = []
                for e_ in range(NE):
                    for g8 in range(8):
                        items.append(("w1", e_, g8))
                    for n in range(4):
                        items.append(("w2", e_, n))

                def issue(it):
                    kind, e_, g = it
                    if kind == "w1":
                        return load_w(w1[e_, :, g * 512:(g + 1) * 512], 512)
                    return load_w(w2[e_, :, g * 512:(g + 1) * 512], 512)
                pend = [issue(items[i]) for i in range(NSLOT)]
                nxt = [NSLOT]

                def next_w():
                    return pend.pop(0)

                def refill():
                    if nxt[0] < len(items):
                        pend.append(issue(items[nxt[0]]))
                        nxt[0] += 1
                hcnt = [0]
                ccnt = [0]

                def build_dispatch(e_):
                    for j in range(8):
                        S.op(DVE, lambda e, j=j: e.tensor_scalar(out=Pe[:, j, :], in0=C("iota")[:, 0:CAP], scalar1=posm[:, j, e_:e_ + 1], scalar2=None, op0=ALU.is_equal),
                             reads=[Bposm], writes=[BPe])
                    pt = PT[e_ % 2]
                    for hf_ in range(2):
                        def ptm(e, hf_=hf_):
                            for j4 in range(4):
                                j = hf_ * 4 + j4
                                ins = e.matmul(psPT[:, j4 * 128:(j4 + 1) * 128], lhsT=posmb[:, j, e_:e_ + 1].to_broadcast([128, 128]), rhs=C("ident", True), start=True, stop=True)
                            return ins
                        S.op(PE, ptm, reads=[Bposmb], writes=[BpsPT])
                        for p in range(2):
                            S.op(DVE, lambda e, p=p, hf_=hf_: e.tensor_scalar(out=pt[:, p, hf_ * 512:(hf_ + 1) * 512], in0=psPT[:], scalar1=C("iotac")[:, p:p + 1], scalar2=None, op0=ALU.is_equal),
                                 reads=[BpsPT], writes=[BPT[e_ % 2]])

                def gather_unit(kq):
                    def gm(e):
                        for k2 in range(2):
                            kc = kq * 2 + k2
                            for j in range(8):
                                ins = e.matmul(psG[:, k2, :], lhsT=h2bf[:, j, kc * 128:(kc + 1) * 128], rhs=Pe[:, j, :], start=(j == 0), stop=(j == 7))
                        return ins
                    S.op(PE, gm, reads=[Bh2bf, BPe], writes=[BpsG])
                    S.op(ACT, lambda e: e.activation(out=xg[:, kq * 2:(kq + 1) * 2, :], in_=psG[:], func=AF.Copy), reads=[BpsG], writes=[Bxg])

                def scatter_unit(e_, u):
                    j, n = u // 4, u % 4
                    k = ccnt[0] % 2
                    ccnt[0] += 1
                    pt = PT[e_ % 2]

                    def sm_(e):
                        for p in range(2):
                            ins = e.matmul(psSc[k][:], lhsT=pt[:, p, j * 128:(j + 1) * 128], rhs=ysb[:, p, n * 512:(n + 1) * 512], start=(p == 0), stop=(p == 1))
                        return ins
                    S.op(PE, sm_, reads=[BPT[e_ % 2], Bysb], writes=[BpsSc[k]])
                    av = accF[:, j, n * 512:(n + 1) * 512]
                    S.op(DVE, lambda e: e.scalar_tensor_tensor(out=av, in0=psSc[k][:], scalar=gate[:, j, e_:e_ + 1], in1=av, op0=ALU.mult, op1=ALU.add),
                         reads=[BpsSc[k], Bgate, BaccF], writes=[BaccF])

                build_dispatch(0)
                for kq in range(8):
                    gather_unit(kq)
                for e_ in range(NE):
                    for g8 in range(8):
                        wt, wb = next_w()
                        for f2 in range(2):
                            fc = g8 * 2 + f2
                            k = hcnt[0] % 2
                            hcnt[0] += 1

                            def hm(e, wt=wt, f2=f2, k=k):
                                for t_ in range(2):
                                    for kc in range(16):
                                        ins = e.matmul(psH[k][:, t_, :], lhsT=wt[:, kc, f2 * 256 + t_:(f2 + 1) * 256:2], rhs=xg[:, kc, :], start=(kc == 0), stop=(kc == 15))
                                return ins
                            S.op(PE, hm, reads=[wb, Bxg], writes=[BpsH[k]])
                            bi = e_ * 32 + fc * 2
                            S.op(DVE, lambda e, k=k, bi=bi: e.tensor_scalar(out=xgl[k][:], in0=psH[k][:, 0, :], scalar1=b1s[:, bi:bi + 1], scalar2=7.0, op0=ALU.add, op1=ALU.min), reads=[BpsH[k], Bb1], writes=[Bxgl[k]])
                            S.op(ACT, lambda e, k=k: e.activation(out=sgd[k][:], in_=xgl[k][:], func=AF.Sigmoid, scale=1.702), reads=[Bxgl[k]], writes=[Bsgd[k]])
                            S.op(DVE, lambda e, k=k, bi=bi: e.tensor_scalar(out=xl[k][:], in0=psH[k][:, 1, :], scalar1=b1s[:, bi + 1:bi + 2], scalar2=7.0, op0=ALU.add, op1=ALU.min), reads=[BpsH[k], Bb1], writes=[Bxl[k]])
                            S.op(DVE, lambda e, k=k: e.tensor_scalar(out=xl[k][:], in0=xl[k][:], scalar1=-7.0, scalar2=1.0, op0=ALU.max, op1=ALU.add), reads=[Bxl[k]], writes=[Bxl[k]])
                            S.op(DVE, lambda e, k=k: e.tensor_tensor(out=xgl[k][:], in0=xgl[k][:], in1=sgd[k][:], op=ALU.mult), reads=[Bxgl[k], Bsgd[k]], writes=[Bxgl[k]])
                            S.op(DVE, lambda e, fc=fc, k=k: e.tensor_tensor(out=actT[:, fc, :], in0=xgl[k][:], in1=xl[k][:], op=ALU.mult), reads=[Bxgl[k], Bxl[k]], writes=[BactT])
                        refill()
                        if e_ > 0:
                            for u in range(g8 * 4, g8 * 4 + 4):
                                scatter_unit(e_ - 1, u)
                    if e_ + 1 < NE:
                        build_dispatch(e_ + 1)
                    for n in range(4):
                        wt, wb = next_w()
                        for p in range(2):
                            k = hcnt[0] % 2
                            hcnt[0] += 1

                            def ym(e, wt=wt, p=p, k=k):
                                for fc in range(16):
                                    ins = e.matmul(psY2[k][:], lhsT=actT[:, fc, p * 128:(p + 1) * 128], rhs=wt[:, fc, :], start=(fc == 0), stop=(fc == 15))
                                return ins
                            S.op(PE, ym, reads=[wb, BactT], writes=[BpsY2[k]])
                            S.op(ACT, lambda e, k=k, p=p, n=n: e.activation(out=ysb[:, p, n * 512:(n + 1) * 512], in_=psY2[k][:], func=AF.Copy), reads=[BpsY2[k]], writes=[Bysb])
                        refill()
                        if e_ + 1 < NE:
                            for kq in range(n * 2, n * 2 + 2):
                                gather_unit(kq)
                for u in range(32):
                    scatter_unit(NE - 1, u)
                S.barrier()
            with ExitStack() as ph:
                gT_sb = sb(ph, [NE, T], F32, "gTsb")
                b2s = sb(ph, [NE, D], F32, "b2s")
                Bg_, Bb2 = Buf("gTsb"), Buf("b2s")
                S.dma(SP, b2s[:], b2, ch_misc, writes=[Bb2])
                gtf, Bgtf = bcast_row(ph, mod_d[5:6, :], ch_misc, reads=[B_mod[5]])
                g2, Bg2 = bcast_row(ph, lnp[2:3, :], ch_misc)
                b2r, Bb2r = bcast_row(ph, lnp[3:4, :], ch_misc)
                xin = sb(ph, [128, D], F32, "xin")
                yb = sb(ph, [128, D], F32, "yb")
                st = sb(ph, [128, 8], F32, "st")
                Bxin, Byb, Bst = Buf("xin"), Buf("yb"), Buf("st")
                chx = S.chan("xinG")
                cho = S.chan("outG")
                psGT = ps(ph, [NE, 128])
                psM = ps(ph, [128, 4, 512])
                BpsGT, BpsM = Buf("psGT"), Buf("psM")
                for j in range(8):
                    S.op(PE, lambda e, j=j: e.transpose(out=psGT[:], in_=gate[:, j, :], identity=C("ident")), reads=[Bgate], writes=[BpsGT])
                    S.op(ACT, lambda e, j=j: e.activation(out=gT_sb[:, j * 128:(j + 1) * 128], in_=psGT[:], func=AF.Copy), reads=[BpsGT], writes=[Bg_])
                for j in range(8):
                    tsl = slice(j * 128, (j + 1) * 128)
                    S.dma(SP, xin[:], x1_d[tsl, :], chx, reads=[B_x1d], writes=[Bxin])

                    def bm(e, tsl=tsl):
                        for n in range(4):
                            ins = e.matmul(psM[:, n, :], lhsT=gT_sb[:, tsl], rhs=b2s[:, n * 512:(n + 1) * 512], start=True, stop=True)
                        return ins
                    S.op(PE, bm, reads=[Bg_, Bb2], writes=[BpsM])
                    S.op(DVE, lambda e, j=j: e.tensor_tensor(out=yb[:], in0=psM[:].rearrange("p n c -> p (n c)"), in1=accF[:, j, :], op=ALU.add), reads=[BpsM, BaccF], writes=[Byb])
                    if debug:
                        S.dma(SP, dbg["ffn"][tsl, :], yb[:], cho, reads=[Byb])
                    S.op(DVE, lambda e: e.tensor_tensor(out=yb[:], in0=yb[:], in1=gtf[:], op=ALU.mult), reads=[Byb, Bgtf], writes=[Byb])
                    S.op(DVE, lambda e: e.scalar_tensor_tensor(out=yb[:], in0=xin[:], scalar=ALPHA, in1=yb[:], op0=ALU.mult, op1=ALU.add), reads=[Bxin, Byb], writes=[Byb])
                    ln_tile(yb, xin, st, Byb, Bxin, Bst, g2, b2r, Bg2, Bb2r)
                    S.dma(SP, out[tsl, :], yb[:], cho, reads=[Byb])
                S.barrier()
    return nc


def make_in_maps(inp, stage=99):
    f = lambda a: np.ascontiguousarray(np.asarray(a), dtype=np.float32)
    x = f(inp["x"])
    c = f(inp["c"])
    posn = np.asarray(inp["positions"]).astype(np.int32)
    shared = {
        "w_ada": f(inp["w_ada"][0]), "b_ada": f(inp["b_ada"][0]).reshape(1, -1), "w_in": f(inp["w_in"][0]),
        "hgrn_lb": f(inp["hgrn_lb"]).reshape(1, 2048),
        "gnw": np.ascontiguousarray(f(inp["gnorm_w"][0]).reshape(8, 128).T),
        "w_o": f(inp["w_o"][0]),
        "lnp": np.stack([f(inp["ln1_g"][0]), f(inp["ln1_b"][0]), f(inp["ln2_g"][0]), f(inp["ln2_b"][0])], 0),
        "router_w": np.ascontiguousarray(f(inp["router_w"][0]).reshape(16, 128, NE).transpose(1, 0, 2).reshape(128, 16 * NE)),
        "router_b": f(inp["router_b"][0]).reshape(1, NE),
    }
    if stage >= 5:
        shared["w1"] = f(inp["w1"][0])
        shared["w2"] = f(inp["w2"][0])
        shared["b1"] = np.ascontiguousarray(f(inp["b1"][0]).reshape(NE, 16, 128, 2).transpose(2, 0, 1, 3).reshape(128, NE * 32))
        shared["b2"] = f(inp["b2"][0])
    maps = []
    cc = [make_consts(0), make_consts(1)]
    for core in range(8):
        b, half = core // 2, core % 2
        m = dict(shared)
        m["x_own"] = np.ascontiguousarray(x[b, half * T:(half + 1) * T])
        m["x_ctx"] = np.ascontiguousarray(x[b, 0:T]) if half == 1 else np.zeros((T, D), np.float32)
        m["cT"] = np.ascontiguousarray(c[b].reshape(16, 128).T)
        pp = np.zeros((1, 2048), np.int32)
        pp[0, T:] = posn[b, half * T:(half + 1) * T]
        if half == 1:
            pp[0, :T] = posn[b, 0:T]
        m["pos"] = pp
        m["consts"] = cc[half]
        maps.append(m)
    return maps


_NC_CACHE = {}


def kernel(**inputs):
    if "nc" not in _NC_CACHE:
        _NC_CACHE["nc"] = build_nc()
    nc = _NC_CACHE["nc"]
    maps = make_in_maps(inputs)
    res = run_bass_kernel_spmd(nc, maps, core_ids=list(range(8)))
    outp = np.zeros((4, 2048, D), np.float32)
    for core in range(8):
        b, half = core // 2, core % 2
        outp[b, half * T:(half + 1) * T] = res.results[core]["out"]
    return outp
```

```python
import numpy as np
from contextlib import ExitStack
import concourse.bass as bass
import concourse.mybir as mybir
from concourse.bass_utils import run_bass_kernel_spmd

F32 = mybir.dt.float32
BF16 = mybir.dt.bfloat16
I32 = mybir.dt.int32
AF = mybir.ActivationFunctionType
ALU = mybir.AluOpType
AX = mybir.AxisListType

D = 2048
T = 1024
NE = 32
CAP = 256
ALPHA = 2.0 ** 0.25
PI = float(np.pi)
STRICT = False

_c = {}
_off = 0
for _n, _w in [("ident", 128), ("mcur", 128), ("mprev", 128), ("mprevctx", 128), ("mask3", 64), ("rmat", 128),
               ("ltri", 128), ("chunkind", 2), ("bdmask", 128), ("ustrict", 128), ("ones", 128), ("iota", 256),
               ("iotac", 2), ("invp", 1), ("signp", 1), ("flag", 1)]:
    _c[_n] = (_off, _w)
    _off += _w
NCONST = _off


def make_consts(half):
    c = np.zeros((128, NCONST), np.float32)

    def put(name, arr):
        o, w = _c[name]
        c[:, o:o + w] = arr
    k = np.arange(128)[:, None]
    q = np.arange(128)[None, :]
    flag = 1.0 if half == 1 else 0.0
    put("ident", np.eye(128))
    put("mcur", (k <= q))
    put("mprev", (k >= q))
    put("mprevctx", (k >= q) * flag)
    m3 = (k <= (np.arange(64)[None, :] + 64)).astype(np.float32)
    m3[:64] *= flag
    put("mask3", m3)
    rm = np.zeros((128, 128), np.float32)
    invp = np.zeros((128, 1), np.float32)
    signp = np.zeros((128, 1), np.float32)
    for m in range(128):
        d = m % 64
        if d < 8:
            rm[m + 8, m] = 1.0
            signp[m] = -1.0
        elif d < 16:
            rm[m - 8, m] = 1.0
            signp[m] = 1.0
        if d < 16:
            invp[m] = 500000.0 ** (-(2.0 * (d % 8)) / 16.0)
    put("rmat", rm)
    put("invp", invp)
    put("signp", signp)
    same = (k // 64) == (q // 64)
    put("ltri", (k <= q) & same)
    put("bdmask", (k <= q) & same)
    ci = np.zeros((128, 2), np.float32)
    ci[:64, 0] = 1
    ci[64:, 1] = 1
    put("chunkind", ci)
    put("ustrict", (k < q))
    put("ones", np.ones((128, 128)))
    put("iota", np.tile(np.arange(256, dtype=np.float32)[None], (128, 1)))
    put("iotac", np.stack([np.arange(128), np.arange(128) + 128], 1))
    put("flag", np.full((128, 1), flag))
    return c


class Buf:
    __slots__ = ("name", "w", "r", "chan")

    def __init__(self, name):
        self.name = name
        self.w = None
        self.r = []
        self.chan = None


class Eng:
    def __init__(self, name, h, sem):
        self.name, self.h, self.sem, self.cnt, self.seen = name, h, sem, 0, {}


class Chan:
    def __init__(self, sem):
        self.sem, self.cnt = sem, 0


class Sched:
    def __init__(self, nc, es):
        self.nc, self.es = nc, es
        self.nsem = 0
        self.pe = Eng("pe", nc.tensor, self.sem("pe"))
        self.act = Eng("act", nc.scalar, self.sem("act"))
        self.dve = Eng("dve", nc.vector, self.sem("dve"))
        self.pool = Eng("pool", nc.gpsimd, self.sem("pool"))
        self.sp = Eng("sp", nc.sync, self.sem("sp"))
        self.engs = [self.pe, self.act, self.dve, self.pool, self.sp]
        self.chans = []

    def sem(self, name):
        self.nsem += 1
        return self.es.enter_context(self.nc.semaphore("s_%s_%d" % (name, self.nsem)))

    def chan(self, name="c", real=True):
        c = Chan(self.sem(name))
        self.chans.append(c)
        return c

    def _wait(self, eng, ev):
        if ev is None:
            return
        sem, val = ev
        if eng.seen.get(sem, 0) >= val:
            return
        eng.h.wait_ge(sem, val)
        eng.seen[sem] = val

    def op(self, eng, fn, reads=(), writes=()):
        for b in reads:
            self._wait(eng, b.w)
        for b in writes:
            if b.w is not None and (STRICT or b.w[0] is not eng.sem):
                self._wait(eng, b.w)
            for ev in b.r:
                if STRICT or ev[0] is not eng.sem:
                    self._wait(eng, ev)
        ins = fn(eng.h)
        eng.cnt += 1
        ins.then_inc(eng.sem, 1)
        ev = (eng.sem, eng.cnt)
        for b in writes:
            b.w = ev
            b.r = []
        for b in reads:
            b.r = [e for e in b.r if e[0] is not eng.sem] + [ev]
        return ev

    def dma(self, q, out, in_, chan=None, reads=(), writes=(), cbuf=None):
        cb = cbuf if cbuf is not None else (writes[0] if writes else reads[0])
        if cb.chan is None:
            cb.chan = self.chan(cb.name)
        chan = cb.chan
        for b in reads:
            self._wait(q, b.w)
        for b in writes:
            if b.w is not None and b.w[0] is not chan.sem:
                self._wait(q, b.w)
            for ev in b.r:
                self._wait(q, ev)
        q.h.dma_start(out=out, in_=in_).then_inc(chan.sem, 16)
        chan.cnt += 16
        ev = (chan.sem, chan.cnt)
        for b in writes:
            b.w = ev
            b.r = []
        for b in reads:
            b.r = b.r + [ev]
        return ev

    def barrier(self):
        for e in self.engs:
            for f in self.engs:
                if f is not e and f.cnt > 0:
                    self._wait(e, (f.sem, f.cnt))
            for c in self.chans:
                if c.cnt > 0:
                    self._wait(e, (c.sem, c.cnt))


def build_nc(stage=99, debug=False):
    nc = bass.Bass("TRN2", target_bir_lowering=False)

    def din(name, shape, dt=F32):
        return nc.dram_tensor(name, list(shape), dt, kind="ExternalInput").ap()
    x_own = din("x_own", [T, D])
    x_ctx = din("x_ctx", [T, D])
    cT = din("cT", [128, 16])
    pos = din("pos", [1, 2048], I32)
    w_ada = din("w_ada", [D, 6 * D])
    b_ada = din("b_ada", [1, 6 * D])
    w_in = din("w_in", [D, 7168])
    hgrn_lb = din("hgrn_lb", [1, 2048])
    gnw = din("gnw", [128, 8])
    w_o = din("w_o", [D, D])
    lnp = din("lnp", [4, D])
    router_w = din("router_w", [128, 16 * NE])
    router_b = din("router_b", [1, NE])
    if stage >= 5:
        w1 = din("w1", [NE, D, 2 * D])
        b1 = din("b1", [128, NE * 32])
        w2 = din("w2", [NE, D, D])
        b2 = din("b2", [NE, D])
    consts = din("consts", [128, NCONST])
    out = nc.dram_tensor("out", [T, D], F32, kind="ExternalOutput").ap()
    mod_d = nc.dram_tensor("mod_d", [6, D], F32, kind="Internal").ap()
    x1_d = nc.dram_tensor("x1_d", [T, D], F32, kind="Internal").ap()
    dbg = {}
    if debug:
        dbg["mixT"] = nc.dram_tensor("d_mixT", [128, 16 * T], BF16, kind="ExternalOutput").ap()
        dbg["x1"] = nc.dram_tensor("d_x1", [T, D], F32, kind="ExternalOutput").ap()
        dbg["mod"] = nc.dram_tensor("d_mod", [6, D], F32, kind="ExternalOutput").ap()
        dbg["logits"] = nc.dram_tensor("d_logits", [128, 8 * NE], F32, kind="ExternalOutput").ap()
        dbg["ffn"] = nc.dram_tensor("d_ffn", [T, D], F32, kind="ExternalOutput").ap()

    with ExitStack() as es:
        S = Sched(nc, es)
        PE, ACT, DVE, POOL, SP = S.pe, S.act, S.dve, S.pool, S.sp
        uid = [0]

        def sb(scope, shape, dt, name="t"):
            uid[0] += 1
            return scope.enter_context(nc.sbuf_tensor("%s_%d" % (name, uid[0]), list(shape), dt))

        def ps(scope, shape, dt=F32, name="p"):
            uid[0] += 1
            return scope.enter_context(nc.psum_tensor("%s_%d" % (name, uid[0]), list(shape), dt))

        cst = sb(es, [128, NCONST], F32, "cst")
        cstb = sb(es, [128, NCONST], BF16, "cstb")
        B_cst = Buf("cst")
        ch_misc = S.chan("misc")
        S.dma(SP, cst[:], consts, ch_misc, writes=[B_cst])
        S.op(DVE, lambda e: e.tensor_copy(out=cstb[:], in_=cst[:]), reads=[B_cst], writes=[B_cst])

        def C(name, bf=False, rows=slice(0, 128)):
            o, w = _c[name]
            return (cstb if bf else cst)[rows, o:o + w]

        def make_ring(scope, nslot, ncols):
            ring = [sb(scope, [128, 16, ncols], BF16, "ring") for _ in range(nslot)]
            ring_b = [Buf("ring%d" % i) for i in range(nslot)]
            ring_c = [S.chan("ring") for _ in range(nslot)]
            ring_i = [0]

            def load_w(src2d, nc_):
                i = ring_i[0] % nslot
                ring_i[0] += 1
                S.dma(POOL, ring[i][:, :, 0:nc_], src2d.rearrange("(k p) c -> p k c", p=128), ring_c[i], writes=[ring_b[i]])
                return ring[i], ring_b[i]
            return load_w

        S.barrier()

        B_mod = [Buf("mod%d" % i) for i in range(6)]
        with ExitStack() as ph:
            NSLOT = 3
            load_w = make_ring(ph, NSLOT, 512)
            cts = sb(ph, [128, 16], F32)
            sg = sb(ph, [128, 16], F32)
            silb = sb(ph, [128, 16], BF16)
            brow = [sb(ph, [1, 512], F32) for _ in range(2)]
            mrow = [sb(ph, [1, 512], F32) for _ in range(2)]
            psA = [ps(ph, [1, 512]) for _ in range(2)]
            Bc, Bsil = Buf("c"), Buf("sil")
            Bbrow = [Buf("brow"), Buf("brow")]
            Bmrow = [Buf("mrow"), Buf("mrow")]
            BpsA = [Buf("psA"), Buf("psA")]
            ch_b = [S.chan("brow"), S.chan("brow")]
            ch_m = [S.chan("mrow"), S.chan("mrow")]
            S.dma(SP, cts[:], cT, ch_misc, writes=[Bc])
            S.op(ACT, lambda e: e.activation(out=sg[:], in_=cts[:], func=AF.Sigmoid), reads=[Bc], writes=[Bsil])
            S.op(DVE, lambda e: e.tensor_tensor(out=silb[:], in0=cts[:], in1=sg[:], op=ALU.mult), reads=[Bc, Bsil], writes=[Bsil])
            pend = [load_w(w_ada[:, g * 512:(g + 1) * 512], 512) for g in range(NSLOT)]
            for g in range(24):
                wt, wb = pend.pop(0)
                k = g % 2
                S.dma(SP, brow[k][:], b_ada[0:1, g * 512:(g + 1) * 512], ch_b[k], writes=[Bbrow[k]])

                def mm(e, wt=wt, k=k):
                    for kc in range(16):
                        ins = e.matmul(psA[k][:], lhsT=silb[:, kc:kc + 1], rhs=wt[:, kc, :], start=(kc == 0), stop=(kc == 15))
                    return ins
                S.op(PE, mm, reads=[wb, Bsil], writes=[BpsA[k]])
                addc = 1.0 if (g // 4) in (1, 2, 4, 5) else 0.0
                S.op(DVE, lambda e, k=k, addc=addc: e.scalar_tensor_tensor(out=mrow[k][:], in0=psA[k][:], scalar=addc, in1=brow[k][:], op0=ALU.add, op1=ALU.add),
                     reads=[BpsA[k], Bbrow[k]], writes=[Bmrow[k]])
                S.dma(SP, mod_d[g // 4:g // 4 + 1, (g % 4) * 512:(g % 4 + 1) * 512], mrow[k][:], ch_m[k], reads=[Bmrow[k]], writes=[B_mod[g // 4]], cbuf=Bmrow[k])
                if g + NSLOT < 24:
                    pend.append(load_w(w_ada[:, (g + NSLOT) * 512:(g + NSLOT + 1) * 512], 512))
            S.barrier()
        if debug:
            with ExitStack() as ph:
                t = sb(ph, [6, D], F32)
                Bt = Buf("t")
                S.dma(SP, t[:], mod_d, ch_misc, reads=B_mod, writes=[Bt])
                S.dma(SP, dbg["mod"], t[:], ch_misc, reads=[Bt])
                S.barrier()

        def bcast_row(scope, src_row_ap, chan, reads=()):
            t = sb(scope, [128, D], F32, "row")
            b = Buf("row")
            S.dma(SP, t[:], src_row_ap.partition_broadcast(128), chan, reads=reads, writes=[b])
            return t, b

        gate = sb(es, [128, 8, NE], F32, "gate")
        mfall = sb(es, [128, 8, NE], F32, "mfall")
        mb = sb(es, [128, 8, NE], BF16, "mb")
        posm = sb(es, [128, 8, NE], F32, "posm")
        posmb = sb(es, [128, 8, NE], BF16, "posmb")
        logit = sb(es, [128, 8, NE], F32, "logit")
        Bh2bf, Bgate, Bmf, Bmb, Bposm, Bposmb, Blogit = [Buf(n) for n in ("h2bf", "gate", "mf", "mb", "posm", "posmb", "logit")]
        mixT_scope = ExitStack()
        es.enter_context(mixT_scope)
        mixT = sb(mixT_scope, [128, 16, T], BF16, "mixT")
        B_mix = [Buf("mix%d" % i) for i in range(16)]
        h2bf = mixT[:].rearrange("p (j a) t -> p j (a t)", a=2)

        with ExitStack() as phBC:
            hT = sb(phBC, [128, 16, 2048], BF16, "hT")
            rope_scope = ExitStack()
            phBC.enter_context(rope_scope)
            Ct = sb(rope_scope, [128, 2048], BF16, "Ct")
            St = sb(rope_scope, [128, 2048], BF16, "St")
            with ExitStack() as ph2:
                posi = sb(ph2, [128, 2048], I32)
                t1 = sb(ph2, [128, 2048], F32)
                t2 = sb(ph2, [128, 2048], F32)
                t3 = sb(ph2, [128, 2048], F32)
                Bp, B1, B2, B3, BCt, BSt = [Buf(n) for n in ("posi", "t1", "t2", "t3", "Ct", "St")]
                S.dma(SP, posi[:], pos.partition_broadcast(128), ch_misc, writes=[Bp])
                S.op(DVE, lambda e: e.tensor_copy(out=t1[:], in_=posi[:]), reads=[Bp], writes=[B1])
                S.op(DVE, lambda e: e.tensor_scalar(out=t1[:], in0=t1[:], scalar1=C("invp"), scalar2=None, op0=ALU.mult), reads=[B1], writes=[B1])
                for which, shift in (("sin", 0.0), ("cos", PI / 2)):
                    S.op(DVE, lambda e, shift=shift: e.tensor_scalar(out=t2[:], in0=t1[:], scalar1=shift, scalar2=None, op0=ALU.add), reads=[B1], writes=[B2])
                    S.op(DVE, lambda e: e.tensor_scalar(out=t3[:], in0=t2[:], scalar1=1.0 / (2 * PI), scalar2=None, op0=ALU.mult), reads=[B2], writes=[B3])
                    S.op(DVE, lambda e: e.tensor_copy(out=posi[:], in_=t3[:]), reads=[B3], writes=[Bp])
                    S.op(DVE, lambda e: e.tensor_copy(out=t3[:], in_=posi[:]), reads=[Bp], writes=[B3])
                    S.op(DVE, lambda e: e.scalar_tensor_tensor(out=t3[:], in0=t3[:], scalar=-2 * PI, in1=t2[:], op0=ALU.mult, op1=ALU.add), reads=[B3, B2], writes=[B3])
                    S.op(DVE, lambda e: e.tensor_scalar(out=t3[:], in0=t3[:], scalar1=3.1415925, scalar2=-3.1415925, op0=ALU.min, op1=ALU.max), reads=[B3], writes=[B3])
                    S.op(ACT, lambda e: e.activation(out=t2[:], in_=t3[:], func=AF.Sin), reads=[B3], writes=[B2])
                    if which == "sin":
                        S.op(DVE, lambda e: e.tensor_scalar(out=St[:], in0=t2[:], scalar1=C("signp"), scalar2=None, op0=ALU.mult), reads=[B2], writes=[BSt])
                    else:
                        S.op(DVE, lambda e: e.tensor_copy(out=Ct[:], in_=t2[:]), reads=[B2], writes=[BCt])
                S.barrier()
            with ExitStack() as ph:
                sc1a, Bsc = bcast_row(ph, mod_d[1:2, :], ch_misc, reads=[B_mod[1]])
                sha, Bsh = bcast_row(ph, mod_d[0:1, :], ch_misc, reads=[B_mod[0]])
                xin = [sb(ph, [128, D], F32, "xin") for _ in range(2)]
                Bx = [Buf("xin"), Buf("xin")]
                chx = [S.chan("xin"), S.chan("xin")]
                hb = sb(ph, [128, D], BF16, "hb")
                Bhb = Buf("hb")
                pst = [ps(ph, [128, 8, 128], BF16) for _ in range(2)]
                Bpst = [Buf("pst"), Buf("pst")]
                BhT = Buf("hT")
                for tt in range(16):
                    k = tt % 2
                    src = x_ctx[tt * 128:(tt + 1) * 128, :] if tt < 8 else x_own[(tt - 8) * 128:(tt - 7) * 128, :]
                    S.dma(SP, xin[k][:], src, chx[k], writes=[Bx[k]])
                    S.op(DVE, lambda e, k=k: e.tensor_tensor(out=xin[k][:], in0=xin[k][:], in1=sc1a[:], op=ALU.mult), reads=[Bx[k], Bsc], writes=[Bx[k]])
                    S.op(DVE, lambda e, k=k: e.tensor_tensor(out=hb[:], in0=xin[k][:], in1=sha[:], op=ALU.add), reads=[Bx[k], Bsh], writes=[Bhb])
                    for hh in range(2):
                        def tr(e, hh=hh):
                            for j in range(8):
                                kc = hh * 8 + j
                                ins = e.transpose(out=pst[hh][:, j, :], in_=hb[:, kc * 128:(kc + 1) * 128], identity=C("ident", True))
                            return ins
                        S.op(PE, tr, reads=[Bhb], writes=[Bpst[hh]])
                        S.op(ACT, lambda e, hh=hh, tt=tt: e.activation(out=hT[:, hh * 8:(hh + 1) * 8, tt * 128:(tt + 1) * 128], in_=pst[hh][:], func=AF.Copy),
                             reads=[Bpst[hh]], writes=[BhT])
                S.barrier()
            if stage >= 2:
                with ExitStack() as ph:
                    load_w = make_ring(ph, 3, 256)
                    V = {o: sb(ph, [128, 16, 256], BF16, "V%d" % o) for o in (1, 4, 16)}
                    QT = sb(ph, [128, 2, T], BF16, "QT")
                    KT = sb(ph, [128, 2, 2048], BF16, "KT")
                    qraw = [sb(ph, [128, 512], BF16, "qraw") for _ in range(2)]
                    ta = sb(ph, [128, 512], F32, "ta")
                    tb = sb(ph, [128, 512], F32, "tb")
                    acc = sb(ph, [128, 2, T], F32, "acc")
                    rden = sb(ph, [128, T], F32, "rden")
                    mpair = sb(ph, [128, 3, 256], BF16, "mpair")
                    pe_sb = [sb(ph, [128, 256], BF16, "pe") for _ in range(4)]
                    pm_sb = [sb(ph, [128, 256], BF16, "pm") for _ in range(4)]
                    psP = [ps(ph, [128, 512]) for _ in range(2)]
                    psS = [ps(ph, [128, 512]) for _ in range(2)]
                    psO_ = [ps(ph, [128, 2, 512]) for _ in range(2)]
                    BV = {o: Buf("V") for o in (1, 4, 16)}
                    BQT, BKT, Bta, Btb, Bacc, Brden, Bmp = [Buf(n) for n in ("QT", "KT", "ta", "tb", "acc", "rden", "mp")]
                    Bqraw = [Buf("qraw"), Buf("qraw")]
                    Bpe = [Buf("pe") for _ in range(4)]
                    Bpm = [Buf("pm") for _ in range(4)]
                    BpsP = [Buf("psP"), Buf("psP")]
                    BpsS = [Buf("psS") for _ in range(4)]
                    BpsO_ = [Buf("psO"), Buf("psO")]
                    ocnt = [0]
                    onesb = C("ones", True)
                    S.op(DVE, lambda e: e.tensor_copy(out=mpair[:, 0, 0:128], in_=C("mprev", True)), writes=[Bmp])
                    S.op(DVE, lambda e: e.tensor_copy(out=mpair[:, 0, 128:256], in_=C("mcur", True)), writes=[Bmp])
                    S.op(DVE, lambda e: e.tensor_copy(out=mpair[:, 1, 0:128], in_=C("mprevctx", True)), writes=[Bmp])
                    S.op(DVE, lambda e: e.tensor_copy(out=mpair[:, 1, 128:256], in_=C("mcur", True)), writes=[Bmp])
                    for s4 in range(4):
                        S.op(DVE, lambda e, s4=s4: e.tensor_copy(out=mpair[:, 2, s4 * 64:(s4 + 1) * 64], in_=C("mask3", True)), writes=[Bmp])
                    pcnt = [0]
                    scnt = [0]

                    def proj_a(wt, wb, colsl, tok0, dst_ap, scale, BD):
                        k = pcnt[0] % 2
                        pcnt[0] += 1

                        def mm(e):
                            for kc in range(16):
                                ins = e.matmul(psP[k][:], lhsT=wt[:, kc, colsl], rhs=hT[:, kc, tok0:tok0 + 512], start=(kc == 0), stop=(kc == 15))
                            return ins
                        S.op(PE, mm, reads=[wb], writes=[BpsP[k]])
                        S.op(ACT, lambda e: e.activation(out=qraw[k][:], in_=psP[k][:], func=AF.Identity, scale=scale), reads=[BpsP[k]], writes=[Bqraw[k]])
                        return (k, tok0, dst_ap, BD)

                    def proj_b(st_):
                        k, tok0, dst_ap, BD = st_
                        S.op(PE, lambda e: e.matmul(psP[k][:], lhsT=C("rmat", True), rhs=qraw[k][:], start=True, stop=True), reads=[Bqraw[k]], writes=[BpsP[k]])
                        S.op(DVE, lambda e: e.tensor_tensor(out=ta[:], in0=qraw[k][:], in1=Ct[:, tok0:tok0 + 512], op=ALU.mult), reads=[Bqraw[k]], writes=[Bta])
                        S.op(DVE, lambda e: e.tensor_tensor(out=tb[:], in0=psP[k][:], in1=St[:, tok0:tok0 + 512], op=ALU.mult), reads=[BpsP[k]], writes=[Btb])
                        S.op(DVE, lambda e: e.tensor_tensor(out=dst_ap, in0=ta[:], in1=tb[:], op=ALU.add), reads=[Bta, Btb], writes=[BD])

                    def tokset(o, kt):
                        if o == 1:
                            return slice(kt * 128, (kt + 1) * 128)
                        if o == 4:
                            m, r = kt // 4, kt % 4
                            return slice(512 * m + r, 512 * (m + 1), 4)
                        return slice(kt, 2048, 16)

                    LAG = 2
                    dq = []

                    def defer(fn, is_pv=True):
                        dq.append((fn, is_pv))
                        while sum(1 for _, p in dq if p) > LAG:
                            f_, _p = dq.pop(0)
                            f_()
                        while dq and not dq[0][1]:
                            dq.pop(0)[0]()

                    def flush():
                        while dq:
                            dq.pop(0)[0]()

                    def score_unit(lhs_list, rhs_list, ncol, mask_ap, pv_list, out_col0, ob):
                        k = scnt[0] % 4
                        k2 = scnt[0] % 2
                        scnt[0] += 1
                        n = len(lhs_list)
                        psO = psO_[ob]
                        BpsO = BpsO_[ob]

                        def mm(e):
                            for i in range(n):
                                ins = e.matmul(psS[k2][:, i * ncol:(i + 1) * ncol], lhsT=lhs_list[i], rhs=rhs_list[i], start=True, stop=True)
                            return ins
                        S.op(PE, mm, reads=[BKT, BQT], writes=[BpsS[k2]])
                        S.op(ACT, lambda e: e.activation(out=pe_sb[k][:, 0:n * ncol], in_=psS[k2][:, 0:n * ncol], func=AF.Exp), reads=[BpsS[k2]], writes=[Bpe[k]])
                        S.op(DVE, lambda e: e.tensor_tensor(out=pm_sb[k][:, 0:n * ncol], in0=pe_sb[k][:, 0:n * ncol], in1=mask_ap, op=ALU.mult), reads=[Bpe[k], Bmp], writes=[Bpm[k]])

                        def pv(e):
                            for t_ in range(2):
                                for i in range(n):
                                    vt, oc, st, sp_ = pv_list[i]
                                    lhs = vt if t_ == 0 else onesb
                                    ins = e.matmul(psO[:, t_, out_col0 + oc:out_col0 + oc + ncol], lhsT=lhs, rhs=pm_sb[k][:, i * ncol:(i + 1) * ncol], start=st, stop=sp_)
                            return ins
                        defer(lambda: S.op(PE, pv, reads=[Bpm[k], BV[1], BV[4], BV[16]], writes=[BpsO]))

                    for grp in range(4):
                        c0 = grp * 256
                        wq, wqb = load_w(w_in[:, c0:c0 + 256], 256)
                        wk, wkb = load_w(w_in[:, 1024 + c0:1024 + c0 + 256], 256)
                        wv, wvb = load_w(w_in[:, 2048 + c0:2048 + c0 + 256], 256)
                        plist = []
                        for j in range(2):
                            for nt in range(2):
                                plist.append((wq, wqb, slice(j * 128, (j + 1) * 128), 1024 + nt * 512, QT[:, j, nt * 512:(nt + 1) * 512], 0.125, BQT))
                            for nt in range(4):
                                plist.append((wk, wkb, slice(j * 128, (j + 1) * 128), nt * 512, KT[:, j, nt * 512:(nt + 1) * 512], 1.0, BKT))
                        prev_ = None
                        for pa in plist:
                            cur_ = proj_a(*pa)
                            if prev_ is not None:
                                proj_b(prev_)
                            prev_ = cur_
                        proj_b(prev_)
                        for o in (1, 4, 16):
                            for kt in range(16):
                                k = pcnt[0] % 2
                                pcnt[0] += 1
                                tok = tokset(o, kt)

                                def mm(e, k=k, tok=tok):
                                    for kc in range(16):
                                        ins = e.matmul(psP[k][:, 0:256], lhsT=hT[:, kc, tok], rhs=wv[:, kc, 0:256], start=(kc == 0), stop=(kc == 15))
                                    return ins
                                S.op(PE, mm, reads=[wvb], writes=[BpsP[k]])
                                S.op(ACT, lambda e, k=k, o=o, kt=kt: e.activation(out=V[o][:, kt, :], in_=psP[k][:, 0:256], func=AF.Copy), reads=[BpsP[k]], writes=[BV[o]])
                        for j in range(2):
                            for hh in range(2):
                                pb = 64 * hh
                                KTh = KT[pb:pb + 64, j, :]
                                QTh = QT[pb:pb + 64, j, :]
                                vc = slice(j * 128, (j + 1) * 128)
                                for batch in range(2):
                                    ob = ocnt[0] % 2
                                    ocnt[0] += 1
                                    for bi in range(4):
                                        n = 8 + batch * 4 + bi
                                        qs = slice((n - 8) * 128, (n - 7) * 128)
                                        score_unit([KTh[:, (n - 1) * 128:n * 128], KTh[:, n * 128:(n + 1) * 128]], [QTh[:, qs], QTh[:, qs]], 128,
                                                   mpair[:, 1 if n == 8 else 0, :],
                                                   [(V[1][:, n - 1, vc], 0, True, False), (V[1][:, n, vc], 0, False, True)], bi * 128, ob)
                                    defer(lambda batch=batch, ob=ob: S.op(DVE, lambda e: e.tensor_copy(out=acc[:, :, batch * 512:(batch + 1) * 512], in_=psO_[ob][:]), reads=[BpsO_[ob]], writes=[Bacc]), False)
                                for m in (2, 3):
                                    ob = ocnt[0] % 2
                                    ocnt[0] += 1
                                    for r in range(4):
                                        kp = slice(512 * (m - 1) + r, 512 * m, 4)
                                        kcu = slice(512 * m + r, 512 * (m + 1), 4)
                                        qs = slice(512 * (m - 2) + r, 512 * (m - 1), 4)
                                        score_unit([KTh[:, kp], KTh[:, kcu]], [QTh[:, qs], QTh[:, qs]], 128, mpair[:, 1 if m == 2 else 0, :],
                                                   [(V[4][:, 4 * (m - 1) + r, vc], 0, True, False), (V[4][:, 4 * m + r, vc], 0, False, True)], r * 128, ob)
                                    av = acc[:, :, 512 * (m - 2):512 * (m - 1)].rearrange("p t (i r) -> p t r i", r=4)
                                    defer(lambda av=av, ob=ob: S.op(DVE, lambda e: e.tensor_tensor(out=av, in0=av, in1=psO_[ob][:].rearrange("p t (r i) -> p t r i", r=4), op=ALU.add), reads=[BpsO_[ob], Bacc], writes=[Bacc]), False)
                                for h8 in range(2):
                                    ob = ocnt[0] % 2
                                    ocnt[0] += 1
                                    for rq in range(2):
                                        rs = [h8 * 8 + rq * 4 + s_ for s_ in range(4)]
                                        score_unit([KTh[:, r:2048:16] for r in rs], [QTh[:, r:1024:16] for r in rs], 64, mpair[:, 2, :],
                                                   [(V[16][:, r, vc], i_ * 64, True, True) for i_, r in enumerate(rs)], rq * 256, ob)
                                    av = acc[:, :, :].rearrange("p t (i r) -> p t r i", r=16)[:, :, h8 * 8:(h8 + 1) * 8, :]
                                    defer(lambda av=av, ob=ob: S.op(DVE, lambda e: e.tensor_tensor(out=av, in0=av, in1=psO_[ob][:].rearrange("p t (r i) -> p t r i", r=8), op=ALU.add), reads=[BpsO_[ob], Bacc], writes=[Bacc]), False)
                                flush()
                                ci_ = grp * 2 + j
                                S.op(DVE, lambda e, pb=pb: e.reciprocal(out=rden[pb:pb + 64, :], in_=acc[pb:pb + 64, 1, :]), reads=[Bacc], writes=[Brden])
                                S.op(DVE, lambda e, pb=pb, ci_=ci_: e.tensor_tensor(out=mixT[pb:pb + 64, ci_, :], in0=acc[pb:pb + 64, 0, :], in1=rden[pb:pb + 64, :], op=ALU.mult),
                                     reads=[Bacc, Brden], writes=[B_mix[ci_]])
                    S.barrier()
            rope_scope.close()
            if stage >= 2:
                with ExitStack() as ph:
                    load_w = make_ring(ph, 4, 256)
                    lb = sb(ph, [128, 1024], F32, "lb")
                    oml = sb(ph, [128, 1024], F32, "oml")
                    gnws = sb(ph, [128, 8], F32, "gnws")
                    ke = sb(ph, [128, 16, 256], BF16, "ke")
                    vv = sb(ph, [128, 16, 256], BF16, "vv")
                    keT = sb(ph, [128, 2, 2048], BF16, "keT")
                    qeT = sb(ph, [128, 2, T], BF16, "qeT")
                    gT = sb(ph, [128, 2, T], BF16, "gT")
                    ebl = sb(ph, [128, 2, 16, 2], F32, "ebl")
                    sgm = [sb(ph, [128, 256], F32, "sgm") for _ in range(2)]
                    gtmp = sb(ph, [128, 512], F32, "gtmp")
                    Bgtmp = Buf("gtmp")
                    fv = [sb(ph, [128, 256], F32, "fv") for _ in range(2)]
                    gl = [sb(ph, [128, 256], F32, "gl") for _ in range(2)]
                    kk = [sb(ph, [128, 256], F32, "kk") for _ in range(2)]
                    eb = [sb(ph, [128, 256], F32, "eb") for _ in range(2)]
                    enb = [sb(ph, [128, 256], F32, "enb") for _ in range(2)]
                    qe_t = [sb(ph, [128, 256], BF16, "qe_t") for _ in range(2)]
                    S32 = [sb(ph, [128, 128], F32, "S32") for _ in range(2)]
                    Sb_ = [sb(ph, [128, 128], BF16, "Sb") for _ in range(2)]
                    stmp = [sb(ph, [128, 128], F32, "stmp") for _ in range(2)]
                    aT = [sb(ph, [128, 128], BF16, "aT") for _ in range(2)]
                    sq = [sb(ph, [128, 128], BF16, "sq") for _ in range(2)]
                    rt = [sb(ph, [128, 128], F32, "rt") for _ in range(2)]
                    ot = [sb(ph, [128, 128], F32, "ot") for _ in range(2)]
                    psP = [ps(ph, [128, 512]) for _ in range(2)]
                    psB = ps(ph, [128, 512])
                    psT = ps(ph, [128, 4, 128], BF16)
                    psX = ps(ph, [128, 4, 128])
                    psOT = ps(ph, [128, 2, 128 * 2])
                    psD = [ps(ph, [128, 512]) for _ in range(2)]
                    (Blb, Bke, Bvv, BkeT, BqeT, BgT, Bebl, BpsB, BpsT, Bgnw) = [
                        Buf(n) for n in ("lb", "ke", "vv", "keT", "qeT", "gT", "ebl", "psB", "psT", "gnw")]
                    Bsgm, Bfv, Bgl, Bkk, Beb, Benb, Bqe = [[Buf(n), Buf(n)] for n in ("sgm", "fv", "gl", "kk", "eb", "enb", "qe")]
                    Bsq, Brt, Bot, BpsYd = [[Buf(n), Buf(n)] for n in ("sq", "rt", "ot", "psYd")]
                    _bx, _bo = Buf("psX"), Buf("psOT")
                    BpsXa, BpsXs, BpsYo = [_bx, _bx], [_bx, _bx], [_bo, _bo]
                    BS32 = [Buf("S32"), Buf("S32")]
                    BSb = [Buf("Sb"), Buf("Sb")]
                    Bstmp = [Buf("stmp"), Buf("stmp")]
                    BaT = [Buf("aT"), Buf("aT")]
                    BpsP = [Buf("psP"), Buf("psP")]
                    onesb = C("ones", True)
                    S.dma(SP, gnws[:], gnw, ch_misc, writes=[Bgnw])
                    S.dma(SP, lb[:], hgrn_lb[0:1, 0:1024].partition_broadcast(128), ch_misc, writes=[Blb])
                    S.dma(SP, oml[:], hgrn_lb[0:1, 1024:2048].partition_broadcast(128), ch_misc, writes=[Blb])
                    S.op(DVE, lambda e: e.tensor_tensor(out=oml[:], in0=lb[:], in1=oml[:], op=ALU.subtract), reads=[Blb], writes=[Blb])
                    S.op(ACT, lambda e: e.activation(out=lb[:], in_=oml[:], func=AF.Sigmoid), reads=[Blb], writes=[Blb])
                    S.op(DVE, lambda e: e.tensor_scalar(out=oml[:], in0=lb[:], scalar1=-1.0, scalar2=1.0, op0=ALU.mult, op1=ALU.add), reads=[Blb], writes=[Blb])
                    pcnt = [0]

                    def proj_tm(wt, wb, tt):
                        k = pcnt[0] % 2
                        pcnt[0] += 1

                        def mm(e):
                            for kc in range(16):
                                ins = e.matmul(psP[k][:, 0:256], lhsT=hT[:, kc, tt * 128:(tt + 1) * 128], rhs=wt[:, kc, 0:256], start=(kc == 0), stop=(kc == 15))
                            return ins
                        S.op(PE, mm, reads=[wb], writes=[BpsP[k]])
                        return k

                    for hp in range(4):
                        wqr, wqrb = load_w(w_in[:, 3072 + hp * 256:3072 + (hp + 1) * 256], 256)
                        wf, wfb = load_w(w_in[:, 4096 + hp * 256:4096 + (hp + 1) * 256], 256)
                        wi, wib = load_w(w_in[:, 5120 + hp * 256:5120 + (hp + 1) * 256], 256)
                        wg, wgb = load_w(w_in[:, 6144 + hp * 256:6144 + (hp + 1) * 256], 256)
                        lsl = slice(hp * 256, (hp + 1) * 256)
                        for hd in range(2):
                            for nt in range(2):
                                k = pcnt[0] % 2
                                pcnt[0] += 1

                                def mm(e, k=k, hd=hd, nt=nt):
                                    for kc in range(16):
                                        ins = e.matmul(psP[k][:], lhsT=wg[:, kc, hd * 128:(hd + 1) * 128], rhs=hT[:, kc, 1024 + nt * 512:1024 + (nt + 1) * 512], start=(kc == 0), stop=(kc == 15))
                                    return ins
                                S.op(PE, mm, reads=[wgb], writes=[BpsP[k]])
                                S.op(ACT, lambda e, k=k: e.activation(out=gtmp[:], in_=psP[k][:], func=AF.Exp, scale=-1.0), reads=[BpsP[k]], writes=[Bgtmp])
                                S.op(DVE, lambda e: e.tensor_scalar(out=gtmp[:], in0=gtmp[:], scalar1=1.0, scalar2=None, op0=ALU.add), reads=[Bgtmp], writes=[Bgtmp])
                                S.op(DVE, lambda e: e.reciprocal(out=gtmp[:], in_=gtmp[:]), reads=[Bgtmp], writes=[Bgtmp])
                                S.op(DVE, lambda e, k=k, hd=hd, nt=nt: e.tensor_tensor(out=gT[:, hd, nt * 512:(nt + 1) * 512], in0=psP[k][:], in1=gtmp[:], op=ALU.mult), reads=[BpsP[k], Bgtmp], writes=[BgT])
                        def st1(tt):
                            b = tt % 2
                            k = proj_tm(wf, wfb, tt)
                            S.op(ACT, lambda e: e.activation(out=sgm[b][:], in_=psP[k][:, 0:256], func=AF.Exp, scale=-1.0), reads=[BpsP[k]], writes=[Bsgm[b]])
                            S.op(DVE, lambda e: e.tensor_scalar(out=sgm[b][:], in0=sgm[b][:], scalar1=1.0, scalar2=None, op0=ALU.add), reads=[Bsgm[b]], writes=[Bsgm[b]])
                            S.op(DVE, lambda e: e.reciprocal(out=sgm[b][:], in_=sgm[b][:]), reads=[Bsgm[b]], writes=[Bsgm[b]])
                            S.op(DVE, lambda e: e.tensor_tensor(out=fv[b][:], in0=sgm[b][:], in1=oml[:, lsl], op=ALU.mult), reads=[Bsgm[b], Blb], writes=[Bfv[b]])
                            S.op(DVE, lambda e: e.tensor_tensor(out=fv[b][:], in0=fv[b][:], in1=lb[:, lsl], op=ALU.add), reads=[Bfv[b], Blb], writes=[Bfv[b]])
                            S.op(ACT, lambda e: e.activation(out=gl[b][:], in_=fv[b][:], func=AF.Ln), reads=[Bfv[b]], writes=[Bgl[b]])
                            S.op(DVE, lambda e: e.tensor_scalar(out=kk[b][:], in0=fv[b][:], scalar1=-1.0, scalar2=1.0, op0=ALU.mult, op1=ALU.add), reads=[Bfv[b]], writes=[Bkk[b]])

                        def st2(tt):
                            own = tt >= 8
                            k = proj_tm(wi, wib, tt)
                            S.op(ACT, lambda e: e.activation(out=vv[:, tt, :], in_=psP[k][:, 0:256], func=AF.Copy), reads=[BpsP[k]], writes=[Bvv])
                            if own:
                                return proj_tm(wqr, wqrb, tt)
                            return None

                        def st3(tt, kq):
                            own = tt >= 8
                            b = tt % 2

                            def cs(e):
                                e.matmul(psB[:, 0:256], lhsT=C("ltri"), rhs=gl[b][:], start=True, stop=True)
                                for hd in range(2):
                                    ins = e.matmul(psB[:, 256 + 2 * hd:258 + 2 * hd], lhsT=gl[b][:, hd * 128:(hd + 1) * 128], rhs=C("chunkind"), start=True, stop=True)
                                return ins
                            S.op(PE, cs, reads=[Bgl[b]], writes=[BpsB])
                            S.op(ACT, lambda e: e.activation(out=enb[b][:], in_=psB[:, 0:256], func=AF.Exp, scale=-1.0), reads=[BpsB], writes=[Benb[b]])
                            if own:
                                S.op(ACT, lambda e: e.activation(out=eb[b][:], in_=psB[:, 0:256], func=AF.Exp), reads=[BpsB], writes=[Beb[b]])
                            S.op(ACT, lambda e: e.activation(out=ebl[:, :, tt, :], in_=psB[:, 256:260].rearrange("p (h c) -> p h c", c=2), func=AF.Exp), reads=[BpsB], writes=[Bebl])
                            S.op(DVE, lambda e: e.tensor_tensor(out=ke[:, tt, :], in0=kk[b][:], in1=enb[b][:], op=ALU.mult), reads=[Bkk[b], Benb[b]], writes=[Bke])
                            if own:
                                S.op(DVE, lambda e: e.tensor_tensor(out=qe_t[b][:], in0=psP[kq][:, 0:256], in1=eb[b][:], op=ALU.mult), reads=[BpsP[kq], Beb[b]], writes=[Bqe[b]])

                            def tr(e):
                                for hd in range(2):
                                    ins = e.transpose(out=psT[:, hd, :], in_=ke[:, tt, hd * 128:(hd + 1) * 128], identity=C("ident", True))
                                if own:
                                    for hd in range(2):
                                        ins = e.transpose(out=psT[:, 2 + hd, :], in_=qe_t[b][:, hd * 128:(hd + 1) * 128], identity=C("ident", True))
                                return ins
                            S.op(PE, tr, reads=[Bke] + ([Bqe[b]] if own else []), writes=[BpsT])
                            S.op(ACT, lambda e: e.activation(out=keT[:, :, tt * 128:(tt + 1) * 128], in_=psT[:, 0:2, :], func=AF.Copy), reads=[BpsT], writes=[BkeT])
                            if own:
                                S.op(ACT, lambda e: e.activation(out=qeT[:, :, (tt - 8) * 128:(tt - 7) * 128], in_=psT[:, 2:4, :], func=AF.Copy), reads=[BpsT], writes=[BqeT])

                        st1(0)
                        for tt in range(16):
                            if tt + 1 < 16:
                                st1(tt + 1)
                            kq_ = st2(tt)
                            st3(tt, kq_)
                        for hd in range(2):
                            S.op(DVE, lambda e, hd=hd: e.memset(S32[hd][:], 0.0), writes=[BS32[hd]])
                            S.op(ACT, lambda e, hd=hd: e.activation(out=Sb_[hd][:], in_=S32[hd][:], func=AF.Copy), reads=[BS32[hd]], writes=[BSb[hd]])
                        for tt in range(16):
                            own = tt >= 8
                            qsl = slice((tt - 8) * 128, (tt - 7) * 128)
                            if own:
                                for hd in range(2):
                                    S.op(PE, lambda e, hd=hd, tt=tt, qsl=qsl: e.matmul(psX[:, hd, :], lhsT=keT[:, hd, tt * 128:(tt + 1) * 128], rhs=qeT[:, hd, qsl], start=True, stop=True),
                                         reads=[BkeT, BqeT], writes=[BpsXa[hd]])
                                    S.op(DVE, lambda e, hd=hd: e.tensor_tensor(out=aT[hd][:], in0=psX[:, hd, :], in1=C("bdmask", True), op=ALU.mult), reads=[BpsXa[hd]], writes=[BaT[hd]])
                            for c in range(2):
                                rows = slice(c * 64, (c + 1) * 64)
                                for hd in range(2):
                                    hsl = slice(hd * 128, (hd + 1) * 128)
                                    if own:
                                        def om(e, hd=hd, tt=tt, c=c, rows=rows, hsl=hsl):
                                            e.matmul(psOT[:, hd, c * 64:(c + 1) * 64], lhsT=vv[rows, tt, hsl], rhs=aT[hd][rows, c * 64:(c + 1) * 64], start=True, stop=False)
                                            return e.matmul(psOT[:, hd, c * 64:(c + 1) * 64], lhsT=Sb_[hd][:], rhs=qeT[:, hd, (tt - 8) * 128 + c * 64:(tt - 8) * 128 + (c + 1) * 64], start=False, stop=True)
                                        S.op(PE, om, reads=[Bvv, BaT[hd], BSb[hd], BqeT], writes=[BpsYo[hd]])
                                    S.op(PE, lambda e, tt=tt, rows=rows, hsl=hsl, hd=hd: e.matmul(psD[hd][:, 0:128], lhsT=ke[rows, tt, hsl], rhs=vv[rows, tt, hsl], start=True, stop=True),
                                         reads=[Bke, Bvv], writes=[BpsYd[hd]])
                                    S.op(DVE, lambda e, hd=hd: e.tensor_tensor(out=stmp[hd][:], in0=psD[hd][:, 0:128], in1=S32[hd][:], op=ALU.add), reads=[BpsYd[hd], BS32[hd]], writes=[Bstmp[hd]])
                                    if tt == 7 and c == 1:
                                        S.op(DVE, lambda e, hd=hd: e.tensor_scalar(out=stmp[hd][:], in0=stmp[hd][:], scalar1=C("flag"), scalar2=None, op0=ALU.mult), reads=[Bstmp[hd]], writes=[Bstmp[hd]])
                                    S.op(ACT, lambda e, hd=hd, tt=tt, c=c: e.activation(out=Sb_[hd][:], in_=stmp[hd][:], func=AF.Identity, scale=ebl[:, hd, tt, c:c + 1]), reads=[Bstmp[hd], Bebl], writes=[BSb[hd]])
                                    S.op(DVE, lambda e, hd=hd, tt=tt, c=c: e.tensor_scalar(out=S32[hd][:], in0=stmp[hd][:], scalar1=ebl[:, hd, tt, c:c + 1], scalar2=None, op0=ALU.mult),
                                         reads=[Bstmp[hd], Bebl], writes=[BS32[hd]])
                            if own:
                                for hd in range(2):
                                    ci_ = 8 + hp * 2 + hd
                                    S.op(ACT, lambda e, hd=hd: e.activation(out=sq[hd][:], in_=psOT[:, hd, 0:128], func=AF.Square), reads=[BpsYo[hd]], writes=[Bsq[hd]])
                                    S.op(PE, lambda e, hd=hd: e.matmul(psX[:, 2 + hd, :], lhsT=onesb, rhs=sq[hd][:], start=True, stop=True), reads=[Bsq[hd]], writes=[BpsXs[hd]])
                                    S.op(ACT, lambda e, hd=hd: e.activation(out=rt[hd][:], in_=psX[:, 2 + hd, :], func=AF.Ln, scale=1.0 / 128.0, bias=1e-6), reads=[BpsXs[hd]], writes=[Brt[hd]])
                                    S.op(ACT, lambda e, hd=hd: e.activation(out=rt[hd][:], in_=rt[hd][:], func=AF.Exp, scale=-0.5), reads=[Brt[hd]], writes=[Brt[hd]])
                                    S.op(DVE, lambda e, hd=hd: e.tensor_tensor(out=ot[hd][:], in0=psOT[:, hd, 0:128], in1=rt[hd][:], op=ALU.mult), reads=[BpsYo[hd], Brt[hd]], writes=[Bot[hd]])
                                    S.op(DVE, lambda e, ci_=ci_, qsl=qsl, hd=hd, hp=hp: e.scalar_tensor_tensor(out=mixT[:, ci_, qsl], in0=ot[hd][:], scalar=gnws[:, hp * 2 + hd:hp * 2 + hd + 1], in1=gT[:, hd, qsl], op0=ALU.mult, op1=ALU.mult),
                                         reads=[Bot[hd], BgT, Bgnw], writes=[B_mix[ci_]])
                    S.barrier()
        if debug:
            S.dma(SP, dbg["mixT"], mixT[:].rearrange("p c t -> p (c t)"), ch_misc, reads=B_mix)
            S.barrier()
        if stage >= 3:
            def ln_tile(yb, junk, st, Byb, Bjunk, Bst, grow, brow_, Bg, Bb):
                S.op(DVE, lambda e: e.memset(st[:], 0.0), writes=[Bst])
                S.op(DVE, lambda e: e.reduce_sum(out=st[:, 0:1], in_=yb[:], axis=AX.X), reads=[Byb], writes=[Bst])
                S.op(DVE, lambda e: e.tensor_scalar(out=st[:, 1:2], in0=st[:, 0:1], scalar1=-1.0 / D, scalar2=None, op0=ALU.mult), reads=[Bst], writes=[Bst])
                S.op(DVE, lambda e: e.tensor_scalar(out=yb[:], in0=yb[:], scalar1=st[:, 1:2], scalar2=None, op0=ALU.add), reads=[Byb, Bst], writes=[Byb])
                S.op(ACT, lambda e: e.activation(out=junk[:], in_=yb[:], func=AF.Square, accum_out=st[:, 2:3]), reads=[Byb, Bst], writes=[Bjunk, Bst])
                S.op(ACT, lambda e: e.activation(out=st[:, 3:4], in_=st[:, 2:3], func=AF.Sqrt, scale=1.0 / D, bias=1e-5), reads=[Bst], writes=[Bst])
                S.op(DVE, lambda e: e.reciprocal(out=st[:, 4:5], in_=st[:, 3:4]), reads=[Bst], writes=[Bst])
                S.op(DVE, lambda e: e.scalar_tensor_tensor(out=yb[:], in0=yb[:], scalar=st[:, 4:5], in1=grow[:], op0=ALU.mult, op1=ALU.mult), reads=[Byb, Bst, Bg], writes=[Byb])
                S.op(DVE, lambda e: e.tensor_tensor(out=yb[:], in0=yb[:], in1=brow_[:], op=ALU.add), reads=[Byb, Bb], writes=[Byb])

            B_x1d = Buf("x1d")
            with ExitStack() as ph:
                wo = sb(ph, [128, 16, D], BF16, "wo")
                Bwo = Buf("wo")
                ch_wo = S.chan("wo")
                for n in range(4):
                    S.dma(POOL, wo[:, :, n * 512:(n + 1) * 512], w_o[:, n * 512:(n + 1) * 512].rearrange("(k p) c -> p k c", p=128), ch_wo, writes=[Bwo])
                gt1, Bgt1 = bcast_row(ph, mod_d[2:3, :], ch_misc, reads=[B_mod[2]])
                g1, Bg1 = bcast_row(ph, lnp[0:1, :], ch_misc)
                b1r, Bb1r = bcast_row(ph, lnp[1:2, :], ch_misc)
                scf, Bscf = bcast_row(ph, mod_d[4:5, :], ch_misc, reads=[B_mod[4]])
                shf, Bshf = bcast_row(ph, mod_d[3:4, :], ch_misc, reads=[B_mod[3]])
                rw = sb(ph, [128, 16, NE], F32, "rw")
                rb = sb(ph, [128, NE], F32, "rb")
                Brw = Buf("rw")
                S.dma(SP, rw[:], router_w.rearrange("p (k e) -> p k e", e=NE), ch_misc, writes=[Brw])
                S.dma(SP, rb[:], router_b.partition_broadcast(128), ch_misc, writes=[Brw])
                xin = sb(ph, [128, D], F32, "xin")
                yb = sb(ph, [128, D], F32, "yb")
                h2T = sb(ph, [128, 16, 128], F32, "h2T")
                st = sb(ph, [128, 8], F32, "st")
                t8 = sb(ph, [128, 8], F32, "t8")
                ex = sb(ph, [128, NE], F32, "ex")
                sm = sb(ph, [128, 4], F32, "sm")
                Bxin, Byb, Bh2T, Bst, Bt8, Bex, Bsm = [Buf(n) for n in ("xin", "yb", "h2T", "st", "t8", "ex", "sm")]
                chx = S.chan("xinD")
                chy = S.chan("x1st")
                psM = ps(ph, [128, 4, 512])
                psT4 = ps(ph, [128, 4, 128])
                psL = ps(ph, [128, NE])
                BpsM, BpsT4, BpsL = Buf("psM"), Buf("psT4"), Buf("psL")
                for j in range(8):
                    tsl = slice(j * 128, (j + 1) * 128)
                    S.dma(SP, xin[:], x_own[tsl, :], chx, writes=[Bxin])

                    def mm(e, tsl=tsl):
                        for n in range(4):
                            for kc in range(16):
                                ins = e.matmul(psM[:, n, :], lhsT=mixT[:, kc, tsl], rhs=wo[:, kc, n * 512:(n + 1) * 512], start=(kc == 0), stop=(kc == 15))
                        return ins
                    S.op(PE, mm, reads=[Bwo] + B_mix, writes=[BpsM])
                    S.op(DVE, lambda e: e.tensor_tensor(out=yb[:], in0=psM[:].rearrange("p n c -> p (n c)"), in1=gt1[:], op=ALU.mult), reads=[BpsM, Bgt1], writes=[Byb])
                    S.op(DVE, lambda e: e.scalar_tensor_tensor(out=yb[:], in0=xin[:], scalar=ALPHA, in1=yb[:], op0=ALU.mult, op1=ALU.add), reads=[Bxin, Byb], writes=[Byb])
                    ln_tile(yb, xin, st, Byb, Bxin, Bst, g1, b1r, Bg1, Bb1r)
                    S.dma(SP, x1_d[tsl, :], yb[:], chy, reads=[Byb], writes=[B_x1d], cbuf=Byb)
                    if debug:
                        S.dma(SP, dbg["x1"][tsl, :], yb[:], chy, reads=[Byb])
                S.barrier()
                for j in range(8):
                    tsl = slice(j * 128, (j + 1) * 128)
                    S.dma(SP, yb[:], x1_d[tsl, :], chx, reads=[B_x1d], writes=[Byb])
                    S.op(DVE, lambda e: e.tensor_tensor(out=xin[:], in0=yb[:], in1=scf[:], op=ALU.mult), reads=[Byb, Bscf], writes=[Bxin])
                    S.op(DVE, lambda e: e.tensor_tensor(out=xin[:], in0=xin[:], in1=shf[:], op=ALU.add), reads=[Bxin, Bshf], writes=[Bxin])
                    S.op(ACT, lambda e, j=j: e.activation(out=h2bf[:, j, :], in_=xin[:], func=AF.Copy), reads=[Bxin], writes=[Bh2bf])
                    for q4 in range(4):
                        def tr(e, q4=q4):
                            for i in range(4):
                                kc = q4 * 4 + i
                                ins = e.transpose(out=psT4[:, i, :], in_=xin[:, kc * 128:(kc + 1) * 128], identity=C("ident"))
                            return ins
                        S.op(PE, tr, reads=[Bxin], writes=[BpsT4])
                        S.op(ACT, lambda e, q4=q4: e.activation(out=h2T[:, q4 * 4:(q4 + 1) * 4, :], in_=psT4[:], func=AF.Copy), reads=[BpsT4], writes=[Bh2T])

                    def lg(e):
                        for kc in range(16):
                            ins = e.matmul(psL[:], lhsT=h2T[:, kc, :], rhs=rw[:, kc, :], start=(kc == 0), stop=(kc == 15))
                        return ins
                    S.op(PE, lg, reads=[Bh2T, Brw], writes=[BpsL])
                    S.op(DVE, lambda e, j=j: e.tensor_tensor(out=logit[:, j, :], in0=psL[:], in1=rb[:], op=ALU.add), reads=[BpsL, Brw], writes=[Blogit])
                    S.op(DVE, lambda e, j=j: e.max(out=t8[:], in_=logit[:, j, :]), reads=[Blogit], writes=[Bt8])
                    S.op(DVE, lambda e, j=j: e.tensor_scalar(out=mfall[:, j, :], in0=logit[:, j, :], scalar1=t8[:, 3:4], scalar2=None, op0=ALU.is_ge), reads=[Blogit, Bt8], writes=[Bmf])
                    S.op(DVE, lambda e: e.tensor_scalar(out=sm[:, 0:1], in0=t8[:, 0:1], scalar1=-1.0, scalar2=None, op0=ALU.mult), reads=[Bt8], writes=[Bsm])
                    S.op(ACT, lambda e, j=j: e.activation(out=ex[:], in_=logit[:, j, :], func=AF.Exp, bias=sm[:, 0:1], scale=1.0), reads=[Blogit, Bsm], writes=[Bex])
                    S.op(DVE, lambda e, j=j: e.tensor_tensor(out=ex[:], in0=ex[:], in1=mfall[:, j, :], op=ALU.mult), reads=[Bex, Bmf], writes=[Bex])
                    S.op(DVE, lambda e: e.reduce_sum(out=sm[:, 1:2], in_=ex[:], axis=AX.X), reads=[Bex], writes=[Bsm])
                    S.op(DVE, lambda e: e.reciprocal(out=sm[:, 2:3], in_=sm[:, 1:2]), reads=[Bsm], writes=[Bsm])
                    S.op(DVE, lambda e, j=j: e.tensor_scalar(out=gate[:, j, :], in0=ex[:], scalar1=sm[:, 2:3], scalar2=None, op0=ALU.mult), reads=[Bex, Bsm], writes=[Bgate])
                    S.op(DVE, lambda e, j=j: e.tensor_copy(out=mb[:, j, :], in_=mfall[:, j, :]), reads=[Bmf], writes=[Bmb])
                for j in range(8):
                    def pc(e, j=j):
                        for i in range(j):
                            e.matmul(psL[:], lhsT=C("ones", True), rhs=mb[:, i, :], start=(i == 0), stop=False)
                        return e.matmul(psL[:], lhsT=C("ustrict", True), rhs=mb[:, j, :], start=(j == 0), stop=True)
                    S.op(PE, pc, reads=[Bmb], writes=[BpsL])
                    S.op(DVE, lambda e, j=j: e.scalar_tensor_tensor(out=posm[:, j, :], in0=psL[:], scalar=1.0, in1=mfall[:, j, :], op0=ALU.add, op1=ALU.mult), reads=[BpsL, Bmf], writes=[Bposm])
                    S.op(DVE, lambda e, j=j: e.tensor_scalar(out=posm[:, j, :], in0=posm[:, j, :], scalar1=-1.0, scalar2=float(CAP), op0=ALU.add, op1=ALU.min), reads=[Bposm], writes=[Bposm])
                    S.op(DVE, lambda e, j=j: e.tensor_copy(out=posmb[:, j, :], in_=posm[:, j, :]), reads=[Bposm], writes=[Bposmb])
                if debug:
                    S.dma(SP, dbg["logits"], logit[:].rearrange("p j e -> p (j e)"), ch_misc, reads=[Blogit])
                S.barrier()
        if stage >= 5:
            accF_scope = ExitStack()
            es.enter_context(accF_scope)
            accF = sb(accF_scope, [128, 8, D], F32, "accF")
            BaccF = Buf("accF")
            S.op(DVE, lambda e: e.memset(accF[:], 0.0), writes=[BaccF])
            with ExitStack() as ph:
                NSLOT = 3
                load_w = make_ring(ph, NSLOT, 512)
                b1s = sb(ph, [128, NE * 32], F32, "b1s")
                Bb1 = Buf("b1s")
                S.dma(SP, b1s[:], b1, ch_misc, writes=[Bb1])
                Pe = sb(ph, [128, 8, CAP], BF16, "Pe")
                PT = [sb(ph, [128, 2, T], BF16, "PT") for _ in range(2)]
                xg = sb(ph, [128, 16, CAP], BF16, "xg")
                actT = sb(ph, [128, 16, CAP], BF16, "actT")
                ysb = sb(ph, [128, 2, D], BF16, "ysb")
                xgl = [sb(ph, [128, CAP], F32, "xgl") for _ in range(2)]
                sgd = [sb(ph, [128, CAP], F32, "sgd") for _ in range(2)]
                xl = [sb(ph, [128, CAP], F32, "xl") for _ in range(2)]
                BPe, Bxg, BactT, Bysb = [Buf(n) for n in ("Pe", "xg", "actT", "ysb")]
                BPT = [Buf("PT"), Buf("PT")]
                Bxgl = [Buf("xgl"), Buf("xgl")]
                Bsgd = [Buf("sgd"), Buf("sgd")]
                Bxl = [Buf("xl"), Buf("xl")]
                psPT = ps(ph, [128, 512])
                psG = ps(ph, [128, 2, CAP])
                psH = [ps(ph, [128, 2, CAP]) for _ in range(2)]
                psY2 = [ps(ph, [128, 512]) for _ in range(2)]
                psSc = [ps(ph, [128, 512]) for _ in range(2)]
                BpsPT, BpsG = Buf("psPT"), Buf("psG")
                BpsH = [Buf("psH"), Buf("psH")]
                BpsY2 = [Buf("psY2"), Buf("psY2")]
                BpsSc = [Buf("psSc"), Buf("psSc")]
                items = []
                for e_ in range(NE):
                    for g8 in range(8):
                        items.append(("w1", e_, g8))
                    for n in range(4):
                        items.append(("w2", e_, n))

                def issue(it):
                    kind, e_, g = it
                    if kind == "w1":
                        return load_w(w1[e_, :, g * 512:(g + 1) * 512], 512)
                    return load_w(w2[e_, :, g * 512:(g + 1) * 512], 512)
                pend = [issue(items[i]) for i in range(NSLOT)]
                nxt = [NSLOT]

                def next_w():
                    return pend.pop(0)

                def refill():
                    if nxt[0] < len(items):
                        pend.append(issue(items[nxt[0]]))
                        nxt[0] += 1
                hcnt = [0]
                ccnt = [0]

                def build_dispatch(e_):
                    for j in range(8):
                        S.op(DVE, lambda e, j=j: e.tensor_scalar(out=Pe[:, j, :], in0=C("iota")[:, 0:CAP], scalar1=posm[:, j, e_:e_ + 1], scalar2=None, op0=ALU.is_equal),
                             reads=[Bposm], writes=[BPe])
                    pt = PT[e_ % 2]
                    for hf_ in range(2):
                        def ptm(e, hf_=hf_):
                            for j4 in range(4):
                                j = hf_ * 4 + j4
                                ins = e.matmul(psPT[:, j4 * 128:(j4 + 1) * 128], lhsT=posmb[:, j, e_:e_ + 1].to_broadcast([128, 128]), rhs=C("ident", True), start=True, stop=True)
                            return ins
                        S.op(PE, ptm, reads=[Bposmb], writes=[BpsPT])
                        for p in range(2):
                            S.op(DVE, lambda e, p=p, hf_=hf_: e.tensor_scalar(out=pt[:, p, hf_ * 512:(hf_ + 1) * 512], in0=psPT[:], scalar1=C("iotac")[:, p:p + 1], scalar2=None, op0=ALU.is_equal),
                                 reads=[BpsPT], writes=[BPT[e_ % 2]])

                def gather_unit(kq):
                    def gm(e):
                        for k2 in range(2):
                            kc = kq * 2 + k2
                            for j in range(8):
                                ins = e.matmul(psG[:, k2, :], lhsT=h2bf[:, j, kc * 128:(kc + 1) * 128], rhs=Pe[:, j, :], start=(j == 0), stop=(j == 7))
                        return ins
                    S.op(PE, gm, reads=[Bh2bf, BPe], writes=[BpsG])
                    S.op(ACT, lambda e: e.activation(out=xg[:, kq * 2:(kq + 1) * 2, :], in_=psG[:], func=AF.Copy), reads=[BpsG], writes=[Bxg])

                def scatter_unit(e_, u):
                    j, n = u // 4, u % 4
                    k = ccnt[0] % 2
                    ccnt[0] += 1
                    pt = PT[e_ % 2]

                    def sm_(e):
                        for p in range(2):
                            ins = e.matmul(psSc[k][:], lhsT=pt[:, p, j * 128:(j + 1) * 128], rhs=ysb[:, p, n * 512:(n + 1) * 512], start=(p == 0), stop=(p == 1))
                        return ins
                    S.op(PE, sm_, reads=[BPT[e_ % 2], Bysb], writes=[BpsSc[k]])
                    av = accF[:, j, n * 512:(n + 1) * 512]
                    S.op(DVE, lambda e: e.scalar_tensor_tensor(out=av, in0=psSc[k][:], scalar=gate[:, j, e_:e_ + 1], in1=av, op0=ALU.mult, op1=ALU.add),
                         reads=[BpsSc[k], Bgate, BaccF], writes=[BaccF])

                build_dispatch(0)
                for kq in range(8):
                    gather_unit(kq)
                for e_ in range(NE):
                    for g8 in range(8):
                        wt, wb = next_w()
                        for f2 in range(2):
                            fc = g8 * 2 + f2
                            k = hcnt[0] % 2
                            hcnt[0] += 1

                            def hm(e, wt=wt, f2=f2, k=k):
                                for t_ in range(2):
                                    for kc in range(16):
                                        ins = e.matmul(psH[k][:, t_, :], lhsT=wt[:, kc, f2 * 256 + t_:(f2 + 1) * 256:2], rhs=xg[:, kc, :], start=(kc == 0), stop=(kc == 15))
                                return ins
                            S.op(PE, hm, reads=[wb, Bxg], writes=[BpsH[k]])
                            bi = e_ * 32 + fc * 2
                            S.op(DVE, lambda e, k=k, bi=bi: e.tensor_scalar(out=xgl[k][:], in0=psH[k][:, 0, :], scalar1=b1s[:, bi:bi + 1], scalar2=7.0, op0=ALU.add, op1=ALU.min), reads=[BpsH[k], Bb1], writes=[Bxgl[k]])
                            S.op(ACT, lambda e, k=k: e.activation(out=sgd[k][:], in_=xgl[k][:], func=AF.Sigmoid, scale=1.702), reads=[Bxgl[k]], writes=[Bsgd[k]])
                            S.op(DVE, lambda e, k=k, bi=bi: e.tensor_scalar(out=xl[k][:], in0=psH[k][:, 1, :], scalar1=b1s[:, bi + 1:bi + 2], scalar2=7.0, op0=ALU.add, op1=ALU.min), reads=[BpsH[k], Bb1], writes=[Bxl[k]])
                            S.op(DVE, lambda e, k=k: e.tensor_scalar(out=xl[k][:], in0=xl[k][:], scalar1=-7.0, scalar2=1.0, op0=ALU.max, op1=ALU.add), reads=[Bxl[k]], writes=[Bxl[k]])
                            S.op(DVE, lambda e, k=k: e.tensor_tensor(out=xgl[k][:], in0=xgl[k][:], in1=sgd[k][:], op=ALU.mult), reads=[Bxgl[k], Bsgd[k]], writes=[Bxgl[k]])
                            S.op(DVE, lambda e, fc=fc, k=k: e.tensor_tensor(out=actT[:, fc, :], in0=xgl[k][:], in1=xl[k][:], op=ALU.mult), reads=[Bxgl[k], Bxl[k]], writes=[BactT])
                        refill()
                        if e_ > 0:
                            for u in range(g8 * 4, g8 * 4 + 4):
                                scatter_unit(e_ - 1, u)
                    if e_ + 1 < NE:
                        build_dispatch(e_ + 1)
                    for n in range(4):
                        wt, wb = next_w()
                        for p in range(2):
                            k = hcnt[0] % 2
                            hcnt[0] += 1

                            def ym(e, wt=wt, p=p, k=k):
                                for fc in range(16):
                                    ins = e.matmul(psY2[k][:], lhsT=actT[:, fc, p * 128:(p + 1) * 128], rhs=wt[:, fc, :], start=(fc == 0), stop=(fc == 15))
                                return ins
                            S.op(PE, ym, reads=[wb, BactT], writes=[BpsY2[k]])
                            S.op(ACT, lambda e, k=k, p=p, n=n: e.activation(out=ysb[:, p, n * 512:(n + 1) * 512], in_=psY2[k][:], func=AF.Copy), reads=[BpsY2[k]], writes=[Bysb])
                        refill()
                        if e_ + 1 < NE:
                            for kq in range(n * 2, n * 2 + 2):
                                gather_unit(kq)
                for u in range(32):
                    scatter_unit(NE - 1, u)
                S.barrier()
            with ExitStack() as ph:
                gT_sb = sb(ph, [NE, T], F32, "gTsb")
                b2s = sb(ph, [NE, D], F32, "b2s")
                Bg_, Bb2 = Buf("gTsb"), Buf("b2s")
                S.dma(SP, b2s[:], b2, ch_misc, writes=[Bb2])
                gtf, Bgtf = bcast_row(ph, mod_d[5:6, :], ch_misc, reads=[B_mod[5]])
                g2, Bg2 = bcast_row(ph, lnp[2:3, :], ch_misc)
                b2r, Bb2r = bcast_row(ph, lnp[3:4, :], ch_misc)
                xin = sb(ph, [128, D], F32, "xin")
                yb = sb(ph, [128, D], F32, "yb")
                st = sb(ph, [128, 8], F32, "st")
                Bxin, Byb, Bst = Buf("xin"), Buf("yb"), Buf("st")
                chx = S.chan("xinG")
                cho = S.chan("outG")
                psGT = ps(ph, [NE, 128])
                psM = ps(ph, [128, 4, 512])
                BpsGT, BpsM = Buf("psGT"), Buf("psM")
                for j in range(8):
                    S.op(PE, lambda e, j=j: e.transpose(out=psGT[:], in_=gate[:, j, :], identity=C("ident")), reads=[Bgate], writes=[BpsGT])
                    S.op(ACT, lambda e, j=j: e.activation(out=gT_sb[:, j * 128:(j + 1) * 128], in_=psGT[:], func=AF.Copy), reads=[BpsGT], writes=[Bg_])
                for j in range(8):
                    tsl = slice(j * 128, (j + 1) * 128)
                    S.dma(SP, xin[:], x1_d[tsl, :], chx, reads=[B_x1d], writes=[Bxin])

                    def bm(e, tsl=tsl):
                        for n in range(4):
                            ins = e.matmul(psM[:, n, :], lhsT=gT_sb[:, tsl], rhs=b2s[:, n * 512:(n + 1) * 512], start=True, stop=True)
                        return ins
                    S.op(PE, bm, reads=[Bg_, Bb2], writes=[BpsM])
                    S.op(DVE, lambda e, j=j: e.tensor_tensor(out=yb[:], in0=psM[:].rearrange("p n c -> p (n c)"), in1=accF[:, j, :], op=ALU.add), reads=[BpsM, BaccF], writes=[Byb])
                    if debug:
                        S.dma(SP, dbg["ffn"][tsl, :], yb[:], cho, reads=[Byb])
                    S.op(DVE, lambda e: e.tensor_tensor(out=yb[:], in0=yb[:], in1=gtf[:], op=ALU.mult), reads=[Byb, Bgtf], writes=[Byb])
                    S.op(DVE, lambda e: e.scalar_tensor_tensor(out=yb[:], in0=xin[:], scalar=ALPHA, in1=yb[:], op0=ALU.mult, op1=ALU.add), reads=[Bxin, Byb], writes=[Byb])
                    ln_tile(yb, xin, st, Byb, Bxin, Bst, g2, b2r, Bg2, Bb2r)
                    S.dma(SP, out[tsl, :], yb[:], cho, reads=[Byb])
                S.barrier()
    return nc


def make_in_maps(inp, stage=99):
    f = lambda a: np.ascontiguousarray(np.asarray(a), dtype=np.float32)
    x = f(inp["x"])
    c = f(inp["c"])
    posn = np.asarray(inp["positions"]).astype(np.int32)
    shared = {
        "w_ada": f(inp["w_ada"][0]), "b_ada": f(inp["b_ada"][0]).reshape(1, -1), "w_in": f(inp["w_in"][0]),
        "hgrn_lb": f(inp["hgrn_lb"]).reshape(1, 2048),
        "gnw": np.ascontiguousarray(f(inp["gnorm_w"][0]).reshape(8, 128).T),
        "w_o": f(inp["w_o"][0]),
        "lnp": np.stack([f(inp["ln1_g"][0]), f(inp["ln1_b"][0]), f(inp["ln2_g"][0]), f(inp["ln2_b"][0])], 0),
        "router_w": np.ascontiguousarray(f(inp["router_w"][0]).reshape(16, 128, NE).transpose(1, 0, 2).reshape(128, 16 * NE)),
        "router_b": f(inp["router_b"][0]).reshape(1, NE),
    }
    if stage >= 5:
        shared["w1"] = f(inp["w1"][0])
        shared["w2"] = f(inp["w2"][0])
        shared["b1"] = np.ascontiguousarray(f(inp["b1"][0]).reshape(NE, 16, 128, 2).transpose(2, 0, 1, 3).reshape(128, NE * 32))
        shared["b2"] = f(inp["b2"][0])
    maps = []
    cc = [make_consts(0), make_consts(1)]
    for core in range(8):
        b, half = core // 2, core % 2
        m = dict(shared)
        m["x_own"] = np.ascontiguousarray(x[b, half * T:(half + 1) * T])
        m["x_ctx"] = np.ascontiguousarray(x[b, 0:T]) if half == 1 else np.zeros((T, D), np.float32)
        m["cT"] = np.ascontiguousarray(c[b].reshape(16, 128).T)
        pp = np.zeros((1, 2048), np.int32)
        pp[0, T:] = posn[b, half * T:(half + 1) * T]
        if half == 1:
            pp[0, :T] = posn[b, 0:T]
        m["pos"] = pp
        m["consts"] = cc[half]
        maps.append(m)
    return maps


_NC_CACHE = {}


def kernel(**inputs):
    if "nc" not in _NC_CACHE:
        _NC_CACHE["nc"] = build_nc()
    nc = _NC_CACHE["nc"]
    maps = make_in_maps(inputs)
    res = run_bass_kernel_spmd(nc, maps, core_ids=list(range(8)))
    outp = np.zeros((4, 2048, D), np.float32)
    for core in range(8):
        b, half = core // 2, core % 2
        outp[b, half * T:(half + 1) * T] = res.results[core]["out"]
    return outp
```

```python
import numpy as np
from contextlib import ExitStack
import concourse.bass as bass
import concourse.mybir as mybir
from concourse.bass_utils import run_bass_kernel_spmd

F32 = mybir.dt.float32
BF16 = mybir.dt.bfloat16
I32 = mybir.dt.int32
AF = mybir.ActivationFunctionType
ALU = mybir.AluOpType
AX = mybir.AxisListType

D = 2048
T = 1024
NE = 32
CAP = 256
ALPHA = 2.0 ** 0.25
PI = float(np.pi)
STRICT = False

_c = {}
_off = 0
for _n, _w in [("ident", 128), ("mcur", 128), ("mprev", 128), ("mprevctx", 128), ("mask3", 64), ("rmat", 128),
               ("ltri", 128), ("chunkind", 2), ("bdmask", 128), ("ustrict", 128), ("ones", 128), ("iota", 256),
               ("iotac", 2), ("invp", 1), ("signp", 1), ("flag", 1)]:
    _c[_n] = (_off, _w)
    _off += _w
NCONST = _off


def make_consts(half):
    c = np.zeros((128, NCONST), np.float32)

    def put(name, arr):
        o, w = _c[name]
        c[:, o:o + w] = arr
    k = np.arange(128)[:, None]
    q = np.arange(128)[None, :]
    flag = 1.0 if half == 1 else 0.0
    put("ident", np.eye(128))
    put("mcur", (k <= q))
    put("mprev", (k >= q))
    put("mprevctx", (k >= q) * flag)
    m3 = (k <= (np.arange(64)[None, :] + 64)).astype(np.float32)
    m3[:64] *= flag
    put("mask3", m3)
    rm = np.zeros((128, 128), np.float32)
    invp = np.zeros((128, 1), np.float32)
    signp = np.zeros((128, 1), np.float32)
    for m in range(128):
        d = m % 64
        if d < 8:
            rm[m + 8, m] = 1.0
            signp[m] = -1.0
        elif d < 16:
            rm[m - 8, m] = 1.0
            signp[m] = 1.0
        if d < 16:
            invp[m] = 500000.0 ** (-(2.0 * (d % 8)) / 16.0)
    put("rmat", rm)
    put("invp", invp)
    put("signp", signp)
    same = (k // 64) == (q // 64)
    put("ltri", (k <= q) & same)
    put("bdmask", (k <= q) & same)
    ci = np.zeros((128, 2), np.float32)
    ci[:64, 0] = 1
    ci[64:, 1] = 1
    put("chunkind", ci)
    put("ustrict", (k < q))
    put("ones", np.ones((128, 128)))
    put("iota", np.tile(np.arange(256, dtype=np.float32)[None], (128, 1)))
    put("iotac", np.stack([np.arange(128), np.arange(128) + 128], 1))
    put("flag", np.full((128, 1), flag))
    return c


class Buf:
    __slots__ = ("name", "w", "r", "chan")

    def __init__(self, name):
        self.name = name
        self.w = None
        self.r = []
        self.chan = None


class Eng:
    def __init__(self, name, h, sem):
        self.name, self.h, self.sem, self.cnt, self.seen = name, h, sem, 0, {}


class Chan:
    def __init__(self, sem):
        self.sem, self.cnt = sem, 0


class Sched:
    def __init__(self, nc, es):
        self.nc, self.es = nc, es
        self.nsem = 0
        self.pe = Eng("pe", nc.tensor, self.sem("pe"))
        self.act = Eng("act", nc.scalar, self.sem("act"))
        self.dve = Eng("dve", nc.vector, self.sem("dve"))
        self.pool = Eng("pool", nc.gpsimd, self.sem("pool"))
        self.sp = Eng("sp", nc.sync, self.sem("sp"))
        self.engs = [self.pe, self.act, self.dve, self.pool, self.sp]
        self.chans = []

    def sem(self, name):
        self.nsem += 1
        return self.es.enter_context(self.nc.semaphore("s_%s_%d" % (name, self.nsem)))

    def chan(self, name="c", real=True):
        c = Chan(self.sem(name))
        self.chans.append(c)
        return c

    def _wait(self, eng, ev):
        if ev is None:
            return
        sem, val = ev
        if eng.seen.get(sem, 0) >= val:
            return
        eng.h.wait_ge(sem, val)
        eng.seen[sem] = val

    def op(self, eng, fn, reads=(), writes=()):
        for b in reads:
            self._wait(eng, b.w)
        for b in writes:
            if b.w is not None and (STRICT or b.w[0] is not eng.sem):
                self._wait(eng, b.w)
            for ev in b.r:
                if STRICT or ev[0] is not eng.sem:
                    self._wait(eng, ev)
        ins = fn(eng.h)
        eng.cnt += 1
        ins.then_inc(eng.sem, 1)
        ev = (eng.sem, eng.cnt)
        for b in writes:
            b.w = ev
            b.r = []
        for b in reads:
            b.r = [e for e in b.r if e[0] is not eng.sem] + [ev]
        return ev

    def dma(self, q, out, in_, chan=None, reads=(), writes=(), cbuf=None):
        cb = cbuf if cbuf is not None else (writes[0] if writes else reads[0])
        if cb.chan is None:
            cb.chan = self.chan(cb.name)
        chan = cb.chan
        for b in reads:
            self._wait(q, b.w)
        for b in writes:
            if b.w is not None and b.w[0] is not chan.sem:
                self._wait(q, b.w)
            for ev in b.r:
                self._wait(q, ev)
        q.h.dma_start(out=out, in_=in_).then_inc(chan.sem, 16)
        chan.cnt += 16
        ev = (chan.sem, chan.cnt)
        for b in writes:
            b.w = ev
            b.r = []
        for b in reads:
            b.r = b.r + [ev]
        return ev

    def barrier(self):
        for e in self.engs:
            for f in self.engs:
                if f is not e and f.cnt > 0:
                    self._wait(e, (f.sem, f.cnt))
            for c in self.chans:
                if c.cnt > 0:
                    self._wait(e, (c.sem, c.cnt))


def build_nc(stage=99, debug=False):
    nc = bass.Bass("TRN2", target_bir_lowering=False)

    def din(name, shape, dt=F32):
        return nc.dram_tensor(name, list(shape), dt, kind="ExternalInput").ap()
    x_own = din("x_own", [T, D])
    x_ctx = din("x_ctx", [T, D])
    cT = din("cT", [128, 16])
    pos = din("pos", [1, 2048], I32)
    w_ada = din("w_ada", [D, 6 * D])
    b_ada = din("b_ada", [1, 6 * D])
    w_in = din("w_in", [D, 7168])
    hgrn_lb = din("hgrn_lb", [1, 2048])
    gnw = din("gnw", [128, 8])
    w_o = din("w_o", [D, D])
    lnp = din("lnp", [4, D])
    router_w = din("router_w", [128, 16 * NE])
    router_b = din("router_b", [1, NE])
    if stage >= 5:
        w1 = din("w1", [NE, D, 2 * D])
        b1 = din("b1", [128, NE * 32])
        w2 = din("w2", [NE, D, D])
        b2 = din("b2", [NE, D])
    consts = din("consts", [128, NCONST])
    out = nc.dram_tensor("out", [T, D], F32, kind="ExternalOutput").ap()
    mod_d = nc.dram_tensor("mod_d", [6, D], F32, kind="Internal").ap()
    x1_d = nc.dram_tensor("x1_d", [T, D], F32, kind="Internal").ap()
    dbg = {}
    if debug:
        dbg["mixT"] = nc.dram_tensor("d_mixT", [128, 16 * T], BF16, kind="ExternalOutput").ap()
        dbg["x1"] = nc.dram_tensor("d_x1", [T, D], F32, kind="ExternalOutput").ap()
        dbg["mod"] = nc.dram_tensor("d_mod", [6, D], F32, kind="ExternalOutput").ap()
        dbg["logits"] = nc.dram_tensor("d_logits", [128, 8 * NE], F32, kind="ExternalOutput").ap()
        dbg["ffn"] = nc.dram_tensor("d_ffn", [T, D], F32, kind="ExternalOutput").ap()

    with ExitStack() as es:
        S = Sched(nc, es)
        PE, ACT, DVE, POOL, SP = S.pe, S.act, S.dve, S.pool, S.sp
        uid = [0]

        def sb(scope, shape, dt, name="t"):
            uid[0] += 1
            return scope.enter_context(nc.sbuf_tensor("%s_%d" % (name, uid[0]), list(shape), dt))

        def ps(scope, shape, dt=F32, name="p"):
            uid[0] += 1
            return scope.enter_context(nc.psum_tensor("%s_%d" % (name, uid[0]), list(shape), dt))

        cst = sb(es, [128, NCONST], F32, "cst")
        cstb = sb(es, [128, NCONST], BF16, "cstb")
        B_cst = Buf("cst")
        ch_misc = S.chan("misc")
        S.dma(SP, cst[:], consts, ch_misc, writes=[B_cst])
        S.op(DVE, lambda e: e.tensor_copy(out=cstb[:], in_=cst[:]), reads=[B_cst], writes=[B_cst])

        def C(name, bf=False, rows=slice(0, 128)):
            o, w = _c[name]
            return (cstb if bf else cst)[rows, o:o + w]

        def make_ring(scope, nslot, ncols):
            ring = [sb(scope, [128, 16, ncols], BF16, "ring") for _ in range(nslot)]
            ring_b = [Buf("ring%d" % i) for i in range(nslot)]
            ring_c = [S.chan("ring") for _ in range(nslot)]
            ring_i = [0]

            def load_w(src2d, nc_):
                i = ring_i[0] % nslot
                ring_i[0] += 1
                S.dma(POOL, ring[i][:, :, 0:nc_], src2d.rearrange("(k p) c -> p k c", p=128), ring_c[i], writes=[ring_b[i]])
                return ring[i], ring_b[i]
            return load_w

        S.barrier()

        B_mod = [Buf("mod%d" % i) for i in range(6)]
        with ExitStack() as ph:
            NSLOT = 3
            load_w = make_ring(ph, NSLOT, 512)
            cts = sb(ph, [128, 16], F32)
            sg = sb(ph, [128, 16], F32)
            silb = sb(ph, [128, 16], BF16)
            brow = [sb(ph, [1, 512], F32) for _ in range(2)]
            mrow = [sb(ph, [1, 512], F32) for _ in range(2)]
            psA = [ps(ph, [1, 512]) for _ in range(2)]
            Bc, Bsil = Buf("c"), Buf("sil")
            Bbrow = [Buf("brow"), Buf("brow")]
            Bmrow = [Buf("mrow"), Buf("mrow")]
            BpsA = [Buf("psA"), Buf("psA")]
            ch_b = [S.chan("brow"), S.chan("brow")]
            ch_m = [S.chan("mrow"), S.chan("mrow")]
            S.dma(SP, cts[:], cT, ch_misc, writes=[Bc])
            S.op(ACT, lambda e: e.activation(out=sg[:], in_=cts[:], func=AF.Sigmoid), reads=[Bc], writes=[Bsil])
            S.op(DVE, lambda e: e.tensor_tensor(out=silb[:], in0=cts[:], in1=sg[:], op=ALU.mult), reads=[Bc, Bsil], writes=[Bsil])
            pend = [load_w(w_ada[:, g * 512:(g + 1) * 512], 512) for g in range(NSLOT)]
            for g in range(24):
                wt, wb = pend.pop(0)
                k = g % 2
                S.dma(SP, brow[k][:], b_ada[0:1, g * 512:(g + 1) * 512], ch_b[k], writes=[Bbrow[k]])

                def mm(e, wt=wt, k=k):
                    for kc in range(16):
                        ins = e.matmul(psA[k][:], lhsT=silb[:, kc:kc + 1], rhs=wt[:, kc, :], start=(kc == 0), stop=(kc == 15))
                    return ins
                S.op(PE, mm, reads=[wb, Bsil], writes=[BpsA[k]])
                addc = 1.0 if (g // 4) in (1, 2, 4, 5) else 0.0
                S.op(DVE, lambda e, k=k, addc=addc: e.scalar_tensor_tensor(out=mrow[k][:], in0=psA[k][:], scalar=addc, in1=brow[k][:], op0=ALU.add, op1=ALU.add),
                     reads=[BpsA[k], Bbrow[k]], writes=[Bmrow[k]])
                S.dma(SP, mod_d[g // 4:g // 4 + 1, (g % 4) * 512:(g % 4 + 1) * 512], mrow[k][:], ch_m[k], reads=[Bmrow[k]], writes=[B_mod[g // 4]], cbuf=Bmrow[k])
                if g + NSLOT < 24:
                    pend.append(load_w(w_ada[:, (g + NSLOT) * 512:(g + NSLOT + 1) * 512], 512))
            S.barrier()
        if debug:
            with ExitStack() as ph:
                t = sb(ph, [6, D], F32)
                Bt = Buf("t")
                S.dma(SP, t[:], mod_d, ch_misc, reads=B_mod, writes=[Bt])
                S.dma(SP, dbg["mod"], t[:], ch_misc, reads=[Bt])
                S.barrier()

        def bcast_row(scope, src_row_ap, chan, reads=()):
            t = sb(scope, [128, D], F32, "row")
            b = Buf("row")
            S.dma(SP, t[:], src_row_ap.partition_broadcast(128), chan, reads=reads, writes=[b])
            return t, b

        gate = sb(es, [128, 8, NE], F32, "gate")
        mfall = sb(es, [128, 8, NE], F32, "mfall")
        mb = sb(es, [128, 8, NE], BF16, "mb")
        posm = sb(es, [128, 8, NE], F32, "posm")
        posmb = sb(es, [128, 8, NE], BF16, "posmb")
        logit = sb(es, [128, 8, NE], F32, "logit")
        Bh2bf, Bgate, Bmf, Bmb, Bposm, Bposmb, Blogit = [Buf(n) for n in ("h2bf", "gate", "mf", "mb", "posm", "posmb", "logit")]
        mixT_scope = ExitStack()
        es.enter_context(mixT_scope)
        mixT = sb(mixT_scope, [128, 16, T], BF16, "mixT")
        B_mix = [Buf("mix%d" % i) for i in range(16)]
        h2bf = mixT[:].rearrange("p (j a) t -> p j (a t)", a=2)

        with ExitStack() as phBC:
            hT = sb(phBC, [128, 16, 2048], BF16, "hT")
            rope_scope = ExitStack()
            phBC.enter_context(rope_scope)
            Ct = sb(rope_scope, [128, 2048], BF16, "Ct")
            St = sb(rope_scope, [128, 2048], BF16, "St")
            with ExitStack() as ph2:
                posi = sb(ph2, [128, 2048], I32)
                t1 = sb(ph2, [128, 2048], F32)
                t2 = sb(ph2, [128, 2048], F32)
                t3 = sb(ph2, [128, 2048], F32)
                Bp, B1, B2, B3, BCt, BSt = [Buf(n) for n in ("posi", "t1", "t2", "t3", "Ct", "St")]
                S.dma(SP, posi[:], pos.partition_broadcast(128), ch_misc, writes=[Bp])
                S.op(DVE, lambda e: e.tensor_copy(out=t1[:], in_=posi[:]), reads=[Bp], writes=[B1])
                S.op(DVE, lambda e: e.tensor_scalar(out=t1[:], in0=t1[:], scalar1=C("invp"), scalar2=None, op0=ALU.mult), reads=[B1], writes=[B1])
                for which, shift in (("sin", 0.0), ("cos", PI / 2)):
                    S.op(DVE, lambda e, shift=shift: e.tensor_scalar(out=t2[:], in0=t1[:], scalar1=shift, scalar2=None, op0=ALU.add), reads=[B1], writes=[B2])
                    S.op(DVE, lambda e: e.tensor_scalar(out=t3[:], in0=t2[:], scalar1=1.0 / (2 * PI), scalar2=None, op0=ALU.mult), reads=[B2], writes=[B3])
                    S.op(DVE, lambda e: e.tensor_copy(out=posi[:], in_=t3[:]), reads=[B3], writes=[Bp])
                    S.op(DVE, lambda e: e.tensor_copy(out=t3[:], in_=posi[:]), reads=[Bp], writes=[B3])
                    S.op(DVE, lambda e: e.scalar_tensor_tensor(out=t3[:], in0=t3[:], scalar=-2 * PI, in1=t2[:], op0=ALU.mult, op1=ALU.add), reads=[B3, B2], writes=[B3])
                    S.op(DVE, lambda e: e.tensor_scalar(out=t3[:], in0=t3[:], scalar1=3.1415925, scalar2=-3.1415925, op0=ALU.min, op1=ALU.max), reads=[B3], writes=[B3])
                    S.op(ACT, lambda e: e.activation(out=t2[:], in_=t3[:], func=AF.Sin), reads=[B3], writes=[B2])
                    if which == "sin":
                        S.op(DVE, lambda e: e.tensor_scalar(out=St[:], in0=t2[:], scalar1=C("signp"), scalar2=None, op0=ALU.mult), reads=[B2], writes=[BSt])
                    else:
                        S.op(DVE, lambda e: e.tensor_copy(out=Ct[:], in_=t2[:]), reads=[B2], writes=[BCt])
                S.barrier()
            with ExitStack() as ph:
                sc1a, Bsc = bcast_row(ph, mod_d[1:2, :], ch_misc, reads=[B_mod[1]])
                sha, Bsh = bcast_row(ph, mod_d[0:1, :], ch_misc, reads=[B_mod[0]])
                xin = [sb(ph, [128, D], F32, "xin") for _ in range(2)]
                Bx = [Buf("xin"), Buf("xin")]
                chx = [S.chan("xin"), S.chan("xin")]
                hb = sb(ph, [128, D], BF16, "hb")
                Bhb = Buf("hb")
                pst = [ps(ph, [128, 8, 128], BF16) for _ in range(2)]
                Bpst = [Buf("pst"), Buf("pst")]
                BhT = Buf("hT")
                for tt in range(16):
                    k = tt % 2
                    src = x_ctx[tt * 128:(tt + 1) * 128, :] if tt < 8 else x_own[(tt - 8) * 128:(tt - 7) * 128, :]
                    S.dma(SP, xin[k][:], src, chx[k], writes=[Bx[k]])
                    S.op(DVE, lambda e, k=k: e.tensor_tensor(out=xin[k][:], in0=xin[k][:], in1=sc1a[:], op=ALU.mult), reads=[Bx[k], Bsc], writes=[Bx[k]])
                    S.op(DVE, lambda e, k=k: e.tensor_tensor(out=hb[:], in0=xin[k][:], in1=sha[:], op=ALU.add), reads=[Bx[k], Bsh], writes=[Bhb])
                    for hh in range(2):
                        def tr(e, hh=hh):
                            for j in range(8):
                                kc = hh * 8 + j
                                ins = e.transpose(out=pst[hh][:, j, :], in_=hb[:, kc * 128:(kc + 1) * 128], identity=C("ident", True))
                            return ins
                        S.op(PE, tr, reads=[Bhb], writes=[Bpst[hh]])
                        S.op(ACT, lambda e, hh=hh, tt=tt: e.activation(out=hT[:, hh * 8:(hh + 1) * 8, tt * 128:(tt + 1) * 128], in_=pst[hh][:], func=AF.Copy),
                             reads=[Bpst[hh]], writes=[BhT])
                S.barrier()
            if stage >= 2:
                with ExitStack() as ph:
                    load_w = make_ring(ph, 3, 256)
                    V = {o: sb(ph, [128, 16, 256], BF16, "V%d" % o) for o in (1, 4, 16)}
                    QT = sb(ph, [128, 2, T], BF16, "QT")
                    KT = sb(ph, [128, 2, 2048], BF16, "KT")
                    qraw = [sb(ph, [128, 512], BF16, "qraw") for _ in range(2)]
                    ta = sb(ph, [128, 512], F32, "ta")
                    tb = sb(ph, [128, 512], F32, "tb")
                    acc = sb(ph, [128, 2, T], F32, "acc")
                    rden = sb(ph, [128, T], F32, "rden")
                    mpair = sb(ph, [128, 3, 256], BF16, "mpair")
                    pe_sb = [sb(ph, [128, 256], BF16, "pe") for _ in range(4)]
                    pm_sb = [sb(ph, [128, 256], BF16, "pm") for _ in range(4)]
                    psP = [ps(ph, [128, 512]) for _ in range(2)]
                    psS = [ps(ph, [128, 512]) for _ in range(2)]
                    psO_ = [ps(ph, [128, 2, 512]) for _ in range(2)]
                    BV = {o: Buf("V") for o in (1, 4, 16)}
                    BQT, BKT, Bta, Btb, Bacc, Brden, Bmp = [Buf(n) for n in ("QT", "KT", "ta", "tb", "acc", "rden", "mp")]
                    Bqraw = [Buf("qraw"), Buf("qraw")]
                    Bpe = [Buf("pe") for _ in range(4)]
                    Bpm = [Buf("pm") for _ in range(4)]
                    BpsP = [Buf("psP"), Buf("psP")]
                    BpsS = [Buf("psS") for _ in range(4)]
                    BpsO_ = [Buf("psO"), Buf("psO")]
                    ocnt = [0]
                    onesb = C("ones", True)
                    S.op(DVE, lambda e: e.tensor_copy(out=mpair[:, 0, 0:128], in_=C("mprev", True)), writes=[Bmp])
                    S.op(DVE, lambda e: e.tensor_copy(out=mpair[:, 0, 128:256], in_=C("mcur", True)), writes=[Bmp])
                    S.op(DVE, lambda e: e.tensor_copy(out=mpair[:, 1, 0:128], in_=C("mprevctx", True)), writes=[Bmp])
                    S.op(DVE, lambda e: e.tensor_copy(out=mpair[:, 1, 128:256], in_=C("mcur", True)), writes=[Bmp])
                    for s4 in range(4):
                        S.op(DVE, lambda e, s4=s4: e.tensor_copy(out=mpair[:, 2, s4 * 64:(s4 + 1) * 64], in_=C("mask3", True)), writes=[Bmp])
                    pcnt = [0]
                    scnt = [0]

                    def proj_a(wt, wb, colsl, tok0, dst_ap, scale, BD):
                        k = pcnt[0] % 2
                        pcnt[0] += 1

                        def mm(e):
                            for kc in range(16):
                                ins = e.matmul(psP[k][:], lhsT=wt[:, kc, colsl], rhs=hT[:, kc, tok0:tok0 + 512], start=(kc == 0), stop=(kc == 15))
                            return ins
                        S.op(PE, mm, reads=[wb], writes=[BpsP[k]])
                        S.op(ACT, lambda e: e.activation(out=qraw[k][:], in_=psP[k][:], func=AF.Identity, scale=scale), reads=[BpsP[k]], writes=[Bqraw[k]])
                        return (k, tok0, dst_ap, BD)

                    def proj_b(st_):
                        k, tok0, dst_ap, BD = st_
                        S.op(PE, lambda e: e.matmul(psP[k][:], lhsT=C("rmat", True), rhs=qraw[k][:], start=True, stop=True), reads=[Bqraw[k]], writes=[BpsP[k]])
                        S.op(DVE, lambda e: e.tensor_tensor(out=ta[:], in0=qraw[k][:], in1=Ct[:, tok0:tok0 + 512], op=ALU.mult), reads=[Bqraw[k]], writes=[Bta])
                        S.op(DVE, lambda e: e.tensor_tensor(out=tb[:], in0=psP[k][:], in1=St[:, tok0:tok0 + 512], op=ALU.mult), reads=[BpsP[k]], writes=[Btb])
                        S.op(DVE, lambda e: e.tensor_tensor(out=dst_ap, in0=ta[:], in1=tb[:], op=ALU.add), reads=[Bta, Btb], writes=[BD])

                    def tokset(o, kt):
                        if o == 1:
                            return slice(kt * 128, (kt + 1) * 128)
                        if o == 4:
                            m, r = kt // 4, kt % 4
                            return slice(512 * m + r, 512 * (m + 1), 4)
                        return slice(kt, 2048, 16)

                    LAG = 2
                    dq = []

                    def defer(fn, is_pv=True):
                        dq.append((fn, is_pv))
                        while sum(1 for _, p in dq if p) > LAG:
                            f_, _p = dq.pop(0)
                            f_()
                        while dq and not dq[0][1]:
                            dq.pop(0)[0]()

                    def flush():
                        while dq:
                            dq.pop(0)[0]()

                    def score_unit(lhs_list, rhs_list, ncol, mask_ap, pv_list, out_col0, ob):
                        k = scnt[0] % 4
                        k2 = scnt[0] % 2
                        scnt[0] += 1
                        n = len(lhs_list)
                        psO = psO_[ob]
                        BpsO = BpsO_[ob]

                        def mm(e):
                            for i in range(n):
                                ins = e.matmul(psS[k2][:, i * ncol:(i + 1) * ncol], lhsT=lhs_list[i], rhs=rhs_list[i], start=True, stop=True)
                            return ins
                        S.op(PE, mm, reads=[BKT, BQT], writes=[BpsS[k2]])
                        S.op(ACT, lambda e: e.activation(out=pe_sb[k][:, 0:n * ncol], in_=psS[k2][:, 0:n * ncol], func=AF.Exp), reads=[BpsS[k2]], writes=[Bpe[k]])
                        S.op(DVE, lambda e: e.tensor_tensor(out=pm_sb[k][:, 0:n * ncol], in0=pe_sb[k][:, 0:n * ncol], in1=mask_ap, op=ALU.mult), reads=[Bpe[k], Bmp], writes=[Bpm[k]])

                        def pv(e):
                            for t_ in range(2):
                                for i in range(n):
                                    vt, oc, st, sp_ = pv_list[i]
                                    lhs = vt if t_ == 0 else onesb
                                    ins = e.matmul(psO[:, t_, out_col0 + oc:out_col0 + oc + ncol], lhsT=lhs, rhs=pm_sb[k][:, i * ncol:(i + 1) * ncol], start=st, stop=sp_)
                            return ins
                        defer(lambda: S.op(PE, pv, reads=[Bpm[k], BV[1], BV[4], BV[16]], writes=[BpsO]))

                    for grp in range(4):
                        c0 = grp * 256
                        wq, wqb = load_w(w_in[:, c0:c0 + 256], 256)
                        wk, wkb = load_w(w_in[:, 1024 + c0:1024 + c0 + 256], 256)
                        wv, wvb = load_w(w_in[:, 2048 + c0:2048 + c0 + 256], 256)
                        plist = []
                        for j in range(2):
                            for nt in range(2):
                                plist.append((wq, wqb, slice(j * 128, (j + 1) * 128), 1024 + nt * 512, QT[:, j, nt * 512:(nt + 1) * 512], 0.125, BQT))
                            for nt in range(4):
                                plist.append((wk, wkb, slice(j * 128, (j + 1) * 128), nt * 512, KT[:, j, nt * 512:(nt + 1) * 512], 1.0, BKT))
                        prev_ = None
                        for pa in plist:
                            cur_ = proj_a(*pa)
                            if prev_ is not None:
                                proj_b(prev_)
                            prev_ = cur_
                        proj_b(prev_)
                        for o in (1, 4, 16):
                            for kt in range(16):
                                k = pcnt[0] % 2
                                pcnt[0] += 1
                                tok = tokset(o, kt)

                                def mm(e, k=k, tok=tok):
                                    for kc in range(16):
                                        ins = e.matmul(psP[k][:, 0:256], lhsT=hT[:, kc, tok], rhs=wv[:, kc, 0:256], start=(kc == 0), stop=(kc == 15))
                                    return ins
                                S.op(PE, mm, reads=[wvb], writes=[BpsP[k]])
                                S.op(ACT, lambda e, k=k, o=o, kt=kt: e.activation(out=V[o][:, kt, :], in_=psP[k][:, 0:256], func=AF.Copy), reads=[BpsP[k]], writes=[BV[o]])
                        for j in range(2):
                            for hh in range(2):
                                pb = 64 * hh
                                KTh = KT[pb:pb + 64, j, :]
                                QTh = QT[pb:pb + 64, j, :]
                                vc = slice(j * 128, (j + 1) * 128)
                                for batch in range(2):
                                    ob = ocnt[0] % 2
                                    ocnt[0] += 1
                                    for bi in range(4):
                                        n = 8 + batch * 4 + bi
                                        qs = slice((n - 8) * 128, (n - 7) * 128)
                                        score_unit([KTh[:, (n - 1) * 128:n * 128], KTh[:, n * 128:(n + 1) * 128]], [QTh[:, qs], QTh[:, qs]], 128,
                                                   mpair[:, 1 if n == 8 else 0, :],
                                                   [(V[1][:, n - 1, vc], 0, True, False), (V[1][:, n, vc], 0, False, True)], bi * 128, ob)
                                    defer(lambda batch=batch, ob=ob: S.op(DVE, lambda e: e.tensor_copy(out=acc[:, :, batch * 512:(batch + 1) * 512], in_=psO_[ob][:]), reads=[BpsO_[ob]], writes=[Bacc]), False)
                                for m in (2, 3):
                                    ob = ocnt[0] % 2
                                    ocnt[0] += 1
                                    for r in range(4):
                                        kp = slice(512 * (m - 1) + r, 512 * m, 4)
                                        kcu = slice(512 * m + r, 512 * (m + 1), 4)
                                        qs = slice(512 * (m - 2) + r, 512 * (m - 1), 4)
                                        score_unit([KTh[:, kp], KTh[:, kcu]], [QTh[:, qs], QTh[:, qs]], 128, mpair[:, 1 if m == 2 else 0, :],
                                                   [(V[4][:, 4 * (m - 1) + r, vc], 0, True, False), (V[4][:, 4 * m + r, vc], 0, False, True)], r * 128, ob)
                                    av = acc[:, :, 512 * (m - 2):512 * (m - 1)].rearrange("p t (i r) -> p t r i", r=4)
                                    defer(lambda av=av, ob=ob: S.op(DVE, lambda e: e.tensor_tensor(out=av, in0=av, in1=psO_[ob][:].rearrange("p t (r i) -> p t r i", r=4), op=ALU.add), reads=[BpsO_[ob], Bacc], writes=[Bacc]), False)
                                for h8 in range(2):
                                    ob = ocnt[0] % 2
                                    ocnt[0] += 1
                                    for rq in range(2):
                                        rs = [h8 * 8 + rq * 4 + s_ for s_ in range(4)]
                                        score_unit([KTh[:, r:2048:16] for r in rs], [QTh[:, r:1024:16] for r in rs], 64, mpair[:, 2, :],
                                                   [(V[16][:, r, vc], i_ * 64, True, True) for i_, r in enumerate(rs)], rq * 256, ob)
                                    av = acc[:, :, :].rearrange("p t (i r) -> p t r i", r=16)[:, :, h8 * 8:(h8 + 1) * 8, :]
                                    defer(lambda av=av, ob=ob: S.op(DVE, lambda e: e.tensor_tensor(out=av, in0=av, in1=psO_[ob][:].rearrange("p t (r i) -> p t r i", r=8), op=ALU.add), reads=[BpsO_[ob], Bacc], writes=[Bacc]), False)
                                flush()
                                ci_ = grp * 2 + j
                                S.op(DVE, lambda e, pb=pb: e.reciprocal(out=rden[pb:pb + 64, :], in_=acc[pb:pb + 64, 1, :]), reads=[Bacc], writes=[Brden])
                                S.op(DVE, lambda e, pb=pb, ci_=ci_: e.tensor_tensor(out=mixT[pb:pb + 64, ci_, :], in0=acc[pb:pb + 64, 0, :], in1=rden[pb:pb + 64, :], op=ALU.mult),
                                     reads=[Bacc, Brden], writes=[B_mix[ci_]])
                    S.barrier()
            rope_scope.close()
            if stage >= 2:
                with ExitStack() as ph:
                    load_w = make_ring(ph, 4, 256)
                    lb = sb(ph, [128, 1024], F32, "lb")
                    oml = sb(ph, [128, 1024], F32, "oml")
                    gnws = sb(ph, [128, 8], F32, "gnws")
                    ke = sb(ph, [128, 16, 256], BF16, "ke")
                    vv = sb(ph, [128, 16, 256], BF16, "vv")
                    keT = sb(ph, [128, 2, 2048], BF16, "keT")
                    qeT = sb(ph, [128, 2, T], BF16, "qeT")
                    gT = sb(ph, [128, 2, T], BF16, "gT")
                    ebl = sb(ph, [128, 2, 16, 2], F32, "ebl")
                    sgm = [sb(ph, [128, 256], F32, "sgm") for _ in range(2)]
                    gtmp = sb(ph, [128, 512], F32, "gtmp")
                    Bgtmp = Buf("gtmp")
                    fv = [sb(ph, [128, 256], F32, "fv") for _ in range(2)]
                    gl = [sb(ph, [128, 256], F32, "gl") for _ in range(2)]
                    kk = [sb(ph, [128, 256], F32, "kk") for _ in range(2)]
                    eb = [sb(ph, [128, 256], F32, "eb") for _ in range(2)]
                    enb = [sb(ph, [128, 256], F32, "enb") for _ in range(2)]
                    qe_t = [sb(ph, [128, 256], BF16, "qe_t") for _ in range(2)]
                    S32 = [sb(ph, [128, 128], F32, "S32") for _ in range(2)]
                    Sb_ = [sb(ph, [128, 128], BF16, "Sb") for _ in range(2)]
                    stmp = [sb(ph, [128, 128], F32, "stmp") for _ in range(2)]
                    aT = [sb(ph, [128, 128], BF16, "aT") for _ in range(2)]
                    sq = [sb(ph, [128, 128], BF16, "sq") for _ in range(2)]
                    rt = [sb(ph, [128, 128], F32, "rt") for _ in range(2)]
                    ot = [sb(ph, [128, 128], F32, "ot") for _ in range(2)]
                    psP = [ps(ph, [128, 512]) for _ in range(2)]
                    psB = ps(ph, [128, 512])
                    psT = ps(ph, [128, 4, 128], BF16)
                    psX = ps(ph, [128, 4, 128])
                    psOT = ps(ph, [128, 2, 128 * 2])
                    psD = [ps(ph, [128, 512]) for _ in range(2)]
                    (Blb, Bke, Bvv, BkeT, BqeT, BgT, Bebl, BpsB, BpsT, Bgnw) = [
                        Buf(n) for n in ("lb", "ke", "vv", "keT", "qeT", "gT", "ebl", "psB", "psT", "gnw")]
                    Bsgm, Bfv, Bgl, Bkk, Beb, Benb, Bqe = [[Buf(n), Buf(n)] for n in ("sgm", "fv", "gl", "kk", "eb", "enb", "qe")]
                    Bsq, Brt, Bot, BpsYd = [[Buf(n), Buf(n)] for n in ("sq", "rt", "ot", "psYd")]
                    _bx, _bo = Buf("psX"), Buf("psOT")
                    BpsXa, BpsXs, BpsYo = [_bx, _bx], [_bx, _bx], [_bo, _bo]
                    BS32 = [Buf("S32"), Buf("S32")]
                    BSb = [Buf("Sb"), Buf("Sb")]
                    Bstmp = [Buf("stmp"), Buf("stmp")]
                    BaT = [Buf("aT"), Buf("aT")]
                    BpsP = [Buf("psP"), Buf("psP")]
                    onesb = C("ones", True)
                    S.dma(SP, gnws[:], gnw, ch_misc, writes=[Bgnw])
                    S.dma(SP, lb[:], hgrn_lb[0:1, 0:1024].partition_broadcast(128), ch_misc, writes=[Blb])
                    S.dma(SP, oml[:], hgrn_lb[0:1, 1024:2048].partition_broadcast(128), ch_misc, writes=[Blb])
                    S.op(DVE, lambda e: e.tensor_tensor(out=oml[:], in0=lb[:], in1=oml[:], op=ALU.subtract), reads=[Blb], writes=[Blb])
                    S.op(ACT, lambda e: e.activation(out=lb[:], in_=oml[:], func=AF.Sigmoid), reads=[Blb], writes=[Blb])
                    S.op(DVE, lambda e: e.tensor_scalar(out=oml[:], in0=lb[:], scalar1=-1.0, scalar2=1.0, op0=ALU.mult, op1=ALU.add), reads=[Blb], writes=[Blb])
                    pcnt = [0]

                    def proj_tm(wt, wb, tt):
                        k = pcnt[0] % 2
                        pcnt[0] += 1

                        def mm(e):
                            for kc in range(16):
                                ins = e.matmul(psP[k][:, 0:256], lhsT=hT[:, kc, tt * 128:(tt + 1) * 128], rhs=wt[:, kc, 0:256], start=(kc == 0), stop=(kc == 15))
                            return ins
                        S.op(PE, mm, reads=[wb], writes=[BpsP[k]])
                        return k

                    for hp in range(4):
                        wqr, wqrb = load_w(w_in[:, 3072 + hp * 256:3072 + (hp + 1) * 256], 256)
                        wf, wfb = load_w(w_in[:, 4096 + hp * 256:4096 + (hp + 1) * 256], 256)
                        wi, wib = load_w(w_in[:, 5120 + hp * 256:5120 + (hp + 1) * 256], 256)
                        wg, wgb = load_w(w_in[:, 6144 + hp * 256:6144 + (hp + 1) * 256], 256)
                        lsl = slice(hp * 256, (hp + 1) * 256)
                        for hd in range(2):
                            for nt in range(2):
                                k = pcnt[0] % 2
                                pcnt[0] += 1

                                def mm(e, k=k, hd=hd, nt=nt):
                                    for kc in range(16):
                                        ins = e.matmul(psP[k][:], lhsT=wg[:, kc, hd * 128:(hd + 1) * 128], rhs=hT[:, kc, 1024 + nt * 512:1024 + (nt + 1) * 512], start=(kc == 0), stop=(kc == 15))
                                    return ins
                                S.op(PE, mm, reads=[wgb], writes=[BpsP[k]])
                                S.op(ACT, lambda e, k=k: e.activation(out=gtmp[:], in_=psP[k][:], func=AF.Exp, scale=-1.0), reads=[BpsP[k]], writes=[Bgtmp])
                                S.op(DVE, lambda e: e.tensor_scalar(out=gtmp[:], in0=gtmp[:], scalar1=1.0, scalar2=None, op0=ALU.add), reads=[Bgtmp], writes=[Bgtmp])
                                S.op(DVE, lambda e: e.reciprocal(out=gtmp[:], in_=gtmp[:]), reads=[Bgtmp], writes=[Bgtmp])
                                S.op(DVE, lambda e, k=k, hd=hd, nt=nt: e.tensor_tensor(out=gT[:, hd, nt * 512:(nt + 1) * 512], in0=psP[k][:], in1=gtmp[:], op=ALU.mult), reads=[BpsP[k], Bgtmp], writes=[BgT])
                        def st1(tt):
                            b = tt % 2
                            k = proj_tm(wf, wfb, tt)
                            S.op(ACT, lambda e: e.activation(out=sgm[b][:], in_=psP[k][:, 0:256], func=AF.Exp, scale=-1.0), reads=[BpsP[k]], writes=[Bsgm[b]])
                            S.op(DVE, lambda e: e.tensor_scalar(out=sgm[b][:], in0=sgm[b][:], scalar1=1.0, scalar2=None, op0=ALU.add), reads=[Bsgm[b]], writes=[Bsgm[b]])
                            S.op(DVE, lambda e: e.reciprocal(out=sgm[b][:], in_=sgm[b][:]), reads=[Bsgm[b]], writes=[Bsgm[b]])
                            S.op(DVE, lambda e: e.tensor_tensor(out=fv[b][:], in0=sgm[b][:], in1=oml[:, lsl], op=ALU.mult), reads=[Bsgm[b], Blb], writes=[Bfv[b]])
                            S.op(DVE, lambda e: e.tensor_tensor(out=fv[b][:], in0=fv[b][:], in1=lb[:, lsl], op=ALU.add), reads=[Bfv[b], Blb], writes=[Bfv[b]])
                            S.op(ACT, lambda e: e.activation(out=gl[b][:], in_=fv[b][:], func=AF.Ln), reads=[Bfv[b]], writes=[Bgl[b]])
                            S.op(DVE, lambda e: e.tensor_scalar(out=kk[b][:], in0=fv[b][:], scalar1=-1.0, scalar2=1.0, op0=ALU.mult, op1=ALU.add), reads=[Bfv[b]], writes=[Bkk[b]])

                        def st2(tt):
                            own = tt >= 8
                            k = proj_tm(wi, wib, tt)
                            S.op(ACT, lambda e: e.activation(out=vv[:, tt, :], in_=psP[k][:, 0:256], func=AF.Copy), reads=[BpsP[k]], writes=[Bvv])
                            if own:
                                return proj_tm(wqr, wqrb, tt)
                            return None

                        def st3(tt, kq):
                            own = tt >= 8
                            b = tt % 2

                            def cs(e):
                                e.matmul(psB[:, 0:256], lhsT=C("ltri"), rhs=gl[b][:], start=True, stop=True)
                                for hd in range(2):
                                    ins = e.matmul(psB[:, 256 + 2 * hd:258 + 2 * hd], lhsT=gl[b][:, hd * 128:(hd + 1) * 128], rhs=C("chunkind"), start=True, stop=True)
                                return ins
                            S.op(PE, cs, reads=[Bgl[b]], writes=[BpsB])
                            S.op(ACT, lambda e: e.activation(out=enb[b][:], in_=psB[:, 0:256], func=AF.Exp, scale=-1.0), reads=[BpsB], writes=[Benb[b]])
                            if own:
                                S.op(ACT, lambda e: e.activation(out=eb[b][:], in_=psB[:, 0:256], func=AF.Exp), reads=[BpsB], writes=[Beb[b]])
                            S.op(ACT, lambda e: e.activation(out=ebl[:, :, tt, :], in_=psB[:, 256:260].rearrange("p (h c) -> p h c", c=2), func=AF.Exp), reads=[BpsB], writes=[Bebl])
                            S.op(DVE, lambda e: e.tensor_tensor(out=ke[:, tt, :], in0=kk[b][:], in1=enb[b][:], op=ALU.mult), reads=[Bkk[b], Benb[b]], writes=[Bke])
                            if own:
                                S.op(DVE, lambda e: e.tensor_tensor(out=qe_t[b][:], in0=psP[kq][:, 0:256], in1=eb[b][:], op=ALU.mult), reads=[BpsP[kq], Beb[b]], writes=[Bqe[b]])

                        def st4(tt):
                            own = tt >= 8
                            b = tt % 2

                            def tr(e):
                                for hd in range(2):
                                    ins = e.transpose(out=psT[:, hd, :], in_=ke[:, tt, hd * 128:(hd + 1) * 128], identity=C("ident", True))
                                if own:
                                    for hd in range(2):
                                        ins = e.transpose(out=psT[:, 2 + hd, :], in_=qe_t[b][:, hd * 128:(hd + 1) * 128], identity=C("ident", True))
                                return ins
                            S.op(PE, tr, reads=[Bke] + ([Bqe[b]] if own else []), writes=[BpsT])
                            S.op(ACT, lambda e: e.activation(out=keT[:, :, tt * 128:(tt + 1) * 128], in_=psT[:, 0:2, :], func=AF.Copy), reads=[BpsT], writes=[BkeT])
                            if own:
                                S.op(ACT, lambda e: e.activation(out=qeT[:, :, (tt - 8) * 128:(tt - 7) * 128], in_=psT[:, 2:4, :], func=AF.Copy), reads=[BpsT], writes=[BqeT])

                        st1(0)
                        for tt in range(16):
                            if tt + 1 < 16:
                                st1(tt + 1)
                            kq_ = st2(tt)
                            st3(tt, kq_)
                            if tt >= 1:
                                st4(tt - 1)
                        st4(15)
                        for hd in range(2):
                            S.op(DVE, lambda e, hd=hd: e.memset(S32[hd][:], 0.0), writes=[BS32[hd]])
                            S.op(ACT, lambda e, hd=hd: e.activation(out=Sb_[hd][:], in_=S32[hd][:], func=AF.Copy), reads=[BS32[hd]], writes=[BSb[hd]])
                        for tt in range(16):
                            own = tt >= 8
                            qsl = slice((tt - 8) * 128, (tt - 7) * 128)
                            if own:
                                for hd in range(2):
                                    S.op(PE, lambda e, hd=hd, tt=tt, qsl=qsl: e.matmul(psX[:, hd, :], lhsT=keT[:, hd, tt * 128:(tt + 1) * 128], rhs=qeT[:, hd, qsl], start=True, stop=True),
                                         reads=[BkeT, BqeT], writes=[BpsXa[hd]])
                                    S.op(DVE, lambda e, hd=hd: e.tensor_tensor(out=aT[hd][:], in0=psX[:, hd, :], in1=C("bdmask", True), op=ALU.mult), reads=[BpsXa[hd]], writes=[BaT[hd]])
                            for c in range(2):
                                rows = slice(c * 64, (c + 1) * 64)
                                for hd in range(2):
                                    hsl = slice(hd * 128, (hd + 1) * 128)
                                    if own:
                                        def om(e, hd=hd, tt=tt, c=c, rows=rows, hsl=hsl):
                                            e.matmul(psOT[:, hd, c * 64:(c + 1) * 64], lhsT=vv[rows, tt, hsl], rhs=aT[hd][rows, c * 64:(c + 1) * 64], start=True, stop=False)
                                            return e.matmul(psOT[:, hd, c * 64:(c + 1) * 64], lhsT=Sb_[hd][:], rhs=qeT[:, hd, (tt - 8) * 128 + c * 64:(tt - 8) * 128 + (c + 1) * 64], start=False, stop=True)
                                        S.op(PE, om, reads=[Bvv, BaT[hd], BSb[hd], BqeT], writes=[BpsYo[hd]])
                                    S.op(PE, lambda e, tt=tt, rows=rows, hsl=hsl, hd=hd: e.matmul(psD[hd][:, 0:128], lhsT=ke[rows, tt, hsl], rhs=vv[rows, tt, hsl], start=True, stop=True),
                                         reads=[Bke, Bvv], writes=[BpsYd[hd]])
                                    S.op(DVE, lambda e, hd=hd: e.tensor_tensor(out=stmp[hd][:], in0=psD[hd][:, 0:128], in1=S32[hd][:], op=ALU.add), reads=[BpsYd[hd], BS32[hd]], writes=[Bstmp[hd]])
                                    if tt == 7 and c == 1:
                                        S.op(DVE, lambda e, hd=hd: e.tensor_scalar(out=stmp[hd][:], in0=stmp[hd][:], scalar1=C("flag"), scalar2=None, op0=ALU.mult), reads=[Bstmp[hd]], writes=[Bstmp[hd]])
                                    S.op(ACT, lambda e, hd=hd, tt=tt, c=c: e.activation(out=Sb_[hd][:], in_=stmp[hd][:], func=AF.Identity, scale=ebl[:, hd, tt, c:c + 1]), reads=[Bstmp[hd], Bebl], writes=[BSb[hd]])
                                    S.op(DVE, lambda e, hd=hd, tt=tt, c=c: e.tensor_scalar(out=S32[hd][:], in0=stmp[hd][:], scalar1=ebl[:, hd, tt, c:c + 1], scalar2=None, op0=ALU.mult),
                                         reads=[Bstmp[hd], Bebl], writes=[BS32[hd]])
                            if own:
                                for hd in range(2):
                                    ci_ = 8 + hp * 2 + hd
                                    S.op(ACT, lambda e, hd=hd: e.activation(out=sq[hd][:], in_=psOT[:, hd, 0:128], func=AF.Square), reads=[BpsYo[hd]], writes=[Bsq[hd]])
                                    S.op(PE, lambda e, hd=hd: e.matmul(psX[:, 2 + hd, :], lhsT=onesb, rhs=sq[hd][:], start=True, stop=True), reads=[Bsq[hd]], writes=[BpsXs[hd]])
                                    S.op(ACT, lambda e, hd=hd: e.activation(out=rt[hd][:], in_=psX[:, 2 + hd, :], func=AF.Ln, scale=1.0 / 128.0, bias=1e-6), reads=[BpsXs[hd]], writes=[Brt[hd]])
                                    S.op(ACT, lambda e, hd=hd: e.activation(out=rt[hd][:], in_=rt[hd][:], func=AF.Exp, scale=-0.5), reads=[Brt[hd]], writes=[Brt[hd]])
                                    S.op(DVE, lambda e, hd=hd: e.tensor_tensor(out=ot[hd][:], in0=psOT[:, hd, 0:128], in1=rt[hd][:], op=ALU.mult), reads=[BpsYo[hd], Brt[hd]], writes=[Bot[hd]])
                                    S.op(DVE, lambda e, ci_=ci_, qsl=qsl, hd=hd, hp=hp: e.scalar_tensor_tensor(out=mixT[:, ci_, qsl], in0=ot[hd][:], scalar=gnws[:, hp * 2 + hd:hp * 2 + hd + 1], in1=gT[:, hd, qsl], op0=ALU.mult, op1=ALU.mult),
                                         reads=[Bot[hd], BgT, Bgnw], writes=[B_mix[ci_]])
                    S.barrier()
        if debug:
            S.dma(SP, dbg["mixT"], mixT[:].rearrange("p c t -> p (c t)"), ch_misc, reads=B_mix)
            S.barrier()
        if stage >= 3:
            def ln_tile(yb, junk, st, Byb, Bjunk, Bst, grow, brow_, Bg, Bb):
                S.op(DVE, lambda e: e.memset(st[:], 0.0), writes=[Bst])
                S.op(DVE, lambda e: e.reduce_sum(out=st[:, 0:1], in_=yb[:], axis=AX.X), reads=[Byb], writes=[Bst])
                S.op(DVE, lambda e: e.tensor_scalar(out=st[:, 1:2], in0=st[:, 0:1], scalar1=-1.0 / D, scalar2=None, op0=ALU.mult), reads=[Bst], writes=[Bst])
                S.op(DVE, lambda e: e.tensor_scalar(out=yb[:], in0=yb[:], scalar1=st[:, 1:2], scalar2=None, op0=ALU.add), reads=[Byb, Bst], writes=[Byb])
                S.op(ACT, lambda e: e.activation(out=junk[:], in_=yb[:], func=AF.Square, accum_out=st[:, 2:3]), reads=[Byb, Bst], writes=[Bjunk, Bst])
                S.op(ACT, lambda e: e.activation(out=st[:, 3:4], in_=st[:, 2:3], func=AF.Sqrt, scale=1.0 / D, bias=1e-5), reads=[Bst], writes=[Bst])
                S.op(DVE, lambda e: e.reciprocal(out=st[:, 4:5], in_=st[:, 3:4]), reads=[Bst], writes=[Bst])
                S.op(DVE, lambda e: e.scalar_tensor_tensor(out=yb[:], in0=yb[:], scalar=st[:, 4:5], in1=grow[:], op0=ALU.mult, op1=ALU.mult), reads=[Byb, Bst, Bg], writes=[Byb])
                S.op(DVE, lambda e: e.tensor_tensor(out=yb[:], in0=yb[:], in1=brow_[:], op=ALU.add), reads=[Byb, Bb], writes=[Byb])

            B_x1d = Buf("x1d")
            with ExitStack() as ph:
                wo = sb(ph, [128, 16, D], BF16, "wo")
                Bwo = Buf("wo")
                ch_wo = S.chan("wo")
                for n in range(4):
                    S.dma(POOL, wo[:, :, n * 512:(n + 1) * 512], w_o[:, n * 512:(n + 1) * 512].rearrange("(k p) c -> p k c", p=128), ch_wo, writes=[Bwo])
                gt1, Bgt1 = bcast_row(ph, mod_d[2:3, :], ch_misc, reads=[B_mod[2]])
                g1, Bg1 = bcast_row(ph, lnp[0:1, :], ch_misc)
                b1r, Bb1r = bcast_row(ph, lnp[1:2, :], ch_misc)
                scf, Bscf = bcast_row(ph, mod_d[4:5, :], ch_misc, reads=[B_mod[4]])
                shf, Bshf = bcast_row(ph, mod_d[3:4, :], ch_misc, reads=[B_mod[3]])
                rw = sb(ph, [128, 16, NE], F32, "rw")
                rb = sb(ph, [128, NE], F32, "rb")
                Brw = Buf("rw")
                S.dma(SP, rw[:], router_w.rearrange("p (k e) -> p k e", e=NE), ch_misc, writes=[Brw])
                S.dma(SP, rb[:], router_b.partition_broadcast(128), ch_misc, writes=[Brw])
                xin = sb(ph, [128, D], F32, "xin")
                yb = sb(ph, [128, D], F32, "yb")
                h2T = sb(ph, [128, 16, 128], F32, "h2T")
                st = sb(ph, [128, 8], F32, "st")
                t8 = sb(ph, [128, 8], F32, "t8")
                ex = sb(ph, [128, NE], F32, "ex")
                sm = sb(ph, [128, 4], F32, "sm")
                Bxin, Byb, Bh2T, Bst, Bt8, Bex, Bsm = [Buf(n) for n in ("xin", "yb", "h2T", "st", "t8", "ex", "sm")]
                chx = S.chan("xinD")
                chy = S.chan("x1st")
                psM = ps(ph, [128, 4, 512])
                psT4 = ps(ph, [128, 4, 128])
                psL = ps(ph, [128, NE])
                BpsM, BpsT4, BpsL = Buf("psM"), Buf("psT4"), Buf("psL")
                for j in range(8):
                    tsl = slice(j * 128, (j + 1) * 128)
                    S.dma(SP, xin[:], x_own[tsl, :], chx, writes=[Bxin])

                    def mm(e, tsl=tsl):
                        for n in range(4):
                            for kc in range(16):
                                ins = e.matmul(psM[:, n, :], lhsT=mixT[:, kc, tsl], rhs=wo[:, kc, n * 512:(n + 1) * 512], start=(kc == 0), stop=(kc == 15))
                        return ins
                    S.op(PE, mm, reads=[Bwo] + B_mix, writes=[BpsM])
                    S.op(DVE, lambda e: e.tensor_tensor(out=yb[:], in0=psM[:].rearrange("p n c -> p (n c)"), in1=gt1[:], op=ALU.mult), reads=[BpsM, Bgt1], writes=[Byb])
                    S.op(DVE, lambda e: e.scalar_tensor_tensor(out=yb[:], in0=xin[:], scalar=ALPHA, in1=yb[:], op0=ALU.mult, op1=ALU.add), reads=[Bxin, Byb], writes=[Byb])
                    ln_tile(yb, xin, st, Byb, Bxin, Bst, g1, b1r, Bg1, Bb1r)
                    S.dma(SP, x1_d[tsl, :], yb[:], chy, reads=[Byb], writes=[B_x1d], cbuf=Byb)
                    if debug:
                        S.dma(SP, dbg["x1"][tsl, :], yb[:], chy, reads=[Byb], cbuf=Buf("dbg"))
                S.barrier()
                for j in range(8):
                    tsl = slice(j * 128, (j + 1) * 128)
                    S.dma(SP, yb[:], x1_d[tsl, :], chx, reads=[B_x1d], writes=[Byb])
                    S.op(DVE, lambda e: e.tensor_tensor(out=xin[:], in0=yb[:], in1=scf[:], op=ALU.mult), reads=[Byb, Bscf], writes=[Bxin])
                    S.op(DVE, lambda e: e.tensor_tensor(out=xin[:], in0=xin[:], in1=shf[:], op=ALU.add), reads=[Bxin, Bshf], writes=[Bxin])
                    S.op(ACT, lambda e, j=j: e.activation(out=h2bf[:, j, :], in_=xin[:], func=AF.Copy), reads=[Bxin], writes=[Bh2bf])
                    for q4 in range(4):
                        def tr(e, q4=q4):
                            for i in range(4):
                                kc = q4 * 4 + i
                                ins = e.transpose(out=psT4[:, i, :], in_=xin[:, kc * 128:(kc + 1) * 128], identity=C("ident"))
                            return ins
                        S.op(PE, tr, reads=[Bxin], writes=[BpsT4])
                        S.op(ACT, lambda e, q4=q4: e.activation(out=h2T[:, q4 * 4:(q4 + 1) * 4, :], in_=psT4[:], func=AF.Copy), reads=[BpsT4], writes=[Bh2T])

                    def lg(e):
                        for kc in range(16):
                            ins = e.matmul(psL[:], lhsT=h2T[:, kc, :], rhs=rw[:, kc, :], start=(kc == 0), stop=(kc == 15))
                        return ins
                    S.op(PE, lg, reads=[Bh2T, Brw], writes=[BpsL])
                    S.op(DVE, lambda e, j=j: e.tensor_tensor(out=logit[:, j, :], in0=psL[:], in1=rb[:], op=ALU.add), reads=[BpsL, Brw], writes=[Blogit])
                    S.op(DVE, lambda e, j=j: e.max(out=t8[:], in_=logit[:, j, :]), reads=[Blogit], writes=[Bt8])
                    S.op(DVE, lambda e, j=j: e.tensor_scalar(out=mfall[:, j, :], in0=logit[:, j, :], scalar1=t8[:, 3:4], scalar2=None, op0=ALU.is_ge), reads=[Blogit, Bt8], writes=[Bmf])
                    S.op(DVE, lambda e: e.tensor_scalar(out=sm[:, 0:1], in0=t8[:, 0:1], scalar1=-1.0, scalar2=None, op0=ALU.mult), reads=[Bt8], writes=[Bsm])
                    S.op(ACT, lambda e, j=j: e.activation(out=ex[:], in_=logit[:, j, :], func=AF.Exp, bias=sm[:, 0:1], scale=1.0), reads=[Blogit, Bsm], writes=[Bex])
                    S.op(DVE, lambda e, j=j: e.tensor_tensor(out=ex[:], in0=ex[:], in1=mfall[:, j, :], op=ALU.mult), reads=[Bex, Bmf], writes=[Bex])
                    S.op(DVE, lambda e: e.reduce_sum(out=sm[:, 1:2], in_=ex[:], axis=AX.X), reads=[Bex], writes=[Bsm])
                    S.op(DVE, lambda e: e.reciprocal(out=sm[:, 2:3], in_=sm[:, 1:2]), reads=[Bsm], writes=[Bsm])
                    S.op(DVE, lambda e, j=j: e.tensor_scalar(out=gate[:, j, :], in0=ex[:], scalar1=sm[:, 2:3], scalar2=None, op0=ALU.mult), reads=[Bex, Bsm], writes=[Bgate])
                    S.op(DVE, lambda e, j=j: e.tensor_copy(out=mb[:, j, :], in_=mfall[:, j, :]), reads=[Bmf], writes=[Bmb])
                for j in range(8):
                    def pc(e, j=j):
                        for i in range(j):
                            e.matmul(psL[:], lhsT=C("ones", True), rhs=mb[:, i, :], start=(i == 0), stop=False)
                        return e.matmul(psL[:], lhsT=C("ustrict", True), rhs=mb[:, j, :], start=(j == 0), stop=True)
                    S.op(PE, pc, reads=[Bmb], writes=[BpsL])
                    S.op(DVE, lambda e, j=j: e.scalar_tensor_tensor(out=posm[:, j, :], in0=psL[:], scalar=1.0, in1=mfall[:, j, :], op0=ALU.add, op1=ALU.mult), reads=[BpsL, Bmf], writes=[Bposm])
                    S.op(DVE, lambda e, j=j: e.tensor_scalar(out=posm[:, j, :], in0=posm[:, j, :], scalar1=-1.0, scalar2=float(CAP), op0=ALU.add, op1=ALU.min), reads=[Bposm], writes=[Bposm])
                    S.op(DVE, lambda e, j=j: e.tensor_copy(out=posmb[:, j, :], in_=posm[:, j, :]), reads=[Bposm], writes=[Bposmb])
                if debug:
                    S.dma(SP, dbg["logits"], logit[:].rearrange("p j e -> p (j e)"), ch_misc, reads=[Blogit])
                S.barrier()
        if stage >= 5:
            accF_scope = ExitStack()
            es.enter_context(accF_scope)
            accF = sb(accF_scope, [128, 8, D], F32, "accF")
            BaccF = Buf("accF")
            S.op(DVE, lambda e: e.memset(accF[:], 0.0), writes=[BaccF])
            with ExitStack() as ph:
                NSLOT = 3
                load_w = make_ring(ph, NSLOT, 512)
                b1s = sb(ph, [128, NE * 32], F32, "b1s")
                Bb1 = Buf("b1s")
                S.dma(SP, b1s[:], b1, ch_misc, writes=[Bb1])
                Pe = sb(ph, [128, 8, CAP], BF16, "Pe")
                PT = [sb(ph, [128, 2, T], BF16, "PT") for _ in range(2)]
                xg = sb(ph, [128, 16, CAP], BF16, "xg")
                actT = sb(ph, [128, 16, CAP], BF16, "actT")
                ysb = sb(ph, [128, 2, D], BF16, "ysb")
                xgl = [sb(ph, [128, CAP], F32, "xgl") for _ in range(2)]
                sgd = [sb(ph, [128, CAP], F32, "sgd") for _ in range(2)]
                xl = [sb(ph, [128, CAP], F32, "xl") for _ in range(2)]
                BPe, Bxg, BactT, Bysb = [Buf(n) for n in ("Pe", "xg", "actT", "ysb")]
                BPT = [Buf("PT"), Buf("PT")]
                Bxgl = [Buf("xgl"), Buf("xgl")]
                Bsgd = [Buf("sgd"), Buf("sgd")]
                Bxl = [Buf("xl"), Buf("xl")]
                psG = ps(ph, [128, 2, CAP])
                psPT = psG[:].rearrange("p a c -> p (a c)")
                psH = [ps(ph, [128, 2, CAP]) for _ in range(2)]
                psY2 = [ps(ph, [128, 512])] * 2
                psSc = [ps(ph, [128, 512]) for _ in range(4)]
                BpsG = Buf("psG")
                BpsPT = BpsG
                BpsH = [Buf("psH"), Buf("psH")]
                _by = Buf("psY2")
                BpsY2 = [_by, _by]
                BpsSc = [Buf("psSc") for _ in range(4)]
                items = []
                for e_ in range(NE):
                    for g8 in range(8):
                        items.append(("w1", e_, g8))
                    for n in range(4):
                        items.append(("w2", e_, n))

                def issue(it):
                    kind, e_, g = it
                    if kind == "w1":
                        return load_w(w1[e_, :, g * 512:(g + 1) * 512], 512)
                    return load_w(w2[e_, :, g * 512:(g + 1) * 512], 512)
                pend = [issue(items[i]) for i in range(NSLOT)]
                nxt = [NSLOT]

                def next_w():
                    return pend.pop(0)

                def refill():
                    if nxt[0] < len(items):
                        pend.append(issue(items[nxt[0]]))
                        nxt[0] += 1
                hcnt = [0]
                ccnt = [0]

                def build_dispatch(e_):
                    for j in range(8):
                        S.op(DVE, lambda e, j=j: e.tensor_scalar(out=Pe[:, j, :], in0=C("iota")[:, 0:CAP], scalar1=posm[:, j, e_:e_ + 1], scalar2=None, op0=ALU.is_equal),
                             reads=[Bposm], writes=[BPe])
                    pt = PT[e_ % 2]
                    for hf_ in range(2):
                        def ptm(e, hf_=hf_):
                            for j4 in range(4):
                                j = hf_ * 4 + j4
                                ins = e.matmul(psPT[:, j4 * 128:(j4 + 1) * 128], lhsT=posmb[:, j, e_:e_ + 1].to_broadcast([128, 128]), rhs=C("ident", True), start=True, stop=True)
                            return ins
                        S.op(PE, ptm, reads=[Bposmb], writes=[BpsPT])
                        for p in range(2):
                            S.op(DVE, lambda e, p=p, hf_=hf_: e.tensor_scalar(out=pt[:, p, hf_ * 512:(hf_ + 1) * 512], in0=psPT, scalar1=C("iotac")[:, p:p + 1], scalar2=None, op0=ALU.is_equal),
                                 reads=[BpsPT], writes=[BPT[e_ % 2]])

                def gather_unit(kq):
                    def gm(e):
                        for k2 in range(2):
                            kc = kq * 2 + k2
                            for j in range(8):
                                ins = e.matmul(psG[:, k2, :], lhsT=h2bf[:, j, kc * 128:(kc + 1) * 128], rhs=Pe[:, j, :], start=(j == 0), stop=(j == 7))
                        return ins
                    S.op(PE, gm, reads=[Bh2bf, BPe], writes=[BpsG])
                    S.op(ACT, lambda e: e.activation(out=xg[:, kq * 2:(kq + 1) * 2, :], in_=psG[:], func=AF.Copy), reads=[BpsG], writes=[Bxg])

                def scatter_unit(e_, u):
                    j, n = u // 4, u % 4
                    k = ccnt[0] % 4
                    ccnt[0] += 1
                    pt = PT[e_ % 2]

                    def sm_(e):
                        for p in range(2):
                            ins = e.matmul(psSc[k][:], lhsT=pt[:, p, j * 128:(j + 1) * 128], rhs=ysb[:, p, n * 512:(n + 1) * 512], start=(p == 0), stop=(p == 1))
                        return ins
                    S.op(PE, sm_, reads=[BPT[e_ % 2], Bysb], writes=[BpsSc[k]])
                    av = accF[:, j, n * 512:(n + 1) * 512]
                    S.op(DVE, lambda e: e.scalar_tensor_tensor(out=av, in0=psSc[k][:], scalar=gate[:, j, e_:e_ + 1], in1=av, op0=ALU.mult, op1=ALU.add),
                         reads=[BpsSc[k], Bgate, BaccF], writes=[BaccF])

                build_dispatch(0)
                for kq in range(8):
                    gather_unit(kq)
                for e_ in range(NE):
                    for g8 in range(8):
                        wt, wb = next_w()
                        for f2 in range(2):
                            fc = g8 * 2 + f2
                            k = hcnt[0] % 2
                            hcnt[0] += 1

                            def hm(e, wt=wt, f2=f2, k=k):
                                for t_ in range(2):
                                    for kc in range(16):
                                        ins = e.matmul(psH[k][:, t_, :], lhsT=wt[:, kc, f2 * 256 + t_:(f2 + 1) * 256:2], rhs=xg[:, kc, :], start=(kc == 0), stop=(kc == 15))
                                return ins
                            S.op(PE, hm, reads=[wb, Bxg], writes=[BpsH[k]])
                            bi = e_ * 32 + fc * 2
                            S.op(DVE, lambda e, k=k, bi=bi: e.tensor_scalar(out=xgl[k][:], in0=psH[k][:, 0, :], scalar1=b1s[:, bi:bi + 1], scalar2=7.0, op0=ALU.add, op1=ALU.min), reads=[BpsH[k], Bb1], writes=[Bxgl[k]])
                            S.op(ACT, lambda e, k=k: e.activation(out=sgd[k][:], in_=xgl[k][:], func=AF.Sigmoid, scale=1.702), reads=[Bxgl[k]], writes=[Bsgd[k]])
                            S.op(DVE, lambda e, k=k, bi=bi: e.tensor_scalar(out=xl[k][:], in0=psH[k][:, 1, :], scalar1=b1s[:, bi + 1:bi + 2], scalar2=7.0, op0=ALU.add, op1=ALU.min), reads=[BpsH[k], Bb1], writes=[Bxl[k]])
                            S.op(DVE, lambda e, k=k: e.tensor_scalar(out=xl[k][:], in0=xl[k][:], scalar1=-7.0, scalar2=1.0, op0=ALU.max, op1=ALU.add), reads=[Bxl[k]], writes=[Bxl[k]])
                            S.op(DVE, lambda e, k=k: e.tensor_tensor(out=xgl[k][:], in0=xgl[k][:], in1=sgd[k][:], op=ALU.mult), reads=[Bxgl[k], Bsgd[k]], writes=[Bxgl[k]])
                            S.op(DVE, lambda e, fc=fc, k=k: e.tensor_tensor(out=actT[:, fc, :], in0=xgl[k][:], in1=xl[k][:], op=ALU.mult), reads=[Bxgl[k], Bxl[k]], writes=[BactT])
                        refill()
                        if e_ > 0:
                            for u in range(g8 * 4, g8 * 4 + 4):
                                scatter_unit(e_ - 1, u)
                    if e_ + 1 < NE:
                        build_dispatch(e_ + 1)
                    for n in range(4):
                        wt, wb = next_w()
                        for p in range(2):
                            k = hcnt[0] % 2
                            hcnt[0] += 1

                            def ym(e, wt=wt, p=p, k=k):
                                for fc in range(16):
                                    ins = e.matmul(psY2[k][:], lhsT=actT[:, fc, p * 128:(p + 1) * 128], rhs=wt[:, fc, :], start=(fc == 0), stop=(fc == 15))
                                return ins
                            S.op(PE, ym, reads=[wb, BactT], writes=[BpsY2[k]])
                            S.op(ACT, lambda e, k=k, p=p, n=n: e.activation(out=ysb[:, p, n * 512:(n + 1) * 512], in_=psY2[k][:], func=AF.Copy), reads=[BpsY2[k]], writes=[Bysb])
                        refill()
                        if e_ + 1 < NE:
                            for kq in range(n * 2, n * 2 + 2):
                                gather_unit(kq)
                for u in range(32):
                    scatter_unit(NE - 1, u)
                S.barrier()
            with ExitStack() as ph:
                gT_sb = sb(ph, [NE, T], F32, "gTsb")
                b2s = sb(ph, [NE, D], F32, "b2s")
                Bg_, Bb2 = Buf("gTsb"), Buf("b2s")
                S.dma(SP, b2s[:], b2, ch_misc, writes=[Bb2])
                gtf, Bgtf = bcast_row(ph, mod_d[5:6, :], ch_misc, reads=[B_mod[5]])
                g2, Bg2 = bcast_row(ph, lnp[2:3, :], ch_misc)
                b2r, Bb2r = bcast_row(ph, lnp[3:4, :], ch_misc)
                xin = sb(ph, [128, D], F32, "xin")
                yb = sb(ph, [128, D], F32, "yb")
                st = sb(ph, [128, 8], F32, "st")
                Bxin, Byb, Bst = Buf("xin"), Buf("yb"), Buf("st")
                chx = S.chan("xinG")
                cho = S.chan("outG")
                psGT = ps(ph, [NE, 128])
                psM = ps(ph, [128, 4, 512])
                BpsGT, BpsM = Buf("psGT"), Buf("psM")
                for j in range(8):
                    S.op(PE, lambda e, j=j: e.transpose(out=psGT[:], in_=gate[:, j, :], identity=C("ident")), reads=[Bgate], writes=[BpsGT])
                    S.op(ACT, lambda e, j=j: e.activation(out=gT_sb[:, j * 128:(j + 1) * 128], in_=psGT[:], func=AF.Copy), reads=[BpsGT], writes=[Bg_])
                for j in range(8):
                    tsl = slice(j * 128, (j + 1) * 128)
                    S.dma(SP, xin[:], x1_d[tsl, :], chx, reads=[B_x1d], writes=[Bxin])

                    def bm(e, tsl=tsl):
                        for n in range(4):
                            ins = e.matmul(psM[:, n, :], lhsT=gT_sb[:, tsl], rhs=b2s[:, n * 512:(n + 1) * 512], start=True, stop=True)
                        return ins
                    S.op(PE, bm, reads=[Bg_, Bb2], writes=[BpsM])
                    S.op(DVE, lambda e, j=j: e.tensor_tensor(out=yb[:], in0=psM[:].rearrange("p n c -> p (n c)"), in1=accF[:, j, :], op=ALU.add), reads=[BpsM, BaccF], writes=[Byb])
                    if debug:
                        S.dma(SP, dbg["ffn"][tsl, :], yb[:], cho, reads=[Byb], cbuf=Buf("dbg"))
                    S.op(DVE, lambda e: e.tensor_tensor(out=yb[:], in0=yb[:], in1=gtf[:], op=ALU.mult), reads=[Byb, Bgtf], writes=[Byb])
                    S.op(DVE, lambda e: e.scalar_tensor_tensor(out=yb[:], in0=xin[:], scalar=ALPHA, in1=yb[:], op0=ALU.mult, op1=ALU.add), reads=[Bxin, Byb], writes=[Byb])
                    ln_tile(yb, xin, st, Byb, Bxin, Bst, g2, b2r, Bg2, Bb2r)
                    S.dma(SP, out[tsl, :], yb[:], cho, reads=[Byb])
                S.barrier()
    return nc


def make_in_maps(inp, stage=99):
    f = lambda a: np.ascontiguousarray(np.asarray(a), dtype=np.float32)
    x = f(inp["x"])
    c = f(inp["c"])
    posn = np.asarray(inp["positions"]).astype(np.int32)
    shared = {
        "w_ada": f(inp["w_ada"][0]), "b_ada": f(inp["b_ada"][0]).reshape(1, -1), "w_in": f(inp["w_in"][0]),
        "hgrn_lb": f(inp["hgrn_lb"]).reshape(1, 2048),
        "gnw": np.ascontiguousarray(f(inp["gnorm_w"][0]).reshape(8, 128).T),
        "w_o": f(inp["w_o"][0]),
        "lnp": np.stack([f(inp["ln1_g"][0]), f(inp["ln1_b"][0]), f(inp["ln2_g"][0]), f(inp["ln2_b"][0])], 0),
        "router_w": np.ascontiguousarray(f(inp["router_w"][0]).reshape(16, 128, NE).transpose(1, 0, 2).reshape(128, 16 * NE)),
        "router_b": f(inp["router_b"][0]).reshape(1, NE),
    }
    if stage >= 5:
        shared["w1"] = f(inp["w1"][0])
        shared["w2"] = f(inp["w2"][0])
        shared["b1"] = np.ascontiguousarray(f(inp["b1"][0]).reshape(NE, 16, 128, 2).transpose(2, 0, 1, 3).reshape(128, NE * 32))
        shared["b2"] = f(inp["b2"][0])
    maps = []
    cc = [make_consts(0), make_consts(1)]
    for core in range(8):
        b, half = core // 2, core % 2
        m = dict(shared)
        m["x_own"] = np.ascontiguousarray(x[b, half * T:(half + 1) * T])
        m["x_ctx"] = np.ascontiguousarray(x[b, 0:T]) if half == 1 else np.zeros((T, D), np.float32)
        m["cT"] = np.ascontiguousarray(c[b].reshape(16, 128).T)
        pp = np.zeros((1, 2048), np.int32)
        pp[0, T:] = posn[b, half * T:(half + 1) * T]
        if half == 1:
            pp[0, :T] = posn[b, 0:T]
        m["pos"] = pp
        m["consts"] = cc[half]
        maps.append(m)
    return maps


_NC_CACHE = {}


def kernel(**inputs):
    if "nc" not in _NC_CACHE:
        _NC_CACHE["nc"] = build_nc()
    nc = _NC_CACHE["nc"]
    maps = make_in_maps(inputs)
    res = run_bass_kernel_spmd(nc, maps, core_ids=list(range(8)))
    outp = np.zeros((4, 2048, D), np.float32)
    for core in range(8):
        b, half = core // 2, core % 2
        outp[b, half * T:(half + 1) * T] = res.results[core]["out"]
    return outp
```
